# Optimizing a Trainium2 kernel written in Bass

```python
import jax, jax.numpy as jnp
from jax import lax
import numpy as np

D_MODEL = 1024
BATCH = 4
SEQ = 4096
DEPTH = 2

N_EVEN = (DEPTH + 1) // 2
N_ODD = DEPTH // 2
D_FF = 2816
RMS_EPS = 1e-6
ROPE_THETA = 10000.0

GLA_HEADS = 4
GLA_DK = 64
GLA_DV = 128
GLA_GATE_RANK = 16
GLA_GATE_TAU = 16.0
GLA_CHUNK = 64

POOL_WINDOWS = (2, 4, 8, 16)
POOL_GROUPS = 4
POOL_GROUP_DIM = 128
POOL_DIM = POOL_GROUPS * POOL_GROUP_DIM

ATT_HEADS = 8
ATT_KV_HEADS = 2
ATT_HEAD_DIM = 128
IDX_HEADS = 4
IDX_DIM = 64
TOPK_MAX = 256
Q_BLOCK = 128

EVEN_SIZES = (GLA_HEADS * GLA_DK, GLA_HEADS * GLA_DK, GLA_HEADS * GLA_DV, GLA_HEADS * GLA_DV, GLA_GATE_RANK, POOL_DIM)
EVEN_IN = sum(EVEN_SIZES)
EVEN_SPLITS = tuple(int(s) for s in np.cumsum(EVEN_SIZES)[:-1])
EVEN_MIX = GLA_HEADS * GLA_DV + POOL_DIM
ODD_SIZES = (ATT_HEADS * ATT_HEAD_DIM, ATT_KV_HEADS * ATT_HEAD_DIM, ATT_KV_HEADS * ATT_HEAD_DIM, IDX_HEADS * IDX_DIM, IDX_DIM, IDX_HEADS)
ODD_IN = sum(ODD_SIZES)
ODD_SPLITS = tuple(int(s) for s in np.cumsum(ODD_SIZES)[:-1])
ODD_MIX = ATT_HEADS * ATT_HEAD_DIM

kernel_name = "hybrid_gla_pool_dsa_macaron"


def rms_norm(x, g):
    xf = x.astype(jnp.float32)
    y = xf * lax.rsqrt(jnp.mean(xf * xf, axis=-1, keepdims=True) + RMS_EPS)
    return (y * g.astype(jnp.float32)).astype(x.dtype)


def swiglu(x, wi, wo):
    gate, up = jnp.split(x @ wi, 2, axis=-1)
    return (jax.nn.silu(gate) * up) @ wo


def rope(x, positions):
    d = x.shape[-1]
    inv = ROPE_THETA ** (-jnp.arange(0, d, 2, dtype=jnp.float32) / d)
    ang = positions.astype(jnp.float32)[..., None] * inv
    cos = jnp.cos(ang)[:, :, None, :]
    sin = jnp.sin(ang)[:, :, None, :]
    xf = x.astype(jnp.float32)
    x1, x2 = jnp.split(xf, 2, axis=-1)
    out = jnp.concatenate([x1 * cos - x2 * sin, x2 * cos + x1 * sin], axis=-1)
    return out.astype(x.dtype)


def gla_chunked(q, k, v, log_a):
    B, S, H, DK = q.shape
    DV = v.shape[-1]
    C = GLA_CHUNK
    n = S // C

    def to_chunks(t):
        return t.astype(jnp.float32).reshape(B, n, C, H, t.shape[-1]).transpose(1, 0, 3, 2, 4)

    qc = to_chunks(q.astype(jnp.float32) * (DK ** -0.5))
    kc, vc, ac = to_chunks(k), to_chunks(v), to_chunks(log_a)
    causal = jnp.tril(jnp.ones((C, C), dtype=bool))[:, :, None]

    def step(state, inp):
        qb, kb, vb, ab = inp
        b = jnp.cumsum(ab, axis=2)
        inter = jnp.einsum('bhcd,bhde->bhce', qb * jnp.exp(b), state)
        rel = jnp.where(causal, b[:, :, :, None, :] - b[:, :, None, :, :], -jnp.inf)
        scores = jnp.einsum('bhid,bhjd,bhijd->bhij', qb, kb, jnp.exp(rel))
        intra = jnp.einsum('bhij,bhje->bhie', scores, vb)
        b_last = b[:, :, -1, :]
        k_dec = kb * jnp.exp(b_last[:, :, None, :] - b)
        state = jnp.exp(b_last)[..., None] * state + jnp.einsum('bhcd,bhce->bhde', k_dec, vb)
        return state, inter + intra

    s0 = jnp.zeros((B, H, DK, DV), jnp.float32)
    _, out = lax.scan(step, s0, (qc, kc, vc, ac))
    return out.transpose(1, 0, 3, 2, 4).reshape(B, S, H, DV)


def multiscale_pool(u):
    B, S, _ = u.shape
    ug = u.astype(jnp.float32).reshape(B, S, POOL_GROUPS, POOL_GROUP_DIM)
    csum = jnp.cumsum(ug, axis=1)
    t = jnp.arange(1, S + 1, dtype=jnp.float32)
    outs = []
    for g, w in enumerate(POOL_WINDOWS):
        c = csum[:, :, g]
        c_prev = jnp.pad(c, ((0, 0), (w, 0), (0, 0)))[:, :S]
        count = jnp.minimum(t, float(w))[None, :, None]
        outs.append((c - c_prev) / count - ug[:, :, g])
    return jnp.stack(outs, axis=2)


def gla_pool_mixer(hn, w_in, gate_w, gate_b, out_norm, pool_w, pool_scale, w_out):
    B, S, _ = hn.shape
    proj = hn @ w_in
    q, k, v, g, a_lr, u = jnp.split(proj, EVEN_SPLITS, axis=-1)
    log_a = jax.nn.log_sigmoid((a_lr @ gate_w + gate_b).astype(jnp.float32)) / GLA_GATE_TAU
    o = gla_chunked(q.reshape(B, S, GLA_HEADS, GLA_DK), k.reshape(B, S, GLA_HEADS, GLA_DK),
                    v.reshape(B, S, GLA_HEADS, GLA_DV), log_a.reshape(B, S, GLA_HEADS, GLA_DK))
    o = rms_norm(o, out_norm.reshape(GLA_HEADS, GLA_DV)).reshape(B, S, GLA_HEADS * GLA_DV)
    o = (o * jax.nn.silu(g.astype(jnp.float32))).astype(hn.dtype)
    p = multiscale_pool(u)
    p = jnp.einsum('bsgc,gcd->bsgd', p, pool_w.astype(jnp.float32)).reshape(B, S, POOL_DIM)
    p = (p * pool_scale.astype(jnp.float32)).astype(hn.dtype)
    return jnp.concatenate([o, p], axis=-1) @ w_out


def dsa_attention(q, k, v, qi, ki, wi):
    B, S, H, Dh = q.shape
    G = k.shape[2]
    R = H // G
    top_k = min(TOPK_MAX, S // 4)
    n_blocks = S // Q_BLOCK
    idx_scale = (IDX_DIM ** -0.5) * (IDX_HEADS ** -0.5)
    key_pos = jnp.arange(S)
    ki_f = ki.astype(jnp.float32)

    def block(i):
        start = i * Q_BLOCK
        qb = lax.dynamic_slice_in_dim(q, start, Q_BLOCK, axis=1).reshape(B, Q_BLOCK, G, R, Dh)
        qib = lax.dynamic_slice_in_dim(qi, start, Q_BLOCK, axis=1).astype(jnp.float32)
        wib = lax.dynamic_slice_in_dim(wi, start, Q_BLOCK, axis=1).astype(jnp.float32)
        q_pos = start + jnp.arange(Q_BLOCK)
        logits = jax.nn.relu(jnp.einsum('bthd,bsd->bths', qib, ki_f))
        iscore = jnp.einsum('bth,bths->bts', wib, logits) * idx_scale
        admissible = key_pos[None, :] <= q_pos[:, None]
        iscore = jnp.where(admissible[None], iscore, -jnp.inf)
        _, sel = lax.top_k(iscore, top_k)
        valid = sel <= q_pos[None, :, None]
        k_sel = jax.vmap(lambda a, ix: a[ix])(k, sel)
        v_sel = jax.vmap(lambda a, ix: a[ix])(v, sel)
        s = jnp.einsum('btgrd,btkgd->btgrk', qb.astype(jnp.float32), k_sel.astype(jnp.float32)) * (Dh ** -0.5)
        s = jnp.where(valid[:, :, None, None, :], s, -jnp.inf)
        p = jax.nn.softmax(s, axis=-1)
        o = jnp.einsum('btgrk,btkgd->btgrd', p, v_sel.astype(jnp.float32))
        return o.reshape(B, Q_BLOCK, H * Dh).astype(q.dtype)

    out = lax.map(block, jnp.arange(n_blocks))
    return out.transpose(1, 0, 2, 3).reshape(B, S, H * Dh)


def dsa_mixer(hn, positions, w_in, w_out):
    B, S, _ = hn.shape
    proj = hn @ w_in
    q, k, v, qi, ki, wi = jnp.split(proj, ODD_SPLITS, axis=-1)
    q = rope(q.reshape(B, S, ATT_HEADS, ATT_HEAD_DIM), positions)
    k = rope(k.reshape(B, S, ATT_KV_HEADS, ATT_HEAD_DIM), positions)
    v = v.reshape(B, S, ATT_KV_HEADS, ATT_HEAD_DIM)
    qi = rope(qi.reshape(B, S, IDX_HEADS, IDX_DIM), positions)
    ki = rope(ki.reshape(B, S, 1, IDX_DIM), positions)[:, :, 0]
    o = dsa_attention(q, k, v, qi, ki, wi)
    return o @ w_out


def setup_inputs(seed: int = 0) -> dict:
    key = jax.random.key(seed)
    ks = jax.random.split(key, 20)
    f32 = jnp.float32

    def nrm(k, shape, scale):
        return jax.random.normal(k, shape, f32) * scale

    def gain(k, shape):
        return 1.0 + 0.02 * jax.random.normal(k, shape, f32)

    x = jax.random.normal(ks[0], (BATCH, SEQ, D_MODEL), f32)
    positions = jnp.broadcast_to(jnp.arange(SEQ, dtype=jnp.int32)[None, :], (BATCH, SEQ)).astype(jnp.int32)
    return {
        "x": x,
        "positions": positions,
        "ffn1_norm": gain(ks[1], (DEPTH, D_MODEL)),
        "ffn1_wi": nrm(ks[2], (DEPTH, D_MODEL, 2 * D_FF), D_MODEL ** -0.5),
        "ffn1_wo": nrm(ks[3], (DEPTH, D_FF, D_MODEL), D_FF ** -0.5),
        "mix_norm": gain(ks[4], (DEPTH, D_MODEL)),
        "ffn2_norm": gain(ks[5], (DEPTH, D_MODEL)),
        "ffn2_wi": nrm(ks[6], (DEPTH, D_MODEL, 2 * D_FF), D_MODEL ** -0.5),
        "ffn2_wo": nrm(ks[7], (DEPTH, D_FF, D_MODEL), D_FF ** -0.5),
        "even_w_in": nrm(ks[8], (N_EVEN, D_MODEL, EVEN_IN), D_MODEL ** -0.5),
        "gla_gate_w": nrm(ks[9], (N_EVEN, GLA_GATE_RANK, GLA_HEADS * GLA_DK), GLA_GATE_RANK ** -0.5),
        "gla_gate_b": nrm(ks[10], (N_EVEN, GLA_HEADS * GLA_DK), 0.1),
        "gla_out_norm": gain(ks[11], (N_EVEN, GLA_HEADS * GLA_DV)),
        "pool_w": nrm(ks[12], (N_EVEN, POOL_GROUPS, POOL_GROUP_DIM, POOL_GROUP_DIM), POOL_GROUP_DIM ** -0.5),
        "pool_scale": gain(ks[13], (N_EVEN, POOL_DIM)),
        "even_w_out": nrm(ks[14], (N_EVEN, EVEN_MIX, D_MODEL), EVEN_MIX ** -0.5),
        "odd_w_in": nrm(ks[15], (N_ODD, D_MODEL, ODD_IN), D_MODEL ** -0.5),
        "odd_w_out": nrm(ks[16], (N_ODD, ODD_MIX, D_MODEL), ODD_MIX ** -0.5),
        "final_norm": gain(ks[17], (D_MODEL,)),
    }


def reference(x, positions, ffn1_norm, ffn1_wi, ffn1_wo, mix_norm, ffn2_norm, ffn2_wi, ffn2_wo,
              even_w_in, gla_gate_w, gla_gate_b, gla_out_norm, pool_w, pool_scale, even_w_out,
              odd_w_in, odd_w_out, final_norm):
    h = x
    for li in range(DEPTH):
        h = h + 0.5 * swiglu(rms_norm(h, ffn1_norm[li]), ffn1_wi[li], ffn1_wo[li])
        hn = rms_norm(h, mix_norm[li])
        if li % 2 == 0:
            j = li // 2
            mix = gla_pool_mixer(hn, even_w_in[j], gla_gate_w[j], gla_gate_b[j], gla_out_norm[j],
                                 pool_w[j], pool_scale[j], even_w_out[j])
        else:
            j = li // 2
            mix = dsa_mixer(hn, positions, odd_w_in[j], odd_w_out[j])
        h = h + mix
        h = h + 0.5 * swiglu(rms_norm(h, ffn2_norm[li]), ffn2_wi[li], ffn2_wo[li])
    return rms_norm(h, final_norm)
```

```python
import contextlib
import numpy as np
import ml_dtypes
import concourse.bass as bass
import concourse.mybir as mybir
from concourse.bass_utils import run_bass_kernel_spmd

F32 = mybir.dt.float32
BF16 = mybir.dt.bfloat16
I32 = mybir.dt.int32
ALU = mybir.AluOpType
AF = mybir.ActivationFunctionType
AX = mybir.AxisListType

D = 1024
KC = 8
DFF = 2816
FC = 22
NTOK = 2048
SEQ = 4096
EPS = 1e-6
N_CORES = 8
NEG = -1.0e30
N_IT = 14
import os
LITE = os.environ.get("KLITE") == "1"
KSTOP = int(os.environ.get("KSTOP", "99"))
KSKIP = set(os.environ.get("KSKIP", "").split(","))
TWO_PI = 2.0 * np.pi
CW1 = 6.28125
CW2 = TWO_PI - 6.28125


class Sched:
    def __init__(self, nc, es, n_dma_sems=6):
        self.nc = nc
        self.eng = {"pe": nc.tensor, "act": nc.scalar, "dve": nc.vector, "pool": nc.gpsimd, "sp": nc.sync}
        self.sems = {}
        self.cnt = {}
        for e in ("pe", "act", "dve", "pool"):
            self.sems[e] = es.enter_context(nc.semaphore("s_" + e))
            self.cnt[e] = 0
        self.dq = {}
        self.dq_next = {}
        for q in ("sp", "pool"):
            names = []
            for i in range(n_dma_sems):
                nm = "d_%s%d" % (q, i)
                self.sems[nm] = es.enter_context(nc.semaphore(nm))
                self.cnt[nm] = 0
                names.append(nm)
            self.dq[q] = names
            self.dq_next[q] = 0
        self.seen = {e: {} for e in self.eng}
        self.lastw = {}
        self.readers = {}

    def _wait(self, e, x, v):
        if v <= 0 or self.seen[e].get(x, 0) >= v:
            return
        self.eng[e].wait_ge(self.sems[x], v)
        self.seen[e][x] = v

    def _deps(self, e, reads, writes):
        d = {}

        def add(tok, war=False):
            if tok is None:
                return
            x, v = tok
            if x == e and e == "pe":
                return
            if d.get(x, 0) < v:
                d[x] = v

        for r in reads:
            add(self.lastw.get(r))
            if r == "psb" or (isinstance(r, tuple) and r[0] == "ps"):
                for x2, tok in self.readers.get(r, {}).items():
                    if x2 != e:
                        add(tok)
        for w in writes:
            add(self.lastw.get(w))
            for tok in self.readers.get(w, {}).values():
                add(tok, war=True)
        return d

    def _record(self, tok, reads, writes):
        for r in reads:
            self.readers.setdefault(r, {})[tok[0]] = tok
        for w in writes:
            self.lastw[w] = tok
            self.readers[w] = {}

    def op(self, e, fn, reads=(), writes=(), inc=True):
        d = self._deps(e, reads, writes)
        for x, v in d.items():
            self._wait(e, x, v)
        ins = fn()
        if inc:
            self.cnt[e] += 1
            ins.then_inc(self.sems[e], 1)
            tok = (e, self.cnt[e])
        else:
            tok = (e, self.cnt[e] + 1)
        self._record(tok, reads, writes)
        return ins

    def dma(self, q, out, in_, reads=(), writes=()):
        d = self._deps(q, reads, writes)
        i = self.dq_next[q]
        self.dq_next[q] = (i + 1) % len(self.dq[q])
        nm = self.dq[q][i]
        self._wait(q, nm, self.cnt[nm])
        for x, v in d.items():
            self._wait(q, x, v)
        self.cnt[nm] += 16
        self.eng[q].dma_start(out=out, in_=in_).then_inc(self.sems[nm], 16)
        self._record((nm, self.cnt[nm]), reads, writes)

    def barrier(self, engines=("pe", "act", "dve", "pool", "sp")):
        for e in engines:
            for x, v in self.cnt.items():
                if x == e and e == "pe":
                    continue
                self._wait(e, x, v)


def build_program(launch):
    nc = bass.Bass("TRN2", target_bir_lowering=False)

    def din(name, shape, dt=F32):
        return nc.dram_tensor(name, list(shape), dt, kind="ExternalInput").ap()

    def dout(name, shape, dt=F32):
        return nc.dram_tensor(name, list(shape), dt, kind="ExternalOutput").ap()

    ident_d = din("c_ident", [128, 128])

    with contextlib.ExitStack() as es:
        S = Sched(nc, es)
        _uid = [0]

        def sb(name, shape, dt=F32, stack=es):
            _uid[0] += 1
            return stack.enter_context(nc.sbuf_tensor("%s_%d" % (name, _uid[0]), list(shape), dt))

        def V(fn, r=(), w=()):
            return S.op("dve", fn, reads=r, writes=w)

        def A(fn, r=(), w=()):
            return S.op("act", fn, reads=r, writes=w)

        def G(fn, r=(), w=()):
            return S.op("pool", fn, reads=r, writes=w)

        def T(fn, r=(), w=(), inc=True):
            return S.op("pe", fn, reads=r, writes=w, inc=inc)

        hT = sb("hT", [128, KC, NTOK])
        ident = sb("ident", [128, 128])
        ident_bf = sb("ident_bf", [128, 128], BF16)
        ones_bf = sb("ones_bf", [128, 128], BF16)
        eps_c = sb("eps_c", [128, 1])
        one_c = sb("one_c", [128, 1])
        ps = [es.enter_context(nc.psum_tensor("ps%d" % i, [128, 512], F32)) for i in range(7)]
        psb = es.enter_context(nc.psum_tensor("psb", [128, 1024], BF16))

        stage = [sb("stage%d" % i, [128, 1024]) for i in range(2)]
        _rr = [0]

        def cast_load(dst_ap, src_ap, view, dkey):
            i = _rr[0] % 2
            eng = "pool" if (_rr[0] // 2) % 2 == 0 else "act"
            _rr[0] += 1
            st = view(stage[i])
            S.dma("sp", st, src_ap, writes=[("stage", i)])
            if eng == "pool":
                G(lambda: nc.gpsimd.tensor_copy(out=dst_ap, in_=st), [("stage", i)], [dkey])
            else:
                A(lambda: nc.scalar.copy(out=dst_ap, in_=st), [("stage", i)], [dkey])

        S.dma("sp", ident[:], ident_d[:, :], writes=["ident"])
        V(lambda: nc.vector.tensor_copy(out=ident_bf[:], in_=ident[:]), ["ident"], ["ident_bf"])
        V(lambda: nc.vector.memset(ones_bf[:], 1.0), [], ["ones_bf"])
        V(lambda: nc.vector.memset(eps_c[:], EPS), [], ["eps"])
        V(lambda: nc.vector.memset(one_c[:], 1.0), [], ["one"])

        def load_gain(dram_row, name):
            g = sb(name, [128, KC])
            with nc.allow_non_contiguous_dma(reason="tiny gain vector"):
                S.dma("sp", g[:], dram_row.rearrange("o (kc p) -> p (o kc)", p=128), writes=[name])
            return g

        def load_state(t_sb, d_ap, key, q="sp"):
            S.dma(q, t_sb, d_ap, writes=[key])

        def rstd_ps(src_fn, nk, n, sq, pbank, key_r):
            for kc in range(nk):
                A(lambda kc=kc: nc.scalar.activation(out=sq[:, kc % 2, 0:n], in_=src_fn(kc), func=AF.Square),
                  key_r(kc), [("sq", kc % 2)])
                T(lambda kc=kc: nc.tensor.matmul(ps[pbank][:, 0:n], lhsT=ones_bf[:], rhs=sq[:, kc % 2, 0:n], start=(kc == 0), stop=(kc == nk - 1)),
                  [("sq", kc % 2), "ones_bf"], [("ps", pbank)])

        def rstd_from_ps(rstd_out, pbank, n, denom):
            A(lambda: nc.scalar.activation(out=rstd_out, in_=ps[pbank][:, 0:n], func=AF.Ln, scale=1.0 / denom, bias=eps_c[:, 0:1]),
              [("ps", pbank), "eps"], ["rstd"])
            A(lambda: nc.scalar.activation(out=rstd_out, in_=rstd_out, func=AF.Exp, scale=-0.5), ["rstd"], ["rstd"])

        def norm_h(cols, gain, gname, sq, rstd, hnT, hn_key):
            rstd_ps(lambda kc: hT[:, kc, cols], KC, 512, sq, 6, lambda kc: [("hT", kc)])
            rstd_from_ps(rstd[:], 6, 512, D)
            for kc in range(KC):
                V(lambda kc=kc: nc.vector.scalar_tensor_tensor(out=hnT(kc), in0=hT[:, kc, cols], scalar=gain[:, kc:kc + 1], in1=rstd[:],
                                                               op0=ALU.mult, op1=ALU.mult),
                  [("hT", kc), gname, "rstd"], [hn_key])

        def ffn(wi_d, wo_d, gain, gname):
            with contextlib.ExitStack() as ph:
                hnT = sb("hnT", [128, KC, 1024], BF16, stack=ph)
                actT = sb("actT", [128, FC, 1024], BF16, stack=ph)
                wo_sb = sb("wo_sb", [128, FC, D], BF16, stack=ph)
                wg_sb = [sb("wg_sb%d" % i, [128, KC, 256], BF16, stack=ph) for i in range(2)]
                wu_sb = [sb("wu_sb%d" % i, [128, KC, 256], BF16, stack=ph) for i in range(2)]
                sq = sb("sq", [128, 2, 512], BF16, stack=ph)
                rstd = sb("rstd", [128, 512], stack=ph)
                sg = [sb("sg%d" % i, [128, 512], stack=ph) for i in range(2)]
                for fc in range(FC):
                    cast_load(wo_sb[:, fc, :], wo_d[fc * 128:(fc + 1) * 128, :], lambda t: t[:, :], ("wo", fc // 2))
                it = 0
                for half in range(2):
                    tok0 = half * 1024
                    for tg in range(2):
                        cols = slice(tok0 + tg * 512, tok0 + (tg + 1) * 512)
                        norm_h(cols, gain, gname, sq, rstd, lambda kc, tg=tg: hnT[:, kc, tg * 512:(tg + 1) * 512], ("hnT", tg))
                    for fg in range(11):
                        wb = fg % 2
                        v4 = lambda t: t[:, :].rearrange("p (k c) -> p k c", k=4)
                        for hk in range(2):
                            cast_load(wg_sb[wb][:, hk * 4:(hk + 1) * 4, :],
                                      wi_d[hk * 512:(hk + 1) * 512, fg * 256:(fg + 1) * 256].rearrange("(k p) c -> p k c", p=128), v4, ("wg", wb))
                            cast_load(wu_sb[wb][:, hk * 4:(hk + 1) * 4, :],
                                      wi_d[hk * 512:(hk + 1) * 512, DFF + fg * 256:DFF + (fg + 1) * 256].rearrange("(k p) c -> p k c", p=128), v4, ("wu", wb))
                        for c in range(2):
                            fc = fg * 2 + c
                            for tg in range(2):
                                pg, pu = (0, 1) if it % 2 == 0 else (2, 3)
                                sgi = it % 2
                                it += 1
                                for kc in range(KC):
                                    T(lambda kc=kc, c=c, tg=tg, pg=pg, wb=wb: nc.tensor.matmul(
                                        ps[pg][:], lhsT=wg_sb[wb][:, kc, c * 128:(c + 1) * 128], rhs=hnT[:, kc, tg * 512:(tg + 1) * 512],
                                        start=(kc == 0), stop=(kc == KC - 1)),
                                      [("wg", wb), ("hnT", tg)], [("ps", pg)], inc=(kc == KC - 1))
                                for kc in range(KC):
                                    T(lambda kc=kc, c=c, tg=tg, pu=pu, wb=wb: nc.tensor.matmul(
                                        ps[pu][:], lhsT=wu_sb[wb][:, kc, c * 128:(c + 1) * 128], rhs=hnT[:, kc, tg * 512:(tg + 1) * 512],
                                        start=(kc == 0), stop=(kc == KC - 1)),
                                      [("wu", wb), ("hnT", tg)], [("ps", pu)], inc=(kc == KC - 1))
                                A(lambda pg=pg, sgi=sgi: nc.scalar.activation(out=sg[sgi][:], in_=ps[pg][:], func=AF.Silu),
                                  [("ps", pg)], [("sg", sgi)])
                                V(lambda pu=pu, sgi=sgi, fc=fc, tg=tg: nc.vector.tensor_tensor(
                                    out=actT[:, fc, tg * 512:(tg + 1) * 512], in0=ps[pu][:], in1=sg[sgi][:], op=ALU.mult),
                                  [("ps", pu), ("sg", sgi)], [("actT", fc, tg)])
                    io = 0
                    for dc in range(KC):
                        for tg in range(2):
                            po = 4 + (io % 2)
                            io += 1
                            cols = slice(tok0 + tg * 512, tok0 + (tg + 1) * 512)
                            for fc in range(FC):
                                T(lambda fc=fc, dc=dc, tg=tg, po=po: nc.tensor.matmul(
                                    ps[po][:], lhsT=wo_sb[:, fc, dc * 128:(dc + 1) * 128], rhs=actT[:, fc, tg * 512:(tg + 1) * 512],
                                    start=(fc == 0), stop=(fc == FC - 1)),
                                  [("wo", fc // 2), ("actT", fc, tg)], [("ps", po)], inc=(fc == FC - 1))
                            V(lambda dc=dc, cols=cols, po=po: nc.vector.scalar_tensor_tensor(
                                out=hT[:, dc, cols], in0=ps[po][:], scalar=0.5, in1=hT[:, dc, cols], op0=ALU.mult, op1=ALU.add),
                              [("ps", po), ("hT", dc)], [("hT", dc)])
                S.barrier()

        def load_wcast(dst, src_d, ncols, key, step=256):
            for c0 in range(0, ncols, step):
                c1 = min(ncols, c0 + step)
                wd = c1 - c0
                for hk in range(2):
                    cast_load(dst[:, hk * 4:(hk + 1) * 4, c0:c1],
                              src_d[hk * 512:(hk + 1) * 512, c0:c1].rearrange("(k p) c -> p k c", p=128),
                              lambda t, wd=wd: t[:, 0:4 * wd].rearrange("p (k c) -> p k c", k=4), key)

        def out_proj(w_sb, wkey, rhs_fn, rkeys):
            io = 0
            for tg in range(4):
                cols = slice(tg * 512, (tg + 1) * 512)
                for dc in range(KC):
                    po = 4 + (io % 2)
                    io += 1
                    for k in range(8):
                        T(lambda k=k, dc=dc, po=po, cols=cols: nc.tensor.matmul(
                            ps[po][:], lhsT=w_sb[:, k, dc * 128:(dc + 1) * 128], rhs=rhs_fn(k, cols), start=(k == 0), stop=(k == 7)),
                          [wkey] + rkeys(k, tg), [("ps", po)], inc=(k == 7))
                    V(lambda dc=dc, cols=cols, po=po: nc.vector.tensor_tensor(out=hT[:, dc, cols], in0=ps[po][:], in1=hT[:, dc, cols], op=ALU.add),
                      [("ps", po), ("hT", dc)], [("hT", dc)])

        def dump3(dram2d, sb3, n, reads, q="sp"):
            dv = dram2d.rearrange("p (n t) -> p n t", n=n)
            for i in range(n):
                S.dma(q, dv[:, i, :], sb3[:, i, :], reads=reads(i))

        def load3(sb3, dram2d, n, writes, q="sp"):
            dv = dram2d.rearrange("p (n t) -> p n t", n=n)
            for i in range(n):
                S.dma(q, sb3[:, i, :], dv[:, i, :], writes=writes(i))

        if launch == "A":
            x_d = din("x", [NTOK, D])
            g_ffn = load_gain(din("g_ffn1_0", [1, D]), "g_ffn")
            g_mix = load_gain(din("g_mix0", [1, D]), "g_mix")
            if not LITE:
                wi_d = din("ffn_wi", [D, 2 * DFF])
                wo_d = din("ffn_wo", [DFF, D])
            w_in_d = din("even_w_in", [D, 2064])
            gw_d = din("gate_wb", [17, 256])
            poolw_d = din("pool_w", [4, 128, 128])
            pscale_d = din("pool_scale", [1, 512])
            tin_d = din("c_tin64", [64, 64])
            uex_d = din("c_uex64", [64, 64])
            cmask_d = din("c_cmask", [64, 256])
            o_hT = dout("st_hT", [128, KC * NTOK])
            o_o = dout("st_o", [128, 4 * NTOK])
            o_qe = dout("st_qe", [64, 4 * NTOK], BF16)
            o_sg = dout("st_sg", [128, 4 * NTOK], BF16)
            o_pp = dout("st_pp", [128, 4 * NTOK], BF16)
            o_bl = dout("st_bl", [64, 128])
            o_u16 = dout("st_u16", [128, 64])
            o_send = dout("st_send", [64, 512])
            o_utail = dout("st_utail", [128, 64])

            with contextlib.ExitStack() as ph:
                xin = sb("xin", [128, 4, D], stack=ph)
                for tg in range(4):
                    for j in range(4):
                        tt = tg * 4 + j
                        S.dma("sp", xin[:, j, :], x_d[tt * 128:(tt + 1) * 128, :], writes=[("xin", j)])
                    for kc in range(KC):
                        pb = kc % 2
                        for j in range(4):
                            T(lambda j=j, kc=kc, pb=pb: nc.tensor.transpose(ps[pb][:, j * 128:(j + 1) * 128], xin[:, j, kc * 128:(kc + 1) * 128], ident[:]),
                              [("xin", j), "ident"], [("ps", pb)], inc=(j == 3))
                        if kc % 2 == 0:
                            V(lambda kc=kc, pb=pb, tg=tg: nc.vector.tensor_copy(out=hT[:, kc, tg * 512:(tg + 1) * 512], in_=ps[pb][:]),
                              [("ps", pb)], [("hT", kc)])
                        else:
                            A(lambda kc=kc, pb=pb, tg=tg: nc.scalar.copy(out=hT[:, kc, tg * 512:(tg + 1) * 512], in_=ps[pb][:]),
                              [("ps", pb)], [("hT", kc)])
                S.barrier()

            if not LITE:
                ffn(wi_d, wo_d, g_ffn, "g_ffn")

            with contextlib.ExitStack() as ph:
                w_in = sb("w_in", [128, KC, 2064], BF16, stack=ph)
                if "wcast" not in KSKIP:
                    load_wcast(w_in, w_in_d, 2064, "w_in")
                gw32 = sb("gw32", [17, 256], stack=ph)
                gw = sb("gw", [17, 256], BF16, stack=ph)
                S.dma("sp", gw32[:], gw_d[:, :], writes=["gw32"])
                A(lambda: nc.scalar.copy(out=gw[:], in_=gw32[:]), ["gw32"], ["gw"])
                poolw = sb("poolw", [128, 4, 128], BF16, stack=ph)
                if "poolw" not in KSKIP:
                    cast_load(poolw[:], poolw_d.rearrange("g c d -> c g d"), lambda t: t[:, 0:512].rearrange("p (g d) -> p g d", g=4), "poolw")
                pscale = sb("pscale", [128, 4], stack=ph)
                if "pscale" not in KSKIP:
                    with nc.allow_non_contiguous_dma(reason="tiny"):
                        S.dma("sp", pscale[:], pscale_d.rearrange("o (g p) -> p (o g)", p=128), writes=["pscale"])
                tin32 = sb("tin32", [64, 64], stack=ph)
                uex32 = sb("uex32", [64, 64], stack=ph)
                tin = sb("tin", [64, 64], BF16, stack=ph)
                uex = sb("uex", [64, 64], BF16, stack=ph)
                cmask = sb("cmask", [64, 256], stack=ph)
                S.dma("sp", tin32[:], tin_d[:, :], writes=["tin32"])
                S.dma("sp", uex32[:], uex_d[:, :], writes=["uex32"])
                S.dma("sp", cmask[:], cmask_d[:, :], writes=["cmask"])
                A(lambda: nc.scalar.copy(out=tin[:], in_=tin32[:]), ["tin32"], ["tin"])
                A(lambda: nc.scalar.copy(out=uex[:], in_=uex32[:]), ["uex32"], ["uex"])
                o_all = sb("o_grp", [128, 4, 512], stack=ph)
                qe = sb("qe_grp", [64, 4, 512], BF16, stack=ph)
                sgT = sb("sg_grp", [128, 4, 512], BF16, stack=ph)
                pp = sb("pp_grp", [128, 4, 512], BF16, stack=ph)
                BL = sb("BL", [64, 4, 32], stack=ph)
                u16 = sb("u16", [128, 4, 16], stack=ph)
                Sst = sb("Sst", [64, 4, 128], stack=ph)
                Sbf = sb("Sbf", [64, 4, 128], BF16, stack=ph)
                hnT = sb("hnT", [128, KC, 512], BF16, stack=ph)
                sq = sb("sq", [128, 2, 512], BF16, stack=ph)
                rstd = sb("rstd", [128, 512], stack=ph)
                a1 = sb("a1", [17, 512], BF16, stack=ph)
                tmpz = sb("tmpz", [64, 256], stack=ph)
                sp32 = sb("sp32", [64, 256], stack=ph)
                sp = sb("sp_hi", [64, 8, 256], BF16, stack=ph)
                spl = sb("sp_lo", [64, 8, 256], BF16, stack=ph)
                eb = sb("eb", [64, 4, 512], stack=ph)
                enb = sb("enb", [64, 4, 512], stack=ph)
                ke = sb("ke", [64, 4, 512], BF16, stack=ph)
                er = sb("er", [64, 256], stack=ph)
                kd = sb("kd", [64, 8, 256], BF16, stack=ph)
                vt = sb("vt", [64, 8, 512], BF16, stack=ph)
                uT1 = sb("uT", [128, 4, 528], stack=ph)
                uT = [uT1, uT1]
                sw = [sb("sw%d" % i, [128, 528], stack=ph) for i in range(2)]
                pT = sb("pT", [128, 512], BF16, stack=ph)
                sTm = sb("sTm", [64, 256], BF16, stack=ph)

                V(lambda: nc.vector.memset(a1[:], 1.0), [], ["a1"])
                V(lambda: nc.vector.memset(Sst[:], 0.0), [], ["Sst"])
                V(lambda: nc.vector.memset(Sbf[:], 0.0), [], ["Sbf"])
                V(lambda: nc.vector.memset(uT1[:, :, 0:16], 0.0), [], [("uT", 0)])

                def proj_fm(pb, c0, m, rows=128):
                    for kc in range(KC):
                        T(lambda kc=kc: nc.tensor.matmul(ps[pb][0:m, :], lhsT=w_in[:, kc, c0:c0 + m], rhs=hnT[:, kc, :],
                                                         start=(kc == 0), stop=(kc == KC - 1)),
                          ["w_in", "hnT"], [("ps", pb)], inc=(kc == KC - 1))

                for tg in range(4 if KSTOP >= 2 else 0):
                    cols = slice(tg * 512, (tg + 1) * 512)
                    lc = slice(0, 512)
                    ub = 0
                    norm_h(cols, g_mix, "g_mix", sq, rstd, lambda kc: hnT[:, kc, :], "hnT")
                    if "alr" not in KSKIP:
                        proj_fm(0, 1536, 16)
                        V(lambda: nc.vector.tensor_copy(out=a1[0:16, :], in_=ps[0][0:16, :]), [("ps", 0)], ["a1"])
                    for ch in range(8 if "z" not in KSKIP else 0):
                        T(lambda ch=ch: nc.tensor.matmul(ps[2][0:64, 0:256], lhsT=a1[0:17, ch * 64:(ch + 1) * 64], rhs=gw[0:17, :], start=True, stop=True),
                          ["a1", "gw"], [("ps", 2)])
                        if "zact" in KSKIP:
                            continue
                        A(lambda: nc.scalar.activation(out=tmpz[:], in_=ps[2][0:64, 0:256], func=AF.Exp, scale=-1.0), [("ps", 2)], ["tmpz"])
                        if "zln" in KSKIP:
                            continue
                        A(lambda ch=ch: nc.scalar.activation(out=sp[:, ch, :], in_=tmpz[:], func=AF.Ln, bias=one_c[0:64, 0:1]), ["tmpz", "one"], [("sp", ch)])
                    for h in range(4 if "bT" not in KSKIP else 0):
                        for ch in range(8):
                            T(lambda ch=ch, h=h: nc.tensor.matmul(ps[3][0:64, ch * 64:(ch + 1) * 64], lhsT=sp[:, ch, h * 64:(h + 1) * 64], rhs=tin[:, :],
                                                                  start=True, stop=True),
                              [("sp", ch), "tin"], [("ps", 3)], inc=(ch == 7))
                        A(lambda h=h: nc.scalar.activation(out=eb[:, h, :], in_=ps[3][0:64, :], func=AF.Exp), [("ps", 3)], [("eb", h)])
                        A(lambda h=h: nc.scalar.activation(out=enb[:, h, :], in_=ps[3][0:64, :], func=AF.Exp, scale=-1.0), [("ps", 3)], [("enb", h)])
                        V(lambda h=h, tg=tg: nc.vector.tensor_copy(out=BL[:, h, tg * 8:(tg + 1) * 8], in_=ps[3][0:64, 63:512:64]), [("ps", 3)], ["BL"])
                    for h in range(4 if KSTOP >= 3 else 0):
                        proj_fm(0, h * 64, 64)
                        V(lambda h=h: nc.vector.scalar_tensor_tensor(out=qe[:, h, :], in0=ps[0][0:64, :], scalar=0.125, in1=eb[:, h, :],
                                                                                op0=ALU.mult, op1=ALU.mult),
                          [("ps", 0), ("eb", h)], ["qe"])
                        proj_fm(1, 256 + h * 64, 64)
                        V(lambda h=h: nc.vector.tensor_tensor(out=ke[:, h, :], in0=ps[1][0:64, :], in1=enb[:, h, :], op=ALU.mult),
                          [("ps", 1), ("enb", h)], ["ke"])
                    for ch in range(8 if KSTOP >= 3 else 0):
                        T(lambda ch=ch: nc.tensor.matmul(ps[2][0:64, 0:256], lhsT=uex[:, :], rhs=sp[:, ch, :], start=True, stop=True),
                          [("sp", ch), "uex"], [("ps", 2)])
                        A(lambda: nc.scalar.activation(out=er[:], in_=ps[2][0:64, 0:256], func=AF.Exp), [("ps", 2)], ["er"])
                        for kc in range(KC):
                            T(lambda kc=kc, ch=ch: nc.tensor.matmul(ps[0][0:64, 0:256], lhsT=hnT[:, kc, ch * 64:(ch + 1) * 64], rhs=w_in[:, kc, 256:512],
                                                                    start=(kc == 0), stop=(kc == KC - 1)),
                              ["w_in", "hnT"], [("ps", 0)], inc=(kc == KC - 1))
                        V(lambda ch=ch: nc.vector.tensor_tensor(out=kd[:, ch, :], in0=ps[0][0:64, 0:256], in1=er[:], op=ALU.mult),
                          [("ps", 0), "er"], [("kd", ch)])
                        for kc in range(KC):
                            T(lambda kc=kc, ch=ch: nc.tensor.matmul(ps[1][0:64, :], lhsT=hnT[:, kc, ch * 64:(ch + 1) * 64], rhs=w_in[:, kc, 512:1024],
                                                                    start=(kc == 0), stop=(kc == KC - 1)),
                              ["w_in", "hnT"], [("ps", 1)], inc=(kc == KC - 1))
                        A(lambda ch=ch: nc.scalar.copy(out=vt[:, ch, :], in_=ps[1][0:64, :]), [("ps", 1)], [("vt", ch)])
                    for h in range(4 if KSTOP >= 4 else 0):
                        proj_fm(h % 2, 1024 + h * 128, 128)
                        A(lambda h=h: nc.scalar.activation(out=sgT[:, h, :], in_=ps[h % 2][:], func=AF.Silu), [("ps", h % 2)], ["sgT"])
                    for g in range(4 if KSTOP >= 4 else 0):
                        proj_fm(g % 2, 1552 + g * 128, 128)
                        V(lambda g=g, ub=ub: nc.vector.tensor_copy(out=uT[ub][:, g, 16:528], in_=ps[g % 2][:]), [("ps", g % 2)], [("uT", ub)])
                    if tg == 0:
                        V(lambda: nc.vector.tensor_copy(out=u16[:], in_=uT[0][:, :, 16:32]), [("uT", 0)], ["u16"])
                    for ch in range(8 if KSTOP >= 5 else 0):
                        cg = tg * 8 + ch
                        ccol = slice(ch * 64, (ch + 1) * 64)
                        gcol = ccol
                        for h in range(4):
                            T(lambda h=h, ccol=ccol, gcol=gcol: nc.tensor.matmul(ps[4][0:64, h * 64:(h + 1) * 64], lhsT=ke[:, h, ccol], rhs=qe[:, h, gcol],
                                                                                start=True, stop=True),
                              ["ke", "qe"], [("ps", 4)], inc=(h == 3))
                        V(lambda: nc.vector.tensor_tensor(out=sTm[:], in0=ps[4][0:64, 0:256], in1=cmask[:], op=ALU.mult), [("ps", 4), "cmask"], ["sTm"])
                        ob = 5 if (ch // 2) % 2 == 0 else 6
                        for h in range(4):
                            oc = slice(h * 128 + (ch % 2) * 64, h * 128 + (ch % 2) * 64 + 64)
                            T(lambda h=h, ch=ch, oc=oc, ob=ob: nc.tensor.matmul(ps[ob][:, oc], lhsT=vt[:, ch, h * 128:(h + 1) * 128], rhs=sTm[:, h * 64:(h + 1) * 64],
                                                                               start=True, stop=False),
                              [("vt", ch), "sTm"], [("ps", ob)], inc=False)
                            T(lambda h=h, gcol=gcol, oc=oc, ob=ob: nc.tensor.matmul(ps[ob][:, oc], lhsT=Sbf[:, h, :], rhs=qe[:, h, gcol], start=False, stop=True),
                              ["Sbf", "qe"], [("ps", ob)], inc=(h == 3))
                        for h in range(4):
                            T(lambda h=h, ch=ch: nc.tensor.matmul(ps[3][0:64, h * 128:(h + 1) * 128], lhsT=kd[:, ch, h * 64:(h + 1) * 64], rhs=vt[:, ch, h * 128:(h + 1) * 128],
                                                                  start=True, stop=True),
                              [("kd", ch), ("vt", ch)], [("ps", 3)], inc=(h == 3))
                        for h in range(4):
                            V(lambda h=h, ch=ch: nc.vector.scalar_tensor_tensor(out=Sst[:, h, :], in0=Sst[:, h, :], scalar=eb[:, h, ch * 64 + 63:ch * 64 + 64],
                                                                               in1=ps[3][0:64, h * 128:(h + 1) * 128], op0=ALU.mult, op1=ALU.add),
                              ["Sst", ("eb", h), ("ps", 3)], ["Sst"])
                        A(lambda: nc.scalar.copy(out=Sbf[:], in_=Sst[:]), ["Sst"], ["Sbf"])
                        if ch % 2 == 1:
                            t0 = (ch // 2) * 128
                            A(lambda ob=ob, t0=t0: nc.scalar.copy(out=o_all[:, :, t0:t0 + 128], in_=ps[ob][:].rearrange("p (h t) -> p h t", h=4)),
                              [("ps", ob)], ["o_all"])
                    for g in range(4 if KSTOP >= 6 else 0):
                        w = 2 ** (g + 1)
                        src = uT[ub][:, g, :]
                        lo = 0
                        step = 1
                        k = 0
                        while step < w:
                            lo += step
                            dst = sw[k % 2]
                            G(lambda src=src, dst=dst, lo=lo, step=step: nc.gpsimd.tensor_tensor(out=dst[:, lo:528], in0=src[:, lo:528], in1=src[:, lo - step:528 - step], op=ALU.add),
                              [("uT", ub), ("sw", 0), ("sw", 1)], [("sw", k % 2)])
                            src = dst
                            step *= 2
                            k += 1
                        V(lambda src=src, g=g, ub=ub, w=w: nc.vector.scalar_tensor_tensor(out=pT[:], in0=src[:, 16:528], scalar=1.0 / w, in1=uT[ub][:, g, 16:528],
                                                                                         op0=ALU.mult, op1=ALU.subtract),
                          [("sw", 0), ("sw", 1), ("uT", ub)], ["pT"])
                        T(lambda g=g: nc.tensor.matmul(ps[g % 2][:], lhsT=poolw[:, g, :], rhs=pT[:], start=True, stop=True), ["poolw", "pT"], [("ps", g % 2)])
                        V(lambda g=g: nc.vector.tensor_scalar(out=pp[:, g, :], in0=ps[g % 2][:], scalar1=pscale[:, g:g + 1], scalar2=None, op0=ALU.mult),
                          [("ps", g % 2), "pscale"], ["pp"])
                    if KSTOP < 7:
                        continue
                    S.dma("sp", o_o.rearrange("p (h t) -> p h t", h=4)[:, :, cols], o_all[:], reads=["o_all"], writes=[("d_o", tg)])
                    S.dma("sp", o_qe.rearrange("p (h t) -> p h t", h=4)[:, :, cols], qe[:], reads=["qe"], writes=[("d_qe", tg)])
                    S.dma("sp", o_sg.rearrange("p (h t) -> p h t", h=4)[:, :, cols], sgT[:], reads=["sgT"], writes=[("d_sg", tg)])
                    S.dma("sp", o_pp.rearrange("p (h t) -> p h t", h=4)[:, :, cols], pp[:], reads=["pp"], writes=[("d_pp", tg)])
                    if tg == 3:
                        S.dma("sp", o_utail.rearrange("p (g t) -> p g t", g=4), uT1[:, :, 512:528], reads=[("uT", 0)], writes=["d_ut"])
                    else:
                        V(lambda: nc.vector.tensor_copy(out=uT1[:, :, 0:16], in_=uT1[:, :, 512:528]), [("uT", 0)], [("uT", 0)])
                S.barrier()
                if "dhT" not in KSKIP:
                    dump3(o_hT, hT, KC, lambda k: [("hT", k)])
                if "dsmall" not in KSKIP:
                    S.dma("sp", o_bl[:, :], BL[:].rearrange("p h t -> p (h t)"), reads=["BL"])
                    S.dma("sp", o_u16[:, :], u16[:].rearrange("p h t -> p (h t)"), reads=["u16"])
                    S.dma("sp", o_send[:, :], Sst[:].rearrange("p h t -> p (h t)"), reads=["Sst"])
                S.barrier()

        if launch == "B":
            i_hT = din("st_hT", [128, KC * NTOK])
            i_o = din("st_o", [128, 4 * NTOK])
            i_qe = din("st_qe", [64, 4 * NTOK], BF16)
            i_sg = din("st_sg", [128, 4 * NTOK], BF16)
            i_pp = din("st_pp", [128, 4 * NTOK], BF16)
            i_bl = din("st_bl", [64, 128])
            i_u16 = din("st_u16", [128, 64])
            i_sin = din("x_sin", [64, 512])
            i_halo = din("x_halo", [128, 64])
            i_invc = din("c_invcnt", [128, 64])
            gon_d = din("gla_out_norm", [1, 512])
            poolw_d = din("pool_w", [4, 128, 128])
            pscale_d = din("pool_scale", [1, 512])
            wout_d = din("even_w_out", [D, D])
            g_f2 = load_gain(din("g_ffn2_0", [1, D]), "g_f2")
            g_f1 = load_gain(din("g_ffn1_1", [1, D]), "g_f1")
            g_mix = load_gain(din("g_mix1", [1, D]), "g_mix")
            wi2_d = din("ffn2_wi", [D, 2 * DFF])
            wo2_d = din("ffn2_wo", [DFF, D])
            wi1_d = din("ffn1_wi", [D, 2 * DFF])
            wo1_d = din("ffn1_wo", [DFF, D])
            w_in_d = din("odd_w_in", [D, 1860])
            w_rot_d = din("odd_w_rot", [D, 1600])
            pos_d = din("positions", [1, NTOK], I32)
            ropec_d = din("c_rope", [128, 4])
            o_hT = dout("st_hT_o", [128, KC * NTOK])
            o_q = dout("st_q", [128, 8 * NTOK], BF16)
            o_k = dout("st_k", [128, 2 * NTOK], BF16)
            o_qi = dout("st_qi", [64, 4 * NTOK], BF16)
            o_ki = dout("st_ki", [64, NTOK], BF16)
            o_v = dout("st_v", [128, 16 * 260], BF16)
            o_wi = dout("st_wi", [128, 16 * 8])

            load3(hT, i_hT, KC, lambda k: [("hT", k)])
            with contextlib.ExitStack() as ph:
                o_all = sb("o_all", [128, 4, NTOK], stack=ph)
                qe = sb("qe", [64, 4, NTOK], BF16, stack=ph)
                sgT = sb("sgT", [128, 4, NTOK], BF16, stack=ph)
                pp = sb("pp", [128, 4, NTOK], BF16, stack=ph)
                BL = sb("BL", [64, 4, 32], stack=ph)
                Sin_ = sb("Sin", [64, 4, 128], stack=ph)
                ue = sb("ue", [128, 4, 32], stack=ph)
                invc = sb("invc", [128, 4, 16], stack=ph)
                load3(o_all, i_o, 4, lambda k: ["o_all"])
                load3(qe, i_qe, 4, lambda k: ["qe"])
                load3(sgT, i_sg, 4, lambda k: ["sgT"])
                load3(pp, i_pp, 4, lambda k: ["pp"])
                S.dma("sp", BL[:].rearrange("p h t -> p (h t)"), i_bl[:, :], writes=["BL"])
                S.dma("sp", Sin_[:].rearrange("p h t -> p (h t)"), i_sin[:, :], writes=["Sin"])
                S.dma("sp", ue[:, :, 0:16], i_halo.rearrange("p (g t) -> p g t", g=4), writes=["ue"])
                S.dma("sp", ue[:, :, 16:32], i_u16.rearrange("p (g t) -> p g t", g=4), writes=["ue"])
                S.dma("sp", invc[:].rearrange("p g t -> p (g t)"), i_invc[:, :], writes=["invc"])
                gon = sb("gon", [128, 4], stack=ph)
                pscale = sb("pscale", [128, 4], stack=ph)
                with nc.allow_non_contiguous_dma(reason="tiny"):
                    S.dma("sp", gon[:], gon_d.rearrange("o (g p) -> p (o g)", p=128), writes=["gon"])
                    S.dma("sp", pscale[:], pscale_d.rearrange("o (g p) -> p (o g)", p=128), writes=["pscale"])
                poolw = sb("poolw", [128, 4, 128], BF16, stack=ph)
                cast_load(poolw[:], poolw_d.rearrange("g c d -> c g d"), lambda t: t[:, 0:512].rearrange("p (g d) -> p g d", g=4), "poolw")
                w_out = sb("w_out", [128, KC, D], BF16, stack=ph)
                load_wcast(w_out, wout_d, D, "w_out")
                zer = sb("zer", [64, 32], stack=ph)
                Ein = sb("Ein", [64, 4, 32], stack=ph)
                E = sb("E", [64, 4, 32], stack=ph)
                Spb = [sb("Spb%d" % i, [64, 128], BF16, stack=ph) for i in range(4)]
                sq = sb("sq", [128, 2, 512], BF16, stack=ph)
                rstd = sb("rstd", [128, 512], stack=ph)
                tmpo = sb("tmpo", [128, 512], stack=ph)
                sw = [sb("sw%d" % i, [128, 32], stack=ph) for i in range(2)]
                p16 = sb("p16", [128, 16], stack=ph)
                p16b = sb("p16b", [128, 16], BF16, stack=ph)

                V(lambda: nc.vector.memset(zer[:], 0.0), [], ["zer"])
                for h in range(4):
                    V(lambda h=h: nc.vector.tensor_tensor_scan(out=Ein[:, h, :], data0=BL[:, h, :], data1=zer[:], initial=0.0, op0=ALU.add, op1=ALU.add),
                      ["BL", "zer"], ["Ein"])
                V(lambda: nc.vector.tensor_tensor(out=Ein[:], in0=Ein[:], in1=BL[:], op=ALU.subtract), ["Ein", "BL"], ["Ein"])
                A(lambda: nc.scalar.activation(out=E[:], in_=Ein[:], func=AF.Exp), ["Ein"], ["E"])
                kk = 0
                for tg in range(4):
                    cols = slice(tg * 512, (tg + 1) * 512)
                    for h in range(4):
                        pb = h % 2
                        for ch in range(8):
                            cg = tg * 8 + ch
                            gcol = slice(tg * 512 + ch * 64, tg * 512 + (ch + 1) * 64)
                            sbi = kk % 4
                            kk += 1
                            V(lambda h=h, cg=cg, sbi=sbi: nc.vector.tensor_scalar(out=Spb[sbi][:], in0=Sin_[:, h, :], scalar1=E[:, h, cg:cg + 1], scalar2=None, op0=ALU.mult),
                              ["Sin", "E"], [("Spb", sbi)])
                            T(lambda h=h, ch=ch, gcol=gcol, sbi=sbi, pb=pb: nc.tensor.matmul(ps[pb][:, ch * 64:(ch + 1) * 64], lhsT=Spb[sbi][:], rhs=qe[:, h, gcol], start=True, stop=True),
                              [("Spb", sbi), "qe"], [("ps", pb)])
                        V(lambda h=h, cols=cols, pb=pb: nc.vector.tensor_tensor(out=o_all[:, h, cols], in0=ps[pb][:], in1=o_all[:, h, cols], op=ALU.add),
                          [("ps", pb), "o_all"], ["o_all"])
                        A(lambda h=h, cols=cols: nc.scalar.activation(out=sq[:, 0, :], in_=o_all[:, h, cols], func=AF.Square), ["o_all"], [("sq", 0)])
                        T(lambda: nc.tensor.matmul(ps[6][:], lhsT=ones_bf[:], rhs=sq[:, 0, :], start=True, stop=True), [("sq", 0), "ones_bf"], [("ps", 6)])
                        rstd_from_ps(rstd[:], 6, 512, 128)
                        V(lambda h=h, cols=cols: nc.vector.scalar_tensor_tensor(out=tmpo[:], in0=o_all[:, h, cols], scalar=gon[:, h:h + 1], in1=rstd[:], op0=ALU.mult, op1=ALU.mult),
                          ["o_all", "gon", "rstd"], ["tmpo"])
                        V(lambda h=h, cols=cols: nc.vector.tensor_tensor(out=sgT[:, h, cols], in0=tmpo[:], in1=sgT[:, h, cols], op=ALU.mult),
                          ["tmpo", "sgT"], ["sgT"])
                for g in range(4):
                    w = 2 ** (g + 1)
                    src = ue[:, g, :]
                    lo, step, k = 0, 1, 0
                    while step < w:
                        lo += step
                        dst = sw[k % 2]
                        G(lambda src=src, dst=dst, lo=lo, step=step: nc.gpsimd.tensor_tensor(out=dst[:, lo:32], in0=src[:, lo:32], in1=src[:, lo - step:32 - step], op=ALU.add),
                          ["ue", ("sw", 0), ("sw", 1)], [("sw", k % 2)])
                        src = dst
                        step *= 2
                        k += 1
                    V(lambda src=src, g=g: nc.vector.tensor_tensor(out=p16[:], in0=src[:, 16:32], in1=invc[:, g, :], op=ALU.mult), [("sw", 0), ("sw", 1), "invc"], ["p16"])
                    V(lambda g=g: nc.vector.tensor_tensor(out=p16b[:], in0=p16[:], in1=ue[:, g, 16:32], op=ALU.subtract), ["p16", "ue"], ["p16b"])
                    T(lambda g=g: nc.tensor.matmul(ps[g % 2][:, 0:16], lhsT=poolw[:, g, :], rhs=p16b[:], start=True, stop=True), ["poolw", "p16b"], [("ps", g % 2)])
                    V(lambda g=g: nc.vector.tensor_scalar(out=pp[:, g, 0:16], in0=ps[g % 2][:, 0:16], scalar1=pscale[:, g:g + 1], scalar2=None, op0=ALU.mult),
                      [("ps", g % 2), "pscale"], ["pp"])
                out_proj(w_out, "w_out", lambda k, cols: (sgT[:, k, cols] if k < 4 else pp[:, k - 4, cols]), lambda k, tg: ["sgT", "pp"])
                S.barrier()

            ffn(wi2_d, wo2_d, g_f2, "g_f2")
            ffn(wi1_d, wo1_d, g_f1, "g_f1")

            with contextlib.ExitStack() as ph:
                w_in = sb("w_in", [128, KC, 1860], BF16, stack=ph)
                load_wcast(w_in, w_in_d, 1860, "w_in")
                w_rot = sb("w_rot", [128, KC, 1600], BF16, stack=ph)
                load_wcast(w_rot, w_rot_d, 1600, "w_rot")
                ropec = sb("ropec", [128, 4], stack=ph)
                S.dma("sp", ropec[:], ropec_d[:, :], writes=["ropec"])
                hnT = sb("hnT", [128, KC, 512], BF16, stack=ph)
                sq = sb("sq", [128, 2, 512], BF16, stack=ph)
                rstd = sb("rstd", [128, 512], stack=ph)
                posi = sb("posi", [128, 512], I32, stack=ph)
                posf = sb("posf", [128, 512], stack=ph)
                ang = sb("ang", [128, 512], stack=ph)
                ang2 = sb("ang2", [128, 512], stack=ph)
                ni = sb("ni", [128, 512], I32, stack=ph)
                nf = sb("nf", [128, 512], stack=ph)
                tabs = {nm: sb(nm, [128, 512], stack=ph) for nm in ("CS128", "SN128", "CS64", "SN64")}
                t1 = sb("t1", [128, 512], stack=ph)
                t2 = sb("t2", [128, 512], stack=ph)
                qbuf = sb("qbuf", [128, 8, 512], BF16, stack=ph)
                kbuf = sb("kbuf", [128, 2, 512], BF16, stack=ph)
                qibuf = sb("qibuf", [64, 4, 512], BF16, stack=ph)
                kibuf = sb("kibuf", [64, 512], BF16, stack=ph)
                vbuf = sb("vbuf", [128, 4, 2, 130], BF16, stack=ph)
                wibuf = sb("wibuf", [128, 4, 8], stack=ph)
                V(lambda: nc.vector.memset(vbuf[:, :, :, 128:129], 1.0), [], ["vbuf"])
                V(lambda: nc.vector.memset(vbuf[:, :, :, 129:130], 0.0), [], ["vbuf"])

                def sin_table(src_ang, out_tab, np_, sgn_col=None):
                    V(lambda: nc.vector.tensor_scalar(out=ni[0:np_, :], in0=src_ang[0:np_, :], scalar1=1.0 / TWO_PI, scalar2=None, op0=ALU.mult), ["ang"], ["ni"])
                    V(lambda: nc.vector.tensor_copy(out=nf[0:np_, :], in_=ni[0:np_, :]), ["ni"], ["nf"])
                    V(lambda: nc.vector.scalar_tensor_tensor(out=t1[0:np_, :], in0=nf[0:np_, :], scalar=-CW1, in1=src_ang[0:np_, :], op0=ALU.mult, op1=ALU.add), ["nf", "ang"], ["t1"])
                    V(lambda: nc.vector.scalar_tensor_tensor(out=t1[0:np_, :], in0=nf[0:np_, :], scalar=-CW2, in1=t1[0:np_, :], op0=ALU.mult, op1=ALU.add), ["nf", "t1"], ["t1"])
                    V(lambda: nc.vector.tensor_scalar(out=t1[0:np_, :], in0=t1[0:np_, :], scalar1=float(np.pi), scalar2=-float(np.pi), op0=ALU.min, op1=ALU.max), ["t1"], ["t1"])
                    A(lambda: nc.scalar.activation(out=out_tab[0:np_, :], in_=t1[0:np_, :], func=AF.Sin), ["t1"], ["tab"])
                    if sgn_col is not None:
                        V(lambda: nc.vector.tensor_scalar(out=out_tab[0:np_, :], in0=out_tab[0:np_, :], scalar1=sgn_col, scalar2=None, op0=ALU.mult), ["tab", "ropec"], ["tab"])

                def rope_proj(c0, r0, m, cs, sn, dst, dkey, pi):
                    pa, pbk = (0, 1) if pi % 2 == 0 else (2, 3)
                    for kc in range(KC):
                        T(lambda kc=kc: nc.tensor.matmul(ps[pa][0:m, :], lhsT=w_in[:, kc, c0:c0 + m], rhs=hnT[:, kc, :], start=(kc == 0), stop=(kc == KC - 1)),
                          ["w_in", "hnT"], [("ps", pa)], inc=(kc == KC - 1))
                    for kc in range(KC):
                        T(lambda kc=kc: nc.tensor.matmul(ps[pbk][0:m, :], lhsT=w_rot[:, kc, r0:r0 + m], rhs=hnT[:, kc, :], start=(kc == 0), stop=(kc == KC - 1)),
                          ["w_rot", "hnT"], [("ps", pbk)], inc=(kc == KC - 1))
                    V(lambda: nc.vector.tensor_tensor(out=t1[0:m, :], in0=ps[pa][0:m, :], in1=cs[0:m, :], op=ALU.mult), [("ps", pa), "tab"], ["t1"])
                    V(lambda: nc.vector.tensor_tensor(out=t2[0:m, :], in0=ps[pbk][0:m, :], in1=sn[0:m, :], op=ALU.mult), [("ps", pbk), "tab"], ["t2"])
                    G(lambda: nc.gpsimd.tensor_tensor(out=dst, in0=t1[0:m, :], in1=t2[0:m, :], op=ALU.add), ["t1", "t2"], [dkey])

                for tg in range(4):
                    cols = slice(tg * 512, (tg + 1) * 512)
                    norm_h(cols, g_mix, "g_mix", sq, rstd, lambda kc: hnT[:, kc, :], "hnT")
                    S.dma("sp", posi[:], pos_d[0:1, cols].to_broadcast([128, 512]), writes=["posi"])
                    V(lambda: nc.vector.tensor_copy(out=posf[:], in_=posi[:]), ["posi"], ["posf"])
                    for (inv_c, sgn_c, np_, csn, snn) in ((0, 1, 128, "CS128", "SN128"), (2, 3, 64, "CS64", "SN64")):
                        V(lambda inv_c=inv_c, np_=np_: nc.vector.tensor_scalar(out=ang[0:np_, :], in0=posf[0:np_, :], scalar1=ropec[0:np_, inv_c:inv_c + 1], scalar2=None, op0=ALU.mult),
                          ["posf", "ropec"], ["ang"])
                        sin_table(ang, tabs[snn], np_, ropec[0:np_, sgn_c:sgn_c + 1])
                        V(lambda np_=np_: nc.vector.tensor_scalar(out=ang2[0:np_, :], in0=ang[0:np_, :], scalar1=float(np.pi / 2), scalar2=None, op0=ALU.add), ["ang"], ["ang"])
                        sin_table(ang2, tabs[csn], np_, None)
                    pi = 0
                    for h in range(8):
                        rope_proj(h * 128, h * 128, 128, tabs["CS128"], tabs["SN128"], qbuf[:, h, :], "qbuf", pi)
                        pi += 1
                    for g in range(2):
                        rope_proj(1024 + g * 128, 1024 + g * 128, 128, tabs["CS128"], tabs["SN128"], kbuf[:, g, :], "kbuf", pi)
                        pi += 1
                    for h in range(4):
                        rope_proj(1536 + h * 64, 1280 + h * 64, 64, tabs["CS64"], tabs["SN64"], qibuf[:, h, :], "qibuf", pi)
                        pi += 1
                    rope_proj(1792, 1536, 64, tabs["CS64"], tabs["SN64"], kibuf[:, :], "kibuf", pi)
                    for j in range(4):
                        tcol = slice(j * 128, (j + 1) * 128)
                        for kc in range(KC):
                            T(lambda kc=kc, tcol=tcol: nc.tensor.matmul(ps[4][:, 0:256], lhsT=hnT[:, kc, tcol], rhs=w_in[:, kc, 1280:1536], start=(kc == 0), stop=(kc == KC - 1)),
                              ["w_in", "hnT"], [("ps", 4)], inc=(kc == KC - 1))
                        A(lambda j=j: nc.scalar.copy(out=vbuf[:, j, :, 0:128], in_=ps[4][:, 0:256].rearrange("p (g d) -> p g d", g=2)), [("ps", 4)], ["vbuf"])
                        for kc in range(KC):
                            T(lambda kc=kc, tcol=tcol: nc.tensor.matmul(ps[5][:, 0:4], lhsT=hnT[:, kc, tcol], rhs=w_in[:, kc, 1856:1860], start=(kc == 0), stop=(kc == KC - 1)),
                              ["w_in", "hnT"], [("ps", 5)], inc=(kc == KC - 1))
                        A(lambda j=j: nc.scalar.activation(out=wibuf[:, j, 0:4], in_=ps[5][:, 0:4], func=AF.Abs), [("ps", 5)], ["wibuf"])
                        A(lambda j=j: nc.scalar.activation(out=wibuf[:, j, 4:8], in_=ps[5][:, 0:4], func=AF.Sign), [("ps", 5)], ["wibuf"])
                    S.dma("sp", o_q.rearrange("p (h t) -> p h t", h=8)[:, :, cols], qbuf[:], reads=["qbuf"])
                    S.dma("sp", o_k.rearrange("p (h t) -> p h t", h=2)[:, :, cols], kbuf[:], reads=["kbuf"])
                    S.dma("sp", o_qi.rearrange("p (h t) -> p h t", h=4)[:, :, cols], qibuf[:], reads=["qibuf"])
                    S.dma("sp", o_ki[:, cols], kibuf[:], reads=["kibuf"])
                    S.dma("sp", o_v[:, tg * 1040:(tg + 1) * 1040], vbuf[:].rearrange("p j g d -> p (j g d)"), reads=["vbuf"])
                    S.dma("sp", o_wi[:, tg * 32:(tg + 1) * 32], wibuf[:].rearrange("p j c -> p (j c)"), reads=["wibuf"])
                S.barrier()
            dump3(o_hT, hT, KC, lambda k: [("hT", k)])
            S.barrier()

        if launch == "C":
            i_hT = din("st_hT", [128, KC * NTOK])
            i_q = din("st_q", [128, 8 * NTOK], BF16)
            i_k = din("st_k", [128, 2 * NTOK], BF16)
            i_qi = din("st_qi", [64, 4 * NTOK], BF16)
            i_ki = din("st_ki", [64, NTOK], BF16)
            i_v = din("st_v", [128, 16 * 260], BF16)
            i_wi = din("st_wi", [128, 16 * 8])
            x_k = din("x_k", [128, 2 * NTOK], BF16)
            x_ki = din("x_ki", [64, NTOK], BF16)
            x_v = din("x_v", [128, 16 * 260], BF16)
            pb_d = din("c_pbias", [128, 1])
            caus_d = din("c_caus", [128, 128])
            wout_d = din("odd_w_out", [D, D])
            g_f2 = load_gain(din("g_ffn2_1", [1, D]), "g_f2")
            g_fin = load_gain(din("g_final", [1, D]), "g_fin")
            wi2_d = din("ffn2_wi", [D, 2 * DFF])
            wo2_d = din("ffn2_wo", [DFF, D])
            y_d = dout("y", [NTOK, D])

            load3(hT, i_hT, KC, lambda k: [("hT", k)])
            with contextlib.ExitStack() as pq:
                qT = sb("qT", [128, 8, NTOK], BF16, stack=pq)
                load3(qT, i_q, 8, lambda k: [("qT", i, k // 4) for i in range(16)])
                with contextlib.ExitStack() as ph:
                    kT = sb("kT", [128, 2, SEQ], BF16, stack=ph)
                    S.dma("sp", kT[:, :, 0:NTOK], x_k.rearrange("p (g t) -> p g t", g=2), writes=["kT"])
                    S.dma("sp", kT[:, :, NTOK:SEQ], i_k.rearrange("p (g t) -> p g t", g=2), writes=["kT"])
                    va = sb("va", [128, 32, 260], BF16, stack=ph)
                    S.dma("sp", va[:, 0:16, :], x_v.rearrange("p (j c) -> p j c", j=16), writes=["va"])
                    S.dma("sp", va[:, 16:32, :], i_v.rearrange("p (j c) -> p j c", j=16), writes=["va"])
                    kiT = sb("kiT", [64, SEQ], BF16, stack=ph)
                    S.dma("sp", kiT[:, 0:NTOK], x_ki[:, :], writes=["kiT"])
                    S.dma("sp", kiT[:, NTOK:SEQ], i_ki[:, :], writes=["kiT"])
                    qiT = sb("qiT", [64, 4, NTOK], BF16, stack=ph)
                    load3(qiT, i_qi, 4, lambda k: ["qiT"])
                    wi = sb("wi", [128, 16, 8], stack=ph)
                    S.dma("sp", wi[:].rearrange("p j c -> p (j c)"), i_wi[:, :], writes=["wi"])
                    pbc = sb("pbc", [128, 1], stack=ph)
                    S.dma("sp", pbc[:], pb_d[:, :], writes=["pbc"])
                    caus = sb("caus", [128, 128], stack=ph)
                    S.dma("sp", caus[:], caus_d[:, :], writes=["caus"])
                    isc = sb("isc", [128, SEQ], stack=ph)
                    junk = sb("junk", [128, SEQ], BF16, stack=ph)
                    selT = sb("selT", [128, 32, 128], BF16, stack=ph)
                    rl = [sb("rl%d" % i, [128, 512], stack=ph) for i in range(2)]
                    ebuf = [sb("ebuf%d" % i, [128, 512], BF16, stack=ph) for i in range(2)]
                    pT = [sb("pTb%d" % i, [128, 4, 128], BF16, stack=ph) for i in range(2)]
                    otok = sb("otok", [128, 4, 128], BF16, stack=ph)
                    sm = sb("sm", [128, 8], stack=ph)

                    for i in range(16):
                        qc = slice(i * 128, (i + 1) * 128)
                        n_kb = 16 + i + 1
                        n_k = n_kb * 128
                        ngr = (n_k + 511) // 512
                        for kg in range(ngr):
                            k0 = kg * 512
                            w = min(512, n_k - k0)
                            for h in range(4):
                                T(lambda h=h, k0=k0, w=w: nc.tensor.matmul(ps[2 + h][:, 0:w], lhsT=qiT[:, h, qc], rhs=kiT[:, k0:k0 + w], start=True, stop=True),
                                  ["qiT", "kiT"], [("ps", 2 + h)])
                            for h in range(4):
                                rb = h % 2
                                A(lambda h=h, rb=rb, w=w: nc.scalar.activation(out=rl[rb][:, 0:w], in_=ps[2 + h][:, 0:w], func=AF.Relu, scale=wi[:, i, h:h + 1]),
                                  [("ps", 2 + h), "wi"], [("rl", rb)])
                                if h == 0:
                                    V(lambda rb=rb, k0=k0, w=w: nc.vector.tensor_scalar(out=isc[:, k0:k0 + w], in0=rl[rb][:, 0:w], scalar1=wi[:, i, 4:5], scalar2=None, op0=ALU.mult),
                                      [("rl", rb), "wi"], ["isc"])
                                else:
                                    V(lambda h=h, rb=rb, k0=k0, w=w: nc.vector.scalar_tensor_tensor(out=isc[:, k0:k0 + w], in0=rl[rb][:, 0:w], scalar=wi[:, i, 4 + h:5 + h],
                                                                                                    in1=isc[:, k0:k0 + w], op0=ALU.mult, op1=ALU.add),
                                      [("rl", rb), "wi", "isc"], ["isc"])
                        V(lambda: nc.vector.tensor_reduce(out=sm[:, 1:2], in_=isc[:, 0:n_k], axis=AX.X, op=ALU.max), ["isc"], ["sm"])
                        V(lambda: nc.vector.tensor_reduce(out=sm[:, 0:1], in_=isc[:, 0:n_k], axis=AX.X, op=ALU.min), ["isc"], ["sm"])
                        V(lambda: nc.vector.tensor_scalar(out=isc[:, 0:NTOK], in0=isc[:, 0:NTOK], scalar1=pbc[:, 0:1], scalar2=None, op0=ALU.add), ["isc", "pbc"], ["isc"])
                        V(lambda: nc.vector.tensor_tensor(out=isc[:, n_k - 128:n_k], in0=isc[:, n_k - 128:n_k], in1=caus[:], op=ALU.add), ["isc", "caus"], ["isc"])
                        for it in range(N_IT):
                            V(lambda: nc.vector.tensor_scalar(out=sm[:, 2:3], in0=sm[:, 0:1], scalar1=sm[:, 1:2], scalar2=0.5, op0=ALU.add, op1=ALU.mult), ["sm"], ["sm"])
                            V(lambda: nc.vector.tensor_scalar(out=junk[:, 0:n_k], in0=isc[:, 0:n_k], scalar1=sm[:, 2:3], scalar2=None, op0=ALU.is_ge, op1=ALU.add, accum_out=sm[:, 3:4]),
                              ["isc", "sm"], ["junk", "sm"])
                            V(lambda: nc.vector.tensor_scalar(out=sm[:, 4:5], in0=sm[:, 3:4], scalar1=255.5, scalar2=None, op0=ALU.is_ge), ["sm"], ["sm"])
                            V(lambda: nc.vector.tensor_tensor(out=sm[:, 5:6], in0=sm[:, 2:3], in1=sm[:, 0:1], op=ALU.subtract), ["sm"], ["sm"])
                            V(lambda: nc.vector.scalar_tensor_tensor(out=sm[:, 0:1], in0=sm[:, 5:6], scalar=sm[:, 4:5], in1=sm[:, 0:1], op0=ALU.mult, op1=ALU.add), ["sm"], ["sm"])
                            V(lambda: nc.vector.tensor_tensor(out=sm[:, 5:6], in0=sm[:, 1:2], in1=sm[:, 2:3], op=ALU.subtract), ["sm"], ["sm"])
                            V(lambda: nc.vector.scalar_tensor_tensor(out=sm[:, 1:2], in0=sm[:, 5:6], scalar=sm[:, 4:5], in1=sm[:, 2:3], op0=ALU.mult, op1=ALU.add), ["sm"], ["sm"])
                        V(lambda: nc.vector.tensor_scalar(out=junk[:, 0:n_k], in0=isc[:, 0:n_k], scalar1=sm[:, 0:1], scalar2=None, op0=ALU.is_ge), ["isc", "sm"], ["junk"])
                        for kb0 in range(0, n_kb, 4):
                            nb = min(4, n_kb - kb0)
                            half = 0
                            for b in range(nb):
                                kb = kb0 + b
                                T(lambda b=b, kb=kb, half=half: nc.tensor.transpose(psb[:, half * 512 + b * 128:half * 512 + (b + 1) * 128], junk[:, kb * 128:(kb + 1) * 128], ident_bf[:]),
                                  ["junk", "ident_bf"], ["psb"], inc=(b == nb - 1))
                            A(lambda kb0=kb0, nb=nb, half=half: nc.scalar.copy(out=selT[:, kb0:kb0 + nb, :], in_=psb[:, half * 512:half * 512 + nb * 128].rearrange("p (b t) -> p b t", b=nb)),
                              ["psb"], [("selT", kb0)])
                        for g in range(2):
                            for kb in range(n_kb):
                                sc = kb % 2
                                T(lambda kb=kb, g=g, sc=sc: nc.tensor.matmul(ps[sc][:], lhsT=kT[:, g, kb * 128:(kb + 1) * 128], rhs=qT[:, 4 * g:4 * g + 4, qc], start=True, stop=True),
                                  ["kT", ("qT", i, g)], [("ps", sc)])
                                A(lambda sc=sc: nc.scalar.activation(out=ebuf[sc][:], in_=ps[sc][:], func=AF.Exp, scale=float(128 ** -0.5)), [("ps", sc)], [("ebuf", sc)])
                                V(lambda kb=kb, sc=sc: nc.vector.tensor_tensor(out=pT[sc][:], in0=ebuf[sc][:].rearrange("p (h t) -> p h t", h=4),
                                                                              in1=selT[:, kb:kb + 1, :].to_broadcast([128, 4, 128]), op=ALU.mult),
                                  [("ebuf", sc), ("selT", (kb // 4) * 4)], [("pT", sc)])
                                for hh in range(4):
                                    T(lambda kb=kb, g=g, sc=sc, hh=hh: nc.tensor.matmul(ps[2 + hh][:, 0:129], lhsT=pT[sc][:, hh, :], rhs=va[:, kb, g * 130:g * 130 + 129],
                                                                                        start=(kb == 0), stop=(kb == n_kb - 1)),
                                      [("pT", sc), "va"], [("ps", 2 + hh)], inc=(hh == 3 or kb == n_kb - 1))
                            for hh in range(4):
                                V(lambda hh=hh: nc.vector.reciprocal(out=sm[:, 6:7], in_=ps[2 + hh][:, 128:129]), [("ps", 2 + hh)], ["sm"])
                                V(lambda hh=hh: nc.vector.tensor_scalar(out=otok[:, hh, :], in0=ps[2 + hh][:, 0:128], scalar1=sm[:, 6:7], scalar2=None, op0=ALU.mult),
                                  [("ps", 2 + hh), "sm"], ["otok"])
                            for hh in range(4):
                                T(lambda hh=hh: nc.tensor.transpose(psb[:, hh * 128:(hh + 1) * 128], otok[:, hh, :], ident_bf[:]), ["otok", "ident_bf"], ["psb"], inc=(hh == 3))
                            A(lambda g=g: nc.scalar.copy(out=qT[:, 4 * g:4 * g + 4, qc], in_=psb[:, 0:512].rearrange("p (h t) -> p h t", h=4)), ["psb"], [("qT", i, g)])
                    S.barrier()
                with contextlib.ExitStack() as ph:
                    w_out = sb("w_out", [128, KC, D], BF16, stack=ph)
                    load_wcast(w_out, wout_d, D, "w_out")
                    out_proj(w_out, "w_out", lambda k, cols: qT[:, k, cols], lambda k, tg: [("qT", 4 * tg + j, gg) for j in range(4) for gg in range(2)])
                    S.barrier()

            ffn(wi2_d, wo2_d, g_f2, "g_f2")

            with contextlib.ExitStack() as ph:
                sq = sb("sq", [128, 2, 512], BF16, stack=ph)
                rstd = sb("rstd", [128, 512], stack=ph)
                yT = sb("yT", [128, KC, 512], stack=ph)
                yo = [sb("yo%d" % i, [128, D], stack=ph) for i in range(2)]
                io = 0
                for tg in range(4):
                    cols = slice(tg * 512, (tg + 1) * 512)
                    rstd_ps(lambda kc: hT[:, kc, cols], KC, 512, sq, 6, lambda kc: [("hT", kc)])
                    rstd_from_ps(rstd[:], 6, 512, D)
                    for kc in range(KC):
                        V(lambda kc=kc, cols=cols: nc.vector.scalar_tensor_tensor(out=yT[:, kc, :], in0=hT[:, kc, cols], scalar=g_fin[:, kc:kc + 1], in1=rstd[:],
                                                                                 op0=ALU.mult, op1=ALU.mult),
                          [("hT", kc), "g_fin", "rstd"], [("yT", kc)])
                    for j in range(4):
                        yb = io % 2
                        io += 1
                        for k2 in range(2):
                            pb = k2
                            for kq in range(4):
                                kc = k2 * 4 + kq
                                T(lambda kc=kc, kq=kq, j=j, pb=pb: nc.tensor.transpose(ps[pb][:, kq * 128:(kq + 1) * 128], yT[:, kc, j * 128:(j + 1) * 128], ident[:]),
                                  [("yT", kc), "ident"], [("ps", pb)], inc=(kq == 3))
                            if k2 == 0:
                                V(lambda yb=yb, pb=pb: nc.vector.tensor_copy(out=yo[yb][:, 0:512], in_=ps[pb][:]), [("ps", pb)], [("yo", yb)])
                            else:
                                A(lambda yb=yb, pb=pb: nc.scalar.copy(out=yo[yb][:, 512:1024], in_=ps[pb][:]), [("ps", pb)], [("yo", yb)])
                        tt = tg * 4 + j
                        S.dma("sp", y_d[tt * 128:(tt + 1) * 128, :], yo[yb][:], reads=[("yo", yb)], writes=[("y_dram", tt)])
                S.barrier()

        S.barrier()
    return nc


_PROG = {}


def _prog(launch):
    if launch not in _PROG:
        _PROG[launch] = build_program(launch)
    return _PROG[launch]


def _f32(a):
    return np.ascontiguousarray(np.asarray(a), dtype=np.float32)


def _consts():
    c = {"c_ident": np.eye(128, dtype=np.float32)}
    t = np.arange(64)
    c["c_tin64"] = np.where(t[:, None] <= t[None, :], -1.0 / 16.0, 0.0).astype(np.float32)
    c["c_uex64"] = np.where(t[:, None] > t[None, :], -1.0 / 16.0, 0.0).astype(np.float32)
    cm = (t[:, None] <= t[None, :]).astype(np.float32)
    c["c_cmask"] = np.tile(cm, (1, 4)).astype(np.float32)
    s = np.arange(128)
    c["c_caus"] = np.where(s[None, :] <= s[:, None], 0.0, NEG).astype(np.float32)
    r = np.arange(128)
    inv128 = (10000.0 ** (-(np.arange(0, 128, 2, dtype=np.float32)) / 128.0)).astype(np.float32)
    inv64 = (10000.0 ** (-(np.arange(0, 64, 2, dtype=np.float32)) / 64.0)).astype(np.float32)
    rope = np.zeros((128, 4), np.float32)
    rope[:, 0] = inv128[r % 64]
    rope[:, 1] = np.where(r < 64, -1.0, 1.0)
    rope[:, 2] = inv64[r % 32]
    rope[:, 3] = np.where((r % 64) < 32, -1.0, 1.0)
    c["c_rope"] = rope
    return c


def _rot_perm(n_heads, hd):
    idx = np.arange(n_heads * hd).reshape(n_heads, hd)
    return np.concatenate([idx[:, hd // 2:], idx[:, :hd // 2]], axis=1).reshape(-1)


def _run(launch, in_maps):
    nc = _prog(launch)
    ncores = int(os.environ.get("KCORES", str(N_CORES)))
    res = run_bass_kernel_spmd(nc, in_maps[:ncores], core_ids=list(range(ncores)))
    r = list(res.results)
    return r + [r[i % ncores] for i in range(ncores, N_CORES)]


def kernel(**inputs):
    debug = inputs.pop("_debug", None)
    x = _f32(inputs["x"])
    pos = np.ascontiguousarray(inputs["positions"], dtype=np.int32)
    C = _consts()
    bf = ml_dtypes.bfloat16

    def row(a):
        return _f32(a).reshape(1, -1)

    gate_wb = np.concatenate([_f32(inputs["gla_gate_w"])[0], _f32(inputs["gla_gate_b"])[0][None, :]], axis=0)
    shared = {
        "c_ident": C["c_ident"], "c_tin64": C["c_tin64"], "c_uex64": C["c_uex64"], "c_cmask": C["c_cmask"],
        "g_ffn1_0": row(inputs["ffn1_norm"][0]), "g_mix0": row(inputs["mix_norm"][0]),
        "ffn_wi": _f32(inputs["ffn1_wi"][0]), "ffn_wo": _f32(inputs["ffn1_wo"][0]),
        "even_w_in": _f32(inputs["even_w_in"][0]), "gate_wb": _f32(gate_wb),
        "pool_w": _f32(inputs["pool_w"][0]), "pool_scale": row(inputs["pool_scale"][0]),
    }
    maps = []
    for c in range(N_CORES):
        b, half = c // 2, c % 2
        m = dict(shared)
        m["x"] = np.ascontiguousarray(x[b, half * NTOK:(half + 1) * NTOK, :])
        maps.append(m)
    if LITE:
        for m in maps:
            m.pop("ffn_wi"); m.pop("ffn_wo")
    ra = _run("A", maps)
    if debug == "A":
        return ra

    w_in1 = _f32(inputs["odd_w_in"][0])
    perm = np.concatenate([_rot_perm(8, 128), 1024 + _rot_perm(2, 128), 1536 + _rot_perm(4, 64), 1792 + _rot_perm(1, 64)])
    w_rot = np.ascontiguousarray(w_in1[:, perm])
    shared = {
        "c_ident": C["c_ident"], "c_rope": C["c_rope"],
        "gla_out_norm": row(inputs["gla_out_norm"][0]), "pool_w": _f32(inputs["pool_w"][0]), "pool_scale": row(inputs["pool_scale"][0]),
        "even_w_out": _f32(inputs["even_w_out"][0]),
        "g_ffn2_0": row(inputs["ffn2_norm"][0]), "g_ffn1_1": row(inputs["ffn1_norm"][1]), "g_mix1": row(inputs["mix_norm"][1]),
        "ffn2_wi": _f32(inputs["ffn2_wi"][0]), "ffn2_wo": _f32(inputs["ffn2_wo"][0]),
        "ffn1_wi": _f32(inputs["ffn1_wi"][1]), "ffn1_wo": _f32(inputs["ffn1_wo"][1]),
        "odd_w_in": w_in1, "odd_w_rot": w_rot,
    }
    tloc = np.arange(16, dtype=np.float32)
    maps = []
    for c in range(N_CORES):
        b, half = c // 2, c % 2
        m = dict(shared)
        for k in ("st_hT", "st_o", "st_qe", "st_sg", "st_pp", "st_bl", "st_u16"):
            m[k] = ra[c][k]
        if half == 1:
            m["x_sin"] = ra[c - 1]["st_send"]
            m["x_halo"] = ra[c - 1]["st_utail"]
        else:
            m["x_sin"] = np.zeros((64, 512), np.float32)
            m["x_halo"] = np.zeros((128, 64), np.float32)
        invc = np.zeros((128, 4, 16), np.float32)
        for g, w in enumerate((2, 4, 8, 16)):
            cnt = np.full(16, float(w), np.float32) if half == 1 else np.minimum(tloc + 1.0, float(w))
            invc[:, g, :] = (1.0 / cnt)[None, :]
        m["c_invcnt"] = invc.reshape(128, 64)
        m["positions"] = np.ascontiguousarray(pos[b, half * NTOK:(half + 1) * NTOK]).reshape(1, NTOK)
        maps.append(m)
    rb = _run("B", maps)
    if debug == "B":
        return ra, rb

    shared = {
        "c_ident": C["c_ident"], "c_caus": C["c_caus"],
        "odd_w_out": _f32(inputs["odd_w_out"][0]),
        "g_ffn2_1": row(inputs["ffn2_norm"][1]), "g_final": row(inputs["final_norm"]),
        "ffn2_wi": _f32(inputs["ffn2_wi"][1]), "ffn2_wo": _f32(inputs["ffn2_wo"][1]),
    }
    maps = []
    for c in range(N_CORES):
        b, half = c // 2, c % 2
        m = dict(shared)
        m["st_hT"] = rb[c]["st_hT_o"]
        for k in ("st_q", "st_k", "st_qi", "st_ki", "st_v", "st_wi"):
            m[k] = rb[c][k]
        if half == 1:
            m["x_k"], m["x_ki"], m["x_v"] = rb[c - 1]["st_k"], rb[c - 1]["st_ki"], rb[c - 1]["st_v"]
            m["c_pbias"] = np.zeros((128, 1), np.float32)
        else:
            m["x_k"] = np.zeros((128, 2 * NTOK), bf)
            m["x_ki"] = np.zeros((64, NTOK), bf)
            m["x_v"] = np.zeros((128, 16 * 260), bf)
            m["c_pbias"] = np.full((128, 1), NEG, np.float32)
        maps.append(m)
    rc = _run("C", maps)
    out = np.zeros((4, SEQ, D), dtype=np.float32)
    for c in range(N_CORES):
        b, half = c // 2, c % 2
        out[b, half * NTOK:(half + 1) * NTOK, :] = rc[c]["y"]
    return out
```

```python
import contextlib
import numpy as np
import ml_dtypes
import concourse.bass as bass
import concourse.mybir as mybir
from concourse.bass_utils import run_bass_kernel_spmd

F32 = mybir.dt.float32
BF16 = mybir.dt.bfloat16
I32 = mybir.dt.int32
ALU = mybir.AluOpType
AF = mybir.ActivationFunctionType
AX = mybir.AxisListType

D = 1024
KC = 8
DFF = 2816
FC = 22
NTOK = 2048
SEQ = 4096
EPS = 1e-6
N_CORES = 8
NEG = -1.0e30
N_IT = 14
import os
LITE = os.environ.get("KLITE") == "1"
KSTOP = int(os.environ.get("KSTOP", "99"))
KSKIP = set(os.environ.get("KSKIP", "").split(","))
TWO_PI = 2.0 * np.pi
CW1 = 6.28125
CW2 = TWO_PI - 6.28125


class Sched:
    def __init__(self, nc, es, n_dma_sems=6):
        self.nc = nc
        self.eng = {"pe": nc.tensor, "act": nc.scalar, "dve": nc.vector, "pool": nc.gpsimd, "sp": nc.sync}
        self.sems = {}
        self.cnt = {}
        for e in ("pe", "act", "dve", "pool"):
            self.sems[e] = es.enter_context(nc.semaphore("s_" + e))
            self.cnt[e] = 0
        self.dq = {}
        self.dq_next = {}
        for q in ("sp", "pool"):
            names = []
            for i in range(n_dma_sems):
                nm = "d_%s%d" % (q, i)
                self.sems[nm] = es.enter_context(nc.semaphore(nm))
                self.cnt[nm] = 0
                names.append(nm)
            self.dq[q] = names
            self.dq_next[q] = 0
        for nm in ("cc1", "cc2", "cc3", "cc4", "cc5"):
            self.sems[nm] = es.enter_context(nc.semaphore("s_" + nm))
            self.cnt[nm] = 0
        self.seen = {e: {} for e in self.eng}
        self.lastw = {}
        self.readers = {}

    def collective(self, name, fn):
        self.barrier()
        fn().then_inc(self.sems[name], 1)
        self.cnt[name] = 1
        self.barrier()

    def _wait(self, e, x, v):
        if v <= 0 or self.seen[e].get(x, 0) >= v:
            return
        self.eng[e].wait_ge(self.sems[x], v)
        self.seen[e][x] = v

    def _deps(self, e, reads, writes):
        d = {}

        def add(tok, war=False):
            if tok is None:
                return
            x, v = tok
            if x == e and e == "pe":
                return
            if d.get(x, 0) < v:
                d[x] = v

        for r in reads:
            add(self.lastw.get(r))
            if r == "psb" or (isinstance(r, tuple) and r[0] == "ps"):
                for x2, tok in self.readers.get(r, {}).items():
                    if x2 != e:
                        add(tok)
        for w in writes:
            add(self.lastw.get(w))
            for tok in self.readers.get(w, {}).values():
                add(tok, war=True)
        return d

    def _record(self, tok, reads, writes):
        for r in reads:
            self.readers.setdefault(r, {})[tok[0]] = tok
        for w in writes:
            self.lastw[w] = tok
            self.readers[w] = {}

    def op(self, e, fn, reads=(), writes=(), inc=True):
        d = self._deps(e, reads, writes)
        for x, v in d.items():
            self._wait(e, x, v)
        ins = fn()
        if inc:
            self.cnt[e] += 1
            ins.then_inc(self.sems[e], 1)
            tok = (e, self.cnt[e])
        else:
            tok = (e, self.cnt[e] + 1)
        self._record(tok, reads, writes)
        return ins

    def dma(self, q, out, in_, reads=(), writes=()):
        d = self._deps(q, reads, writes)
        i = self.dq_next[q]
        self.dq_next[q] = (i + 1) % len(self.dq[q])
        nm = self.dq[q][i]
        self._wait(q, nm, self.cnt[nm])
        for x, v in d.items():
            self._wait(q, x, v)
        self.cnt[nm] += 16
        self.eng[q].dma_start(out=out, in_=in_).then_inc(self.sems[nm], 16)
        self._record((nm, self.cnt[nm]), reads, writes)

    def barrier(self, engines=("pe", "act", "dve", "pool", "sp")):
        for e in engines:
            for x, v in self.cnt.items():
                if x == e and e == "pe":
                    continue
                self._wait(e, x, v)


def build_program(launch):
    nc = bass.Bass("TRN2", target_bir_lowering=False)

    FUSED = (launch == "F")
    parts = "ABC" if FUSED else launch
    CUR = ["A"]
    STATE = {}
    EXCH_IN = {}
    EXCH_OUT = {}
    if FUSED:
        def internal(name, shape, dt):
            return nc.dram_tensor(name, list(shape), dt, kind="Internal", addr_space="Local").ap()
        xa_src = internal("xa_src", [128, 1024], F32)
        xa_dst = internal("xa_dst", [256, 1024], F32)
        xk_src = internal("xk_src", [128, 4096], BF16)
        xk_dst = internal("xk_dst", [256, 4096], BF16)
        xki_src = internal("xki_src", [128, 2048], BF16)
        xki_dst = internal("xki_dst", [256, 2048], BF16)
        xv_src = [internal("xv_src%d" % i, [128, 2080], BF16) for i in range(2)]
        xv_dst = [internal("xv_dst%d" % i, [256, 2080], BF16) for i in range(2)]
        EXCH_OUT = {"st_send": xa_src[0:64, 0:512], "st_utail": xa_src[:, 512:576],
                    "st_k": xk_src[:, :], "st_ki": xki_src[0:64, :]}
        EXCH_IN = {"x_sin": xa_dst[0:64, 0:512], "x_halo": xa_dst[0:128, 512:576],
                   "x_k": xk_dst[0:128, :], "x_ki": xki_dst[0:64, :]}
        XCH2 = [("cc2", xk_src, xk_dst), ("cc3", xki_src, xki_dst), ("cc4", xv_src[0], xv_dst[0]), ("cc5", xv_src[1], xv_dst[1])]

    def din(name, shape, dt=F32):
        if FUSED:
            if name in STATE:
                return STATE[name]
            if name in EXCH_IN:
                return EXCH_IN[name]
            if name != "c_ident":
                name = CUR[0] + "_" + name
        return nc.dram_tensor(name, list(shape), dt, kind="ExternalInput").ap()

    def dout(name, shape, dt=F32):
        if FUSED and name != "y":
            if name in EXCH_OUT:
                STATE[name] = EXCH_OUT[name]
            else:
                STATE[name] = nc.dram_tensor("i_" + name, list(shape), dt, kind="Internal", addr_space="Local").ap()
            return STATE[name]
        return nc.dram_tensor(name, list(shape), dt, kind="ExternalOutput").ap()

    PAIRS = [[0, 1], [2, 3], [4, 5], [6, 7]]
    ident_d = din("c_ident", [128, 128])

    with contextlib.ExitStack() as es:
        S = Sched(nc, es)
        _uid = [0]

        def sb(name, shape, dt=F32, stack=es):
            _uid[0] += 1
            return stack.enter_context(nc.sbuf_tensor("%s_%d" % (name, _uid[0]), list(shape), dt))

        def V(fn, r=(), w=()):
            return S.op("dve", fn, reads=r, writes=w)

        def A(fn, r=(), w=()):
            return S.op("act", fn, reads=r, writes=w)

        def G(fn, r=(), w=()):
            return S.op("pool", fn, reads=r, writes=w)

        def T(fn, r=(), w=(), inc=True):
            return S.op("pe", fn, reads=r, writes=w, inc=inc)

        hT = sb("hT", [128, KC, NTOK])
        ident = sb("ident", [128, 128])
        ident_bf = sb("ident_bf", [128, 128], BF16)
        ones_bf = sb("ones_bf", [128, 128], BF16)
        eps_c = sb("eps_c", [128, 1])
        one_c = sb("one_c", [128, 1])
        ps = [es.enter_context(nc.psum_tensor("ps%d" % i, [128, 512], F32)) for i in range(7)]
        psb = es.enter_context(nc.psum_tensor("psb", [128, 1024], BF16))

        stage = [sb("stage%d" % i, [128, 1024]) for i in range(2)]
        _rr = [0]

        def cast_load(dst_ap, src_ap, view, dkey):
            i = _rr[0] % 2
            eng = "pool" if (_rr[0] // 2) % 2 == 0 else "act"
            _rr[0] += 1
            st = view(stage[i])
            S.dma("sp", st, src_ap, writes=[("stage", i)])
            if eng == "pool":
                G(lambda: nc.gpsimd.tensor_copy(out=dst_ap, in_=st), [("stage", i)], [dkey])
            else:
                A(lambda: nc.scalar.copy(out=dst_ap, in_=st), [("stage", i)], [dkey])

        S.dma("sp", ident[:], ident_d[:, :], writes=["ident"])
        V(lambda: nc.vector.tensor_copy(out=ident_bf[:], in_=ident[:]), ["ident"], ["ident_bf"])
        V(lambda: nc.vector.memset(ones_bf[:], 1.0), [], ["ones_bf"])
        V(lambda: nc.vector.memset(eps_c[:], EPS), [], ["eps"])
        V(lambda: nc.vector.memset(one_c[:], 1.0), [], ["one"])

        def load_gain(dram_row, name):
            g = sb(name, [128, KC])
            with nc.allow_non_contiguous_dma(reason="tiny gain vector"):
                S.dma("sp", g[:], dram_row.rearrange("o (kc p) -> p (o kc)", p=128), writes=[name])
            return g

        def load_state(t_sb, d_ap, key, q="sp"):
            S.dma(q, t_sb, d_ap, writes=[key])

        def rstd_ps(src_fn, nk, n, sq, pbank, key_r):
            for kc in range(nk):
                A(lambda kc=kc: nc.scalar.activation(out=sq[:, kc % 2, 0:n], in_=src_fn(kc), func=AF.Square),
                  key_r(kc), [("sq", kc % 2)])
                T(lambda kc=kc: nc.tensor.matmul(ps[pbank][:, 0:n], lhsT=ones_bf[:], rhs=sq[:, kc % 2, 0:n], start=(kc == 0), stop=(kc == nk - 1)),
                  [("sq", kc % 2), "ones_bf"], [("ps", pbank)])

        def rstd_from_ps(rstd_out, pbank, n, denom):
            A(lambda: nc.scalar.activation(out=rstd_out, in_=ps[pbank][:, 0:n], func=AF.Ln, scale=1.0 / denom, bias=eps_c[:, 0:1]),
              [("ps", pbank), "eps"], ["rstd"])
            A(lambda: nc.scalar.activation(out=rstd_out, in_=rstd_out, func=AF.Exp, scale=-0.5), ["rstd"], ["rstd"])

        def norm_h(cols, gain, gname, sq, rstd, hnT, hn_key):
            rstd_ps(lambda kc: hT[:, kc, cols], KC, 512, sq, 6, lambda kc: [("hT", kc)])
            rstd_from_ps(rstd[:], 6, 512, D)
            for kc in range(KC):
                V(lambda kc=kc: nc.vector.scalar_tensor_tensor(out=hnT(kc), in0=hT[:, kc, cols], scalar=gain[:, kc:kc + 1], in1=rstd[:],
                                                               op0=ALU.mult, op1=ALU.mult),
                  [("hT", kc), gname, "rstd"], [hn_key])

        def ffn(wi_d, wo_d, gain, gname):
            with contextlib.ExitStack() as ph:
                hnT = sb("hnT", [128, KC, 1024], BF16, stack=ph)
                actT = sb("actT", [128, FC, 1024], BF16, stack=ph)
                wo_sb = sb("wo_sb", [128, FC, D], BF16, stack=ph)
                wg_sb = [sb("wg_sb%d" % i, [128, KC, 256], BF16, stack=ph) for i in range(2)]
                wu_sb = [sb("wu_sb%d" % i, [128, KC, 256], BF16, stack=ph) for i in range(2)]
                sq = sb("sq", [128, 2, 512], BF16, stack=ph)
                rstd = sb("rstd", [128, 512], stack=ph)
                sg = [sb("sg%d" % i, [128, 512], stack=ph) for i in range(2)]
                for fc in range(FC):
                    cast_load(wo_sb[:, fc, :], wo_d[fc * 128:(fc + 1) * 128, :], lambda t: t[:, :], ("wo", fc // 2))
                it = 0
                for half in range(2):
                    tok0 = half * 1024
                    for tg in range(2):
                        cols = slice(tok0 + tg * 512, tok0 + (tg + 1) * 512)
                        norm_h(cols, gain, gname, sq, rstd, lambda kc, tg=tg: hnT[:, kc, tg * 512:(tg + 1) * 512], ("hnT", tg))
                    for fg in range(11):
                        wb = fg % 2
                        v4 = lambda t: t[:, :].rearrange("p (k c) -> p k c", k=4)
                        for hk in range(2):
                            cast_load(wg_sb[wb][:, hk * 4:(hk + 1) * 4, :],
                                      wi_d[hk * 512:(hk + 1) * 512, fg * 256:(fg + 1) * 256].rearrange("(k p) c -> p k c", p=128), v4, ("wg", wb))
                            cast_load(wu_sb[wb][:, hk * 4:(hk + 1) * 4, :],
                                      wi_d[hk * 512:(hk + 1) * 512, DFF + fg * 256:DFF + (fg + 1) * 256].rearrange("(k p) c -> p k c", p=128), v4, ("wu", wb))
                        for c in range(2):
                            fc = fg * 2 + c
                            for tg in range(2):
                                pg, pu = (0, 1) if it % 2 == 0 else (2, 3)
                                sgi = it % 2
                                it += 1
                                for kc in range(KC):
                                    T(lambda kc=kc, c=c, tg=tg, pg=pg, wb=wb: nc.tensor.matmul(
                                        ps[pg][:], lhsT=wg_sb[wb][:, kc, c * 128:(c + 1) * 128], rhs=hnT[:, kc, tg * 512:(tg + 1) * 512],
                                        start=(kc == 0), stop=(kc == KC - 1)),
                                      [("wg", wb), ("hnT", tg)], [("ps", pg)], inc=(kc == KC - 1))
                                for kc in range(KC):
                                    T(lambda kc=kc, c=c, tg=tg, pu=pu, wb=wb: nc.tensor.matmul(
                                        ps[pu][:], lhsT=wu_sb[wb][:, kc, c * 128:(c + 1) * 128], rhs=hnT[:, kc, tg * 512:(tg + 1) * 512],
                                        start=(kc == 0), stop=(kc == KC - 1)),
                                      [("wu", wb), ("hnT", tg)], [("ps", pu)], inc=(kc == KC - 1))
                                A(lambda pg=pg, sgi=sgi: nc.scalar.activation(out=sg[sgi][:], in_=ps[pg][:], func=AF.Silu),
                                  [("ps", pg)], [("sg", sgi)])
                                V(lambda pu=pu, sgi=sgi, fc=fc, tg=tg: nc.vector.tensor_tensor(
                                    out=actT[:, fc, tg * 512:(tg + 1) * 512], in0=ps[pu][:], in1=sg[sgi][:], op=ALU.mult),
                                  [("ps", pu), ("sg", sgi)], [("actT", fc, tg)])
                    io = 0
                    for dc in range(KC):
                        for tg in range(2):
                            po = 4 + (io % 2)
                            io += 1
                            cols = slice(tok0 + tg * 512, tok0 + (tg + 1) * 512)
                            for fc in range(FC):
                                T(lambda fc=fc, dc=dc, tg=tg, po=po: nc.tensor.matmul(
                                    ps[po][:], lhsT=wo_sb[:, fc, dc * 128:(dc + 1) * 128], rhs=actT[:, fc, tg * 512:(tg + 1) * 512],
                                    start=(fc == 0), stop=(fc == FC - 1)),
                                  [("wo", fc // 2), ("actT", fc, tg)], [("ps", po)], inc=(fc == FC - 1))
                            V(lambda dc=dc, cols=cols, po=po: nc.vector.scalar_tensor_tensor(
                                out=hT[:, dc, cols], in0=ps[po][:], scalar=0.5, in1=hT[:, dc, cols], op0=ALU.mult, op1=ALU.add),
                              [("ps", po), ("hT", dc)], [("hT", dc)])
                S.barrier()

        def load_wcast(dst, src_d, ncols, key, step=256):
            for c0 in range(0, ncols, step):
                c1 = min(ncols, c0 + step)
                wd = c1 - c0
                for hk in range(2):
                    cast_load(dst[:, hk * 4:(hk + 1) * 4, c0:c1],
                              src_d[hk * 512:(hk + 1) * 512, c0:c1].rearrange("(k p) c -> p k c", p=128),
                              lambda t, wd=wd: t[:, 0:4 * wd].rearrange("p (k c) -> p k c", k=4), key)

        def out_proj(w_sb, wkey, rhs_fn, rkeys):
            io = 0
            for tg in range(4):
                cols = slice(tg * 512, (tg + 1) * 512)
                for dc in range(KC):
                    po = 4 + (io % 2)
                    io += 1
                    for k in range(8):
                        T(lambda k=k, dc=dc, po=po, cols=cols: nc.tensor.matmul(
                            ps[po][:], lhsT=w_sb[:, k, dc * 128:(dc + 1) * 128], rhs=rhs_fn(k, cols), start=(k == 0), stop=(k == 7)),
                          [wkey] + rkeys(k, tg), [("ps", po)], inc=(k == 7))
                    V(lambda dc=dc, cols=cols, po=po: nc.vector.tensor_tensor(out=hT[:, dc, cols], in0=ps[po][:], in1=hT[:, dc, cols], op=ALU.add),
                      [("ps", po), ("hT", dc)], [("hT", dc)])

        def dump3(dram2d, sb3, n, reads, q="sp"):
            dv = dram2d.rearrange("p (n t) -> p n t", n=n)
            for i in range(n):
                S.dma(q, dv[:, i, :], sb3[:, i, :], reads=reads(i))

        def load3(sb3, dram2d, n, writes, q="sp"):
            dv = dram2d.rearrange("p (n t) -> p n t", n=n)
            for i in range(n):
                S.dma(q, sb3[:, i, :], dv[:, i, :], writes=writes(i))

        if "A" in parts:
            CUR[0] = "A"
            x_d = din("x", [NTOK, D])
            g_ffn = load_gain(din("g_ffn1_0", [1, D]), "g_ffn")
            g_mix = load_gain(din("g_mix0", [1, D]), "g_mix")
            if not LITE:
                wi_d = din("ffn_wi", [D, 2 * DFF])
                wo_d = din("ffn_wo", [DFF, D])
            w_in_d = din("even_w_in", [D, 2064])
            gw_d = din("gate_wb", [17, 256])
            poolw_d = din("pool_w", [4, 128, 128])
            pscale_d = din("pool_scale", [1, 512])
            tin_d = din("c_tin64", [64, 64])
            uex_d = din("c_uex64", [64, 64])
            cmask_d = din("c_cmask", [64, 256])
            o_hT = dout("st_hT", [128, KC * NTOK])
            o_o = dout("st_o", [128, 4 * NTOK])
            o_qe = dout("st_qe", [64, 4 * NTOK], BF16)
            o_sg = dout("st_sg", [128, 4 * NTOK], BF16)
            o_pp = dout("st_pp", [128, 4 * NTOK], BF16)
            o_bl = dout("st_bl", [64, 128])
            o_u16 = dout("st_u16", [128, 64])
            o_send = dout("st_send", [64, 512])
            o_utail = dout("st_utail", [128, 64])

            with contextlib.ExitStack() as ph:
                xin = sb("xin", [128, 4, D], stack=ph)
                if FUSED:
                    zf = sb("zf", [128, 1024], stack=ph)
                    zb = sb("zb", [128, 2048], BF16, stack=ph)
                    V(lambda: nc.vector.memset(zf[:], 0.0), [], ["zf"])
                    V(lambda: nc.vector.memset(zb[:], 0.0), [], ["zb"])
                    S.dma("sp", xa_src[:, :], zf[:], reads=["zf"])
                    S.dma("sp", xki_src[64:128, :], zb[64:128, :], reads=["zb"])
                for tg in range(4):
                    for j in range(4):
                        tt = tg * 4 + j
                        S.dma("sp", xin[:, j, :], x_d[tt * 128:(tt + 1) * 128, :], writes=[("xin", j)])
                    for kc in range(KC):
                        pb = kc % 2
                        for j in range(4):
                            T(lambda j=j, kc=kc, pb=pb: nc.tensor.transpose(ps[pb][:, j * 128:(j + 1) * 128], xin[:, j, kc * 128:(kc + 1) * 128], ident[:]),
                              [("xin", j), "ident"], [("ps", pb)], inc=(j == 3))
                        if kc % 2 == 0:
                            V(lambda kc=kc, pb=pb, tg=tg: nc.vector.tensor_copy(out=hT[:, kc, tg * 512:(tg + 1) * 512], in_=ps[pb][:]),
                              [("ps", pb)], [("hT", kc)])
                        else:
                            A(lambda kc=kc, pb=pb, tg=tg: nc.scalar.copy(out=hT[:, kc, tg * 512:(tg + 1) * 512], in_=ps[pb][:]),
                              [("ps", pb)], [("hT", kc)])
                S.barrier()

            if not LITE:
                ffn(wi_d, wo_d, g_ffn, "g_ffn")

            with contextlib.ExitStack() as ph:
                w_in = sb("w_in", [128, KC, 2064], BF16, stack=ph)
                if "wcast" not in KSKIP:
                    load_wcast(w_in, w_in_d, 2064, "w_in")
                gw32 = sb("gw32", [17, 256], stack=ph)
                gw = sb("gw", [17, 256], BF16, stack=ph)
                S.dma("sp", gw32[:], gw_d[:, :], writes=["gw32"])
                A(lambda: nc.scalar.copy(out=gw[:], in_=gw32[:]), ["gw32"], ["gw"])
                poolw = sb("poolw", [128, 4, 128], BF16, stack=ph)
                if "poolw" not in KSKIP:
                    cast_load(poolw[:], poolw_d.rearrange("g c d -> c g d"), lambda t: t[:, 0:512].rearrange("p (g d) -> p g d", g=4), "poolw")
                pscale = sb("pscale", [128, 4], stack=ph)
                if "pscale" not in KSKIP:
                    with nc.allow_non_contiguous_dma(reason="tiny"):
                        S.dma("sp", pscale[:], pscale_d.rearrange("o (g p) -> p (o g)", p=128), writes=["pscale"])
                tin32 = sb("tin32", [64, 64], stack=ph)
                uex32 = sb("uex32", [64, 64], stack=ph)
                tin = sb("tin", [64, 64], BF16, stack=ph)
                uex = sb("uex", [64, 64], BF16, stack=ph)
                cmask = sb("cmask", [64, 256], stack=ph)
                S.dma("sp", tin32[:], tin_d[:, :], writes=["tin32"])
                S.dma("sp", uex32[:], uex_d[:, :], writes=["uex32"])
                S.dma("sp", cmask[:], cmask_d[:, :], writes=["cmask"])
                A(lambda: nc.scalar.copy(out=tin[:], in_=tin32[:]), ["tin32"], ["tin"])
                A(lambda: nc.scalar.copy(out=uex[:], in_=uex32[:]), ["uex32"], ["uex"])
                o_all = sb("o_grp", [128, 4, 512], stack=ph)
                qe = sb("qe_grp", [64, 4, 512], BF16, stack=ph)
                sgT = sb("sg_grp", [128, 4, 512], BF16, stack=ph)
                pp = sb("pp_grp", [128, 4, 512], BF16, stack=ph)
                BL = sb("BL", [64, 4, 32], stack=ph)
                u16 = sb("u16", [128, 4, 16], stack=ph)
                Sst = sb("Sst", [64, 4, 128], stack=ph)
                Sbf = sb("Sbf", [64, 4, 128], BF16, stack=ph)
                hnT = sb("hnT", [128, KC, 512], BF16, stack=ph)
                sq = sb("sq", [128, 2, 512], BF16, stack=ph)
                rstd = sb("rstd", [128, 512], stack=ph)
                a1 = sb("a1", [17, 512], BF16, stack=ph)
                tmpz = sb("tmpz", [64, 256], stack=ph)
                sp32 = sb("sp32", [64, 256], stack=ph)
                sp = sb("sp_hi", [64, 8, 256], BF16, stack=ph)
                spl = sb("sp_lo", [64, 8, 256], BF16, stack=ph)
                eb = sb("eb", [64, 4, 512], stack=ph)
                enb = sb("enb", [64, 4, 512], stack=ph)
                ke = sb("ke", [64, 4, 512], BF16, stack=ph)
                er = sb("er", [64, 256], stack=ph)
                kd = sb("kd", [64, 8, 256], BF16, stack=ph)
                vt = sb("vt", [64, 8, 512], BF16, stack=ph)
                uT1 = sb("uT", [128, 4, 528], stack=ph)
                uT = [uT1, uT1]
                sw = [sb("sw%d" % i, [128, 528], stack=ph) for i in range(2)]
                pT = sb("pT", [128, 512], BF16, stack=ph)
                sTm = sb("sTm", [64, 256], BF16, stack=ph)

                V(lambda: nc.vector.memset(a1[:], 1.0), [], ["a1"])
                V(lambda: nc.vector.memset(Sst[:], 0.0), [], ["Sst"])
                V(lambda: nc.vector.memset(Sbf[:], 0.0), [], ["Sbf"])
                V(lambda: nc.vector.memset(uT1[:, :, 0:16], 0.0), [], [("uT", 0)])

                def proj_fm(pb, c0, m, rows=128):
                    for kc in range(KC):
                        T(lambda kc=kc: nc.tensor.matmul(ps[pb][0:m, :], lhsT=w_in[:, kc, c0:c0 + m], rhs=hnT[:, kc, :],
                                                         start=(kc == 0), stop=(kc == KC - 1)),
                          ["w_in", "hnT"], [("ps", pb)], inc=(kc == KC - 1))

                for tg in range(4 if KSTOP >= 2 else 0):
                    cols = slice(tg * 512, (tg + 1) * 512)
                    lc = slice(0, 512)
                    ub = 0
                    norm_h(cols, g_mix, "g_mix", sq, rstd, lambda kc: hnT[:, kc, :], "hnT")
                    if "alr" not in KSKIP:
                        proj_fm(0, 1536, 16)
                        V(lambda: nc.vector.tensor_copy(out=a1[0:16, :], in_=ps[0][0:16, :]), [("ps", 0)], ["a1"])
                    for ch in range(8 if "z" not in KSKIP else 0):
                        T(lambda ch=ch: nc.tensor.matmul(ps[2][0:64, 0:256], lhsT=a1[0:17, ch * 64:(ch + 1) * 64], rhs=gw[0:17, :], start=True, stop=True),
                          ["a1", "gw"], [("ps", 2)])
                        if "zact" in KSKIP:
                            continue
                        A(lambda: nc.scalar.activation(out=tmpz[:], in_=ps[2][0:64, 0:256], func=AF.Exp, scale=-1.0), [("ps", 2)], ["tmpz"])
                        if "zln" in KSKIP:
                            continue
                        A(lambda ch=ch: nc.scalar.activation(out=sp[:, ch, :], in_=tmpz[:], func=AF.Ln, bias=one_c[0:64, 0:1]), ["tmpz", "one"], [("sp", ch)])
                    for h in range(4 if "bT" not in KSKIP else 0):
                        for ch in range(8):
                            T(lambda ch=ch, h=h: nc.tensor.matmul(ps[3][0:64, ch * 64:(ch + 1) * 64], lhsT=sp[:, ch, h * 64:(h + 1) * 64], rhs=tin[:, :],
                                                                  start=True, stop=True),
                              [("sp", ch), "tin"], [("ps", 3)], inc=(ch == 7))
                        A(lambda h=h: nc.scalar.activation(out=eb[:, h, :], in_=ps[3][0:64, :], func=AF.Exp), [("ps", 3)], [("eb", h)])
                        A(lambda h=h: nc.scalar.activation(out=enb[:, h, :], in_=ps[3][0:64, :], func=AF.Exp, scale=-1.0), [("ps", 3)], [("enb", h)])
                        V(lambda h=h, tg=tg: nc.vector.tensor_copy(out=BL[:, h, tg * 8:(tg + 1) * 8], in_=ps[3][0:64, 63:512:64]), [("ps", 3)], ["BL"])
                    for h in range(4 if KSTOP >= 3 else 0):
                        proj_fm(0, h * 64, 64)
                        V(lambda h=h: nc.vector.scalar_tensor_tensor(out=qe[:, h, :], in0=ps[0][0:64, :], scalar=0.125, in1=eb[:, h, :],
                                                                                op0=ALU.mult, op1=ALU.mult),
                          [("ps", 0), ("eb", h)], ["qe"])
                        proj_fm(1, 256 + h * 64, 64)
                        V(lambda h=h: nc.vector.tensor_tensor(out=ke[:, h, :], in0=ps[1][0:64, :], in1=enb[:, h, :], op=ALU.mult),
                          [("ps", 1), ("enb", h)], ["ke"])
                    for ch in range(8 if KSTOP >= 3 else 0):
                        T(lambda ch=ch: nc.tensor.matmul(ps[2][0:64, 0:256], lhsT=uex[:, :], rhs=sp[:, ch, :], start=True, stop=True),
                          [("sp", ch), "uex"], [("ps", 2)])
                        A(lambda: nc.scalar.activation(out=er[:], in_=ps[2][0:64, 0:256], func=AF.Exp), [("ps", 2)], ["er"])
                        for kc in range(KC):
                            T(lambda kc=kc, ch=ch: nc.tensor.matmul(ps[0][0:64, 0:256], lhsT=hnT[:, kc, ch * 64:(ch + 1) * 64], rhs=w_in[:, kc, 256:512],
                                                                    start=(kc == 0), stop=(kc == KC - 1)),
                              ["w_in", "hnT"], [("ps", 0)], inc=(kc == KC - 1))
                        V(lambda ch=ch: nc.vector.tensor_tensor(out=kd[:, ch, :], in0=ps[0][0:64, 0:256], in1=er[:], op=ALU.mult),
                          [("ps", 0), "er"], [("kd", ch)])
                        for kc in range(KC):
                            T(lambda kc=kc, ch=ch: nc.tensor.matmul(ps[1][0:64, :], lhsT=hnT[:, kc, ch * 64:(ch + 1) * 64], rhs=w_in[:, kc, 512:1024],
                                                                    start=(kc == 0), stop=(kc == KC - 1)),
                              ["w_in", "hnT"], [("ps", 1)], inc=(kc == KC - 1))
                        A(lambda ch=ch: nc.scalar.copy(out=vt[:, ch, :], in_=ps[1][0:64, :]), [("ps", 1)], [("vt", ch)])
                    for h in range(4 if KSTOP >= 4 else 0):
                        proj_fm(h % 2, 1024 + h * 128, 128)
                        A(lambda h=h: nc.scalar.activation(out=sgT[:, h, :], in_=ps[h % 2][:], func=AF.Silu), [("ps", h % 2)], ["sgT"])
                    for g in range(4 if KSTOP >= 4 else 0):
                        proj_fm(g % 2, 1552 + g * 128, 128)
                        V(lambda g=g, ub=ub: nc.vector.tensor_copy(out=uT[ub][:, g, 16:528], in_=ps[g % 2][:]), [("ps", g % 2)], [("uT", ub)])
                    if tg == 0:
                        V(lambda: nc.vector.tensor_copy(out=u16[:], in_=uT[0][:, :, 16:32]), [("uT", 0)], ["u16"])
                    for ch in range(8 if KSTOP >= 5 else 0):
                        cg = tg * 8 + ch
                        ccol = slice(ch * 64, (ch + 1) * 64)
                        gcol = ccol
                        for h in range(4):
                            T(lambda h=h, ccol=ccol, gcol=gcol: nc.tensor.matmul(ps[4][0:64, h * 64:(h + 1) * 64], lhsT=ke[:, h, ccol], rhs=qe[:, h, gcol],
                                                                                start=True, stop=True),
                              ["ke", "qe"], [("ps", 4)], inc=(h == 3))
                        V(lambda: nc.vector.tensor_tensor(out=sTm[:], in0=ps[4][0:64, 0:256], in1=cmask[:], op=ALU.mult), [("ps", 4), "cmask"], ["sTm"])
                        ob = 5 if (ch // 2) % 2 == 0 else 6
                        for h in range(4):
                            oc = slice(h * 128 + (ch % 2) * 64, h * 128 + (ch % 2) * 64 + 64)
                            T(lambda h=h, ch=ch, oc=oc, ob=ob: nc.tensor.matmul(ps[ob][:, oc], lhsT=vt[:, ch, h * 128:(h + 1) * 128], rhs=sTm[:, h * 64:(h + 1) * 64],
                                                                               start=True, stop=False),
                              [("vt", ch), "sTm"], [("ps", ob)], inc=False)
                            T(lambda h=h, gcol=gcol, oc=oc, ob=ob: nc.tensor.matmul(ps[ob][:, oc], lhsT=Sbf[:, h, :], rhs=qe[:, h, gcol], start=False, stop=True),
                              ["Sbf", "qe"], [("ps", ob)], inc=(h == 3))
                        for h in range(4):
                            T(lambda h=h, ch=ch: nc.tensor.matmul(ps[3][0:64, h * 128:(h + 1) * 128], lhsT=kd[:, ch, h * 64:(h + 1) * 64], rhs=vt[:, ch, h * 128:(h + 1) * 128],
                                                                  start=True, stop=True),
                              [("kd", ch), ("vt", ch)], [("ps", 3)], inc=(h == 3))
                        for h in range(4):
                            V(lambda h=h, ch=ch: nc.vector.scalar_tensor_tensor(out=Sst[:, h, :], in0=Sst[:, h, :], scalar=eb[:, h, ch * 64 + 63:ch * 64 + 64],
                                                                               in1=ps[3][0:64, h * 128:(h + 1) * 128], op0=ALU.mult, op1=ALU.add),
                              ["Sst", ("eb", h), ("ps", 3)], ["Sst"])
                        A(lambda: nc.scalar.copy(out=Sbf[:], in_=Sst[:]), ["Sst"], ["Sbf"])
                        if ch % 2 == 1:
                            t0 = (ch // 2) * 128
                            A(lambda ob=ob, t0=t0: nc.scalar.copy(out=o_all[:, :, t0:t0 + 128], in_=ps[ob][:].rearrange("p (h t) -> p h t", h=4)),
                              [("ps", ob)], ["o_all"])
                    for g in range(4 if KSTOP >= 6 else 0):
                        w = 2 ** (g + 1)
                        src = uT[ub][:, g, :]
                        lo = 0
                        step = 1
                        k = 0
                        while step < w:
                            lo += step
                            dst = sw[k % 2]
                            G(lambda src=src, dst=dst, lo=lo, step=step: nc.gpsimd.tensor_tensor(out=dst[:, lo:528], in0=src[:, lo:528], in1=src[:, lo - step:528 - step], op=ALU.add),
                              [("uT", ub), ("sw", 0), ("sw", 1)], [("sw", k % 2)])
                            src = dst
                            step *= 2
                            k += 1
                        V(lambda src=src, g=g, ub=ub, w=w: nc.vector.scalar_tensor_tensor(out=pT[:], in0=src[:, 16:528], scalar=1.0 / w, in1=uT[ub][:, g, 16:528],
                                                                                         op0=ALU.mult, op1=ALU.subtract),
                          [("sw", 0), ("sw", 1), ("uT", ub)], ["pT"])
                        T(lambda g=g: nc.tensor.matmul(ps[g % 2][:], lhsT=poolw[:, g, :], rhs=pT[:], start=True, stop=True), ["poolw", "pT"], [("ps", g % 2)])
                        V(lambda g=g: nc.vector.tensor_scalar(out=pp[:, g, :], in0=ps[g % 2][:], scalar1=pscale[:, g:g + 1], scalar2=None, op0=ALU.mult),
                          [("ps", g % 2), "pscale"], ["pp"])
                    if KSTOP < 7:
                        continue
                    S.dma("sp", o_o.rearrange("p (h t) -> p h t", h=4)[:, :, cols], o_all[:], reads=["o_all"], writes=[("d_o", tg)])
                    S.dma("sp", o_qe.rearrange("p (h t) -> p h t", h=4)[:, :, cols], qe[:], reads=["qe"], writes=[("d_qe", tg)])
                    S.dma("sp", o_sg.rearrange("p (h t) -> p h t", h=4)[:, :, cols], sgT[:], reads=["sgT"], writes=[("d_sg", tg)])
                    S.dma("sp", o_pp.rearrange("p (h t) -> p h t", h=4)[:, :, cols], pp[:], reads=["pp"], writes=[("d_pp", tg)])
                    if tg == 3:
                        S.dma("sp", o_utail.rearrange("p (g t) -> p g t", g=4), uT1[:, :, 512:528], reads=[("uT", 0)], writes=["d_ut"])
                    else:
                        V(lambda: nc.vector.tensor_copy(out=uT1[:, :, 0:16], in_=uT1[:, :, 512:528]), [("uT", 0)], [("uT", 0)])
                S.barrier()
                if not FUSED:
                    dump3(o_hT, hT, KC, lambda k: [("hT", k)])
                if "dsmall" not in KSKIP:
                    S.dma("sp", o_bl[:, :], BL[:].rearrange("p h t -> p (h t)"), reads=["BL"])
                    S.dma("sp", o_u16[:, :], u16[:].rearrange("p h t -> p (h t)"), reads=["u16"])
                    S.dma("sp", o_send[:, :], Sst[:].rearrange("p h t -> p (h t)"), reads=["Sst"])
                S.barrier()

        if "B" in parts:
            CUR[0] = "B"
            if FUSED:
                S.collective("cc1", lambda: nc.gpsimd.collective_compute("AllGather", ALU.bypass, replica_groups=PAIRS, ins=[xa_src[:, :]], outs=[xa_dst[:, :]]))
                flag_d = din("c_flag", [128, 1])
            i_hT = din("st_hT", [128, KC * NTOK])
            i_o = din("st_o", [128, 4 * NTOK])
            i_qe = din("st_qe", [64, 4 * NTOK], BF16)
            i_sg = din("st_sg", [128, 4 * NTOK], BF16)
            i_pp = din("st_pp", [128, 4 * NTOK], BF16)
            i_bl = din("st_bl", [64, 128])
            i_u16 = din("st_u16", [128, 64])
            i_sin = din("x_sin", [64, 512])
            i_halo = din("x_halo", [128, 64])
            i_invc = din("c_invcnt", [128, 64])
            gon_d = din("gla_out_norm", [1, 512])
            poolw_d = din("pool_w", [4, 128, 128])
            pscale_d = din("pool_scale", [1, 512])
            wout_d = din("even_w_out", [D, D])
            g_f2 = load_gain(din("g_ffn2_0", [1, D]), "g_f2")
            g_f1 = load_gain(din("g_ffn1_1", [1, D]), "g_f1")
            g_mix = load_gain(din("g_mix1", [1, D]), "g_mix")
            wi2_d = din("ffn2_wi", [D, 2 * DFF])
            wo2_d = din("ffn2_wo", [DFF, D])
            wi1_d = din("ffn1_wi", [D, 2 * DFF])
            wo1_d = din("ffn1_wo", [DFF, D])
            w_in_d = din("odd_w_in", [D, 1860])
            w_rot_d = din("odd_w_rot", [D, 1600])
            pos_d = din("positions", [1, NTOK], I32)
            ropec_d = din("c_rope", [128, 4])
            o_hT = dout("st_hT_o", [128, KC * NTOK])
            o_q = dout("st_q", [128, 8 * NTOK], BF16)
            o_k = dout("st_k", [128, 2 * NTOK], BF16)
            o_qi = dout("st_qi", [64, 4 * NTOK], BF16)
            o_ki = dout("st_ki", [64, NTOK], BF16)
            o_v = dout("st_v", [128, 16 * 260], BF16)
            ov_parts = xv_src if FUSED else [o_v[:, 0:2080], o_v[:, 2080:4160]]
            o_wi = dout("st_wi", [128, 16 * 8])

            if not FUSED:
                load3(hT, i_hT, KC, lambda k: [("hT", k)])
            with contextlib.ExitStack() as ph:
                o_all = sb("o_all", [128, 4, NTOK], stack=ph)
                qe = sb("qe", [64, 4, NTOK], BF16, stack=ph)
                sgT = sb("sgT", [128, 4, NTOK], BF16, stack=ph)
                pp = sb("pp", [128, 4, NTOK], BF16, stack=ph)
                BL = sb("BL", [64, 4, 32], stack=ph)
                Sin_ = sb("Sin", [64, 4, 128], stack=ph)
                ue = sb("ue", [128, 4, 32], stack=ph)
                invc = sb("invc", [128, 4, 16], stack=ph)
                load3(o_all, i_o, 4, lambda k: ["o_all"])
                load3(qe, i_qe, 4, lambda k: ["qe"])
                load3(sgT, i_sg, 4, lambda k: ["sgT"])
                load3(pp, i_pp, 4, lambda k: ["pp"])
                S.dma("sp", BL[:].rearrange("p h t -> p (h t)"), i_bl[:, :], writes=["BL"])
                S.dma("sp", Sin_[:].rearrange("p h t -> p (h t)"), i_sin[:, :], writes=["Sin"])
                S.dma("sp", ue[:, :, 0:16], i_halo.rearrange("p (g t) -> p g t", g=4), writes=["ue"])
                S.dma("sp", ue[:, :, 16:32], i_u16.rearrange("p (g t) -> p g t", g=4), writes=["ue"])
                if FUSED:
                    flag = sb("flag", [128, 1], stack=ph)
                    S.dma("sp", flag[:], flag_d[:, :], writes=["flag"])
                    V(lambda: nc.vector.tensor_scalar(out=Sin_[:], in0=Sin_[:], scalar1=flag[0:64, 0:1], scalar2=None, op0=ALU.mult), ["Sin", "flag"], ["Sin"])
                    V(lambda: nc.vector.tensor_scalar(out=ue[:, :, 0:16], in0=ue[:, :, 0:16], scalar1=flag[:, 0:1], scalar2=None, op0=ALU.mult), ["ue", "flag"], ["ue"])
                S.dma("sp", invc[:].rearrange("p g t -> p (g t)"), i_invc[:, :], writes=["invc"])
                gon = sb("gon", [128, 4], stack=ph)
                pscale = sb("pscale", [128, 4], stack=ph)
                with nc.allow_non_contiguous_dma(reason="tiny"):
                    S.dma("sp", gon[:], gon_d.rearrange("o (g p) -> p (o g)", p=128), writes=["gon"])
                    S.dma("sp", pscale[:], pscale_d.rearrange("o (g p) -> p (o g)", p=128), writes=["pscale"])
                poolw = sb("poolw", [128, 4, 128], BF16, stack=ph)
                cast_load(poolw[:], poolw_d.rearrange("g c d -> c g d"), lambda t: t[:, 0:512].rearrange("p (g d) -> p g d", g=4), "poolw")
                w_out = sb("w_out", [128, KC, D], BF16, stack=ph)
                load_wcast(w_out, wout_d, D, "w_out")
                zer = sb("zer", [64, 32], stack=ph)
                Ein = sb("Ein", [64, 4, 32], stack=ph)
                E = sb("E", [64, 4, 32], stack=ph)
                Spb = [sb("Spb%d" % i, [64, 128], BF16, stack=ph) for i in range(4)]
                sq = sb("sq", [128, 2, 512], BF16, stack=ph)
                rstd = sb("rstd", [128, 512], stack=ph)
                tmpo = sb("tmpo", [128, 512], stack=ph)
                sw = [sb("sw%d" % i, [128, 32], stack=ph) for i in range(2)]
                p16 = sb("p16", [128, 16], stack=ph)
                p16b = sb("p16b", [128, 16], BF16, stack=ph)

                V(lambda: nc.vector.memset(zer[:], 0.0), [], ["zer"])
                for h in range(4):
                    V(lambda h=h: nc.vector.tensor_tensor_scan(out=Ein[:, h, :], data0=BL[:, h, :], data1=zer[:], initial=0.0, op0=ALU.add, op1=ALU.add),
                      ["BL", "zer"], ["Ein"])
                V(lambda: nc.vector.tensor_tensor(out=Ein[:], in0=Ein[:], in1=BL[:], op=ALU.subtract), ["Ein", "BL"], ["Ein"])
                A(lambda: nc.scalar.activation(out=E[:], in_=Ein[:], func=AF.Exp), ["Ein"], ["E"])
                kk = 0
                for tg in range(4):
                    cols = slice(tg * 512, (tg + 1) * 512)
                    for h in range(4):
                        pb = h % 2
                        for ch in range(8):
                            cg = tg * 8 + ch
                            gcol = slice(tg * 512 + ch * 64, tg * 512 + (ch + 1) * 64)
                            sbi = kk % 4
                            kk += 1
                            V(lambda h=h, cg=cg, sbi=sbi: nc.vector.tensor_scalar(out=Spb[sbi][:], in0=Sin_[:, h, :], scalar1=E[:, h, cg:cg + 1], scalar2=None, op0=ALU.mult),
                              ["Sin", "E"], [("Spb", sbi)])
                            T(lambda h=h, ch=ch, gcol=gcol, sbi=sbi, pb=pb: nc.tensor.matmul(ps[pb][:, ch * 64:(ch + 1) * 64], lhsT=Spb[sbi][:], rhs=qe[:, h, gcol], start=True, stop=True),
                              [("Spb", sbi), "qe"], [("ps", pb)])
                        V(lambda h=h, cols=cols, pb=pb: nc.vector.tensor_tensor(out=o_all[:, h, cols], in0=ps[pb][:], in1=o_all[:, h, cols], op=ALU.add),
                          [("ps", pb), "o_all"], ["o_all"])
                        A(lambda h=h, cols=cols: nc.scalar.activation(out=sq[:, 0, :], in_=o_all[:, h, cols], func=AF.Square), ["o_all"], [("sq", 0)])
                        T(lambda: nc.tensor.matmul(ps[6][:], lhsT=ones_bf[:], rhs=sq[:, 0, :], start=True, stop=True), [("sq", 0), "ones_bf"], [("ps", 6)])
                        rstd_from_ps(rstd[:], 6, 512, 128)
                        V(lambda h=h, cols=cols: nc.vector.scalar_tensor_tensor(out=tmpo[:], in0=o_all[:, h, cols], scalar=gon[:, h:h + 1], in1=rstd[:], op0=ALU.mult, op1=ALU.mult),
                          ["o_all", "gon", "rstd"], ["tmpo"])
                        V(lambda h=h, cols=cols: nc.vector.tensor_tensor(out=sgT[:, h, cols], in0=tmpo[:], in1=sgT[:, h, cols], op=ALU.mult),
                          ["tmpo", "sgT"], ["sgT"])
                for g in range(4):
                    w = 2 ** (g + 1)
                    src = ue[:, g, :]
                    lo, step, k = 0, 1, 0
                    while step < w:
                        lo += step
                        dst = sw[k % 2]
                        G(lambda src=src, dst=dst, lo=lo, step=step: nc.gpsimd.tensor_tensor(out=dst[:, lo:32], in0=src[:, lo:32], in1=src[:, lo - step:32 - step], op=ALU.add),
                          ["ue", ("sw", 0), ("sw", 1)], [("sw", k % 2)])
                        src = dst
                        step *= 2
                        k += 1
                    V(lambda src=src, g=g: nc.vector.tensor_tensor(out=p16[:], in0=src[:, 16:32], in1=invc[:, g, :], op=ALU.mult), [("sw", 0), ("sw", 1), "invc"], ["p16"])
                    V(lambda g=g: nc.vector.tensor_tensor(out=p16b[:], in0=p16[:], in1=ue[:, g, 16:32], op=ALU.subtract), ["p16", "ue"], ["p16b"])
                    T(lambda g=g: nc.tensor.matmul(ps[g % 2][:, 0:16], lhsT=poolw[:, g, :], rhs=p16b[:], start=True, stop=True), ["poolw", "p16b"], [("ps", g % 2)])
                    V(lambda g=g: nc.vector.tensor_scalar(out=pp[:, g, 0:16], in0=ps[g % 2][:, 0:16], scalar1=pscale[:, g:g + 1], scalar2=None, op0=ALU.mult),
                      [("ps", g % 2), "pscale"], ["pp"])
                out_proj(w_out, "w_out", lambda k, cols: (sgT[:, k, cols] if k < 4 else pp[:, k - 4, cols]), lambda k, tg: ["sgT", "pp"])
                S.barrier()

            ffn(wi2_d, wo2_d, g_f2, "g_f2")
            ffn(wi1_d, wo1_d, g_f1, "g_f1")

            with contextlib.ExitStack() as ph:
                w_in = sb("w_in", [128, KC, 1860], BF16, stack=ph)
                load_wcast(w_in, w_in_d, 1860, "w_in")
                w_rot = sb("w_rot", [128, KC, 1600], BF16, stack=ph)
                load_wcast(w_rot, w_rot_d, 1600, "w_rot")
                ropec = sb("ropec", [128, 4], stack=ph)
                S.dma("sp", ropec[:], ropec_d[:, :], writes=["ropec"])
                hnT = sb("hnT", [128, KC, 512], BF16, stack=ph)
                sq = sb("sq", [128, 2, 512], BF16, stack=ph)
                rstd = sb("rstd", [128, 512], stack=ph)
                posi = sb("posi", [128, 512], I32, stack=ph)
                posf = sb("posf", [128, 512], stack=ph)
                ang = sb("ang", [128, 512], stack=ph)
                ang2 = sb("ang2", [128, 512], stack=ph)
                ni = sb("ni", [128, 512], I32, stack=ph)
                nf = sb("nf", [128, 512], stack=ph)
                tabs = {nm: sb(nm, [128, 512], stack=ph) for nm in ("CS128", "SN128", "CS64", "SN64")}
                t1 = sb("t1", [128, 512], stack=ph)
                t2 = sb("t2", [128, 512], stack=ph)
                qbuf = sb("qbuf", [128, 8, 512], BF16, stack=ph)
                kbuf = sb("kbuf", [128, 2, 512], BF16, stack=ph)
                qibuf = sb("qibuf", [64, 4, 512], BF16, stack=ph)
                kibuf = sb("kibuf", [64, 512], BF16, stack=ph)
                vbuf = sb("vbuf", [128, 4, 2, 130], BF16, stack=ph)
                wibuf = sb("wibuf", [128, 4, 8], stack=ph)
                V(lambda: nc.vector.memset(vbuf[:, :, :, 128:129], 1.0), [], ["vbuf"])
                V(lambda: nc.vector.memset(vbuf[:, :, :, 129:130], 0.0), [], ["vbuf"])

                def sin_table(src_ang, out_tab, np_, sgn_col=None):
                    V(lambda: nc.vector.tensor_scalar(out=ni[0:np_, :], in0=src_ang[0:np_, :], scalar1=1.0 / TWO_PI, scalar2=None, op0=ALU.mult), ["ang"], ["ni"])
                    V(lambda: nc.vector.tensor_copy(out=nf[0:np_, :], in_=ni[0:np_, :]), ["ni"], ["nf"])
                    V(lambda: nc.vector.scalar_tensor_tensor(out=t1[0:np_, :], in0=nf[0:np_, :], scalar=-CW1, in1=src_ang[0:np_, :], op0=ALU.mult, op1=ALU.add), ["nf", "ang"], ["t1"])
                    V(lambda: nc.vector.scalar_tensor_tensor(out=t1[0:np_, :], in0=nf[0:np_, :], scalar=-CW2, in1=t1[0:np_, :], op0=ALU.mult, op1=ALU.add), ["nf", "t1"], ["t1"])
                    V(lambda: nc.vector.tensor_scalar(out=t1[0:np_, :], in0=t1[0:np_, :], scalar1=float(np.pi), scalar2=-float(np.pi), op0=ALU.min, op1=ALU.max), ["t1"], ["t1"])
                    A(lambda: nc.scalar.activation(out=out_tab[0:np_, :], in_=t1[0:np_, :], func=AF.Sin), ["t1"], ["tab"])
                    if sgn_col is not None:
                        V(lambda: nc.vector.tensor_scalar(out=out_tab[0:np_, :], in0=out_tab[0:np_, :], scalar1=sgn_col, scalar2=None, op0=ALU.mult), ["tab", "ropec"], ["tab"])

                def rope_proj(c0, r0, m, cs, sn, dst, dkey, pi):
                    pa, pbk = (0, 1) if pi % 2 == 0 else (2, 3)
                    for kc in range(KC):
                        T(lambda kc=kc: nc.tensor.matmul(ps[pa][0:m, :], lhsT=w_in[:, kc, c0:c0 + m], rhs=hnT[:, kc, :], start=(kc == 0), stop=(kc == KC - 1)),
                          ["w_in", "hnT"], [("ps", pa)], inc=(kc == KC - 1))
                    for kc in range(KC):
                        T(lambda kc=kc: nc.tensor.matmul(ps[pbk][0:m, :], lhsT=w_rot[:, kc, r0:r0 + m], rhs=hnT[:, kc, :], start=(kc == 0), stop=(kc == KC - 1)),
                          ["w_rot", "hnT"], [("ps", pbk)], inc=(kc == KC - 1))
                    V(lambda: nc.vector.tensor_tensor(out=t1[0:m, :], in0=ps[pa][0:m, :], in1=cs[0:m, :], op=ALU.mult), [("ps", pa), "tab"], ["t1"])
                    V(lambda: nc.vector.tensor_tensor(out=t2[0:m, :], in0=ps[pbk][0:m, :], in1=sn[0:m, :], op=ALU.mult), [("ps", pbk), "tab"], ["t2"])
                    G(lambda: nc.gpsimd.tensor_tensor(out=dst, in0=t1[0:m, :], in1=t2[0:m, :], op=ALU.add), ["t1", "t2"], [dkey])

                for tg in range(4):
                    cols = slice(tg * 512, (tg + 1) * 512)
                    norm_h(cols, g_mix, "g_mix", sq, rstd, lambda kc: hnT[:, kc, :], "hnT")
                    S.dma("sp", posi[:], pos_d[0:1, cols].to_broadcast([128, 512]), writes=["posi"])
                    V(lambda: nc.vector.tensor_copy(out=posf[:], in_=posi[:]), ["posi"], ["posf"])
                    for (inv_c, sgn_c, np_, csn, snn) in ((0, 1, 128, "CS128", "SN128"), (2, 3, 64, "CS64", "SN64")):
                        V(lambda inv_c=inv_c, np_=np_: nc.vector.tensor_scalar(out=ang[0:np_, :], in0=posf[0:np_, :], scalar1=ropec[0:np_, inv_c:inv_c + 1], scalar2=None, op0=ALU.mult),
                          ["posf", "ropec"], ["ang"])
                        sin_table(ang, tabs[snn], np_, ropec[0:np_, sgn_c:sgn_c + 1])
                        V(lambda np_=np_: nc.vector.tensor_scalar(out=ang2[0:np_, :], in0=ang[0:np_, :], scalar1=float(np.pi / 2), scalar2=None, op0=ALU.add), ["ang"], ["ang"])
                        sin_table(ang2, tabs[csn], np_, None)
                    pi = 0
                    for h in range(8):
                        rope_proj(h * 128, h * 128, 128, tabs["CS128"], tabs["SN128"], qbuf[:, h, :], "qbuf", pi)
                        pi += 1
                    for g in range(2):
                        rope_proj(1024 + g * 128, 1024 + g * 128, 128, tabs["CS128"], tabs["SN128"], kbuf[:, g, :], "kbuf", pi)
                        pi += 1
                    for h in range(4):
                        rope_proj(1536 + h * 64, 1280 + h * 64, 64, tabs["CS64"], tabs["SN64"], qibuf[:, h, :], "qibuf", pi)
                        pi += 1
                    rope_proj(1792, 1536, 64, tabs["CS64"], tabs["SN64"], kibuf[:, :], "kibuf", pi)
                    for j in range(4):
                        tcol = slice(j * 128, (j + 1) * 128)
                        for kc in range(KC):
                            T(lambda kc=kc, tcol=tcol: nc.tensor.matmul(ps[4][:, 0:256], lhsT=hnT[:, kc, tcol], rhs=w_in[:, kc, 1280:1536], start=(kc == 0), stop=(kc == KC - 1)),
                              ["w_in", "hnT"], [("ps", 4)], inc=(kc == KC - 1))
                        A(lambda j=j: nc.scalar.copy(out=vbuf[:, j, :, 0:128], in_=ps[4][:, 0:256].rearrange("p (g d) -> p g d", g=2)), [("ps", 4)], ["vbuf"])
                        for kc in range(KC):
                            T(lambda kc=kc, tcol=tcol: nc.tensor.matmul(ps[5][:, 0:4], lhsT=hnT[:, kc, tcol], rhs=w_in[:, kc, 1856:1860], start=(kc == 0), stop=(kc == KC - 1)),
                              ["w_in", "hnT"], [("ps", 5)], inc=(kc == KC - 1))
                        A(lambda j=j: nc.scalar.activation(out=wibuf[:, j, 0:4], in_=ps[5][:, 0:4], func=AF.Abs), [("ps", 5)], ["wibuf"])
                        A(lambda j=j: nc.scalar.activation(out=wibuf[:, j, 4:8], in_=ps[5][:, 0:4], func=AF.Sign), [("ps", 5)], ["wibuf"])
                    S.dma("sp", o_q.rearrange("p (h t) -> p h t", h=8)[:, :, cols], qbuf[:], reads=["qbuf"])
                    S.dma("sp", o_k.rearrange("p (h t) -> p h t", h=2)[:, :, cols], kbuf[:], reads=["kbuf"])
                    S.dma("sp", o_qi.rearrange("p (h t) -> p h t", h=4)[:, :, cols], qibuf[:], reads=["qibuf"])
                    S.dma("sp", o_ki[:, cols], kibuf[:], reads=["kibuf"])
                    S.dma("sp", ov_parts[tg // 2][:, (tg % 2) * 1040:(tg % 2 + 1) * 1040], vbuf[:].rearrange("p j g d -> p (j g d)"), reads=["vbuf"])
                    S.dma("sp", o_wi[:, tg * 32:(tg + 1) * 32], wibuf[:].rearrange("p j c -> p (j c)"), reads=["wibuf"])
                S.barrier()
            if not FUSED:
                dump3(o_hT, hT, KC, lambda k: [("hT", k)])
            S.barrier()

        if "C" in parts:
            CUR[0] = "C"
            if FUSED:
                for (cn, csrc, cdst) in XCH2:
                    S.collective(cn, lambda csrc=csrc, cdst=cdst: nc.gpsimd.collective_compute("AllGather", ALU.bypass, replica_groups=PAIRS, ins=[csrc[:, :]], outs=[cdst[:, :]]))
            i_hT = din("st_hT", [128, KC * NTOK])
            i_q = din("st_q", [128, 8 * NTOK], BF16)
            i_k = din("st_k", [128, 2 * NTOK], BF16)
            i_qi = din("st_qi", [64, 4 * NTOK], BF16)
            i_ki = din("st_ki", [64, NTOK], BF16)
            i_v = None if FUSED else din("st_v", [128, 16 * 260], BF16)
            i_wi = din("st_wi", [128, 16 * 8])
            x_k = din("x_k", [128, 2 * NTOK], BF16)
            x_ki = din("x_ki", [64, NTOK], BF16)
            x_v = None if FUSED else din("x_v", [128, 16 * 260], BF16)
            pb_d = din("c_pbias", [128, 1])
            caus_d = din("c_caus", [128, 128])
            wout_d = din("odd_w_out", [D, D])
            g_f2 = load_gain(din("g_ffn2_1", [1, D]), "g_f2")
            g_fin = load_gain(din("g_final", [1, D]), "g_fin")
            wi2_d = din("ffn2_wi", [D, 2 * DFF])
            wo2_d = din("ffn2_wo", [DFF, D])
            y_d = dout("y", [NTOK, D])

            if not FUSED:
                load3(hT, i_hT, KC, lambda k: [("hT", k)])
            with contextlib.ExitStack() as pq:
                qT = sb("qT", [128, 8, NTOK], BF16, stack=pq)
                load3(qT, i_q, 8, lambda k: [("qT", i, k // 4) for i in range(16)])
                with contextlib.ExitStack() as ph:
                    kT = sb("kT", [128, 2, SEQ], BF16, stack=ph)
                    S.dma("sp", kT[:, :, 0:NTOK], x_k.rearrange("p (g t) -> p g t", g=2), writes=["kT"])
                    S.dma("sp", kT[:, :, NTOK:SEQ], i_k.rearrange("p (g t) -> p g t", g=2), writes=["kT"])
                    va = sb("va", [128, 32, 260], BF16, stack=ph)
                    xv_parts = [d[0:128, :] for d in xv_dst] if FUSED else [x_v[:, 0:2080], x_v[:, 2080:4160]]
                    iv_parts = xv_src if FUSED else [i_v[:, 0:2080], i_v[:, 2080:4160]]
                    for hv in range(2):
                        S.dma("sp", va[:, hv * 8:(hv + 1) * 8, :], xv_parts[hv].rearrange("p (j c) -> p j c", j=8), writes=["va"])
                        S.dma("sp", va[:, 16 + hv * 8:16 + (hv + 1) * 8, :], iv_parts[hv].rearrange("p (j c) -> p j c", j=8), writes=["va"])
                    kiT = sb("kiT", [64, SEQ], BF16, stack=ph)
                    S.dma("sp", kiT[:, 0:NTOK], x_ki[:, :], writes=["kiT"])
                    S.dma("sp", kiT[:, NTOK:SEQ], i_ki[:, :], writes=["kiT"])
                    qiT = sb("qiT", [64, 4, NTOK], BF16, stack=ph)
                    load3(qiT, i_qi, 4, lambda k: ["qiT"])
                    wi = sb("wi", [128, 16, 8], stack=ph)
                    S.dma("sp", wi[:].rearrange("p j c -> p (j c)"), i_wi[:, :], writes=["wi"])
                    pbc = sb("pbc", [128, 1], stack=ph)
                    S.dma("sp", pbc[:], pb_d[:, :], writes=["pbc"])
                    caus = sb("caus", [128, 128], stack=ph)
                    S.dma("sp", caus[:], caus_d[:, :], writes=["caus"])
                    isc = sb("isc", [128, SEQ], stack=ph)
                    junk = sb("junk", [128, SEQ], BF16, stack=ph)
                    selT = sb("selT", [128, 32, 128], BF16, stack=ph)
                    rl = [sb("rl%d" % i, [128, 512], stack=ph) for i in range(2)]
                    ebuf = [sb("ebuf%d" % i, [128, 512], BF16, stack=ph) for i in range(2)]
                    pT = [sb("pTb%d" % i, [128, 4, 128], BF16, stack=ph) for i in range(2)]
                    otok = sb("otok", [128, 4, 128], BF16, stack=ph)
                    sm = sb("sm", [128, 8], stack=ph)

                    for i in range(16):
                        qc = slice(i * 128, (i + 1) * 128)
                        n_kb = 16 + i + 1
                        n_k = n_kb * 128
                        ngr = (n_k + 511) // 512
                        for kg in range(ngr):
                            k0 = kg * 512
                            w = min(512, n_k - k0)
                            for h in range(4):
                                T(lambda h=h, k0=k0, w=w: nc.tensor.matmul(ps[2 + h][:, 0:w], lhsT=qiT[:, h, qc], rhs=kiT[:, k0:k0 + w], start=True, stop=True),
                                  ["qiT", "kiT"], [("ps", 2 + h)])
                            for h in range(4):
                                rb = h % 2
                                A(lambda h=h, rb=rb, w=w: nc.scalar.activation(out=rl[rb][:, 0:w], in_=ps[2 + h][:, 0:w], func=AF.Relu, scale=wi[:, i, h:h + 1]),
                                  [("ps", 2 + h), "wi"], [("rl", rb)])
                                if h == 0:
                                    V(lambda rb=rb, k0=k0, w=w: nc.vector.tensor_scalar(out=isc[:, k0:k0 + w], in0=rl[rb][:, 0:w], scalar1=wi[:, i, 4:5], scalar2=None, op0=ALU.mult),
                                      [("rl", rb), "wi"], ["isc"])
                                else:
                                    V(lambda h=h, rb=rb, k0=k0, w=w: nc.vector.scalar_tensor_tensor(out=isc[:, k0:k0 + w], in0=rl[rb][:, 0:w], scalar=wi[:, i, 4 + h:5 + h],
                                                                                                    in1=isc[:, k0:k0 + w], op0=ALU.mult, op1=ALU.add),
                                      [("rl", rb), "wi", "isc"], ["isc"])
                        V(lambda: nc.vector.tensor_reduce(out=sm[:, 1:2], in_=isc[:, 0:n_k], axis=AX.X, op=ALU.max), ["isc"], ["sm"])
                        V(lambda: nc.vector.tensor_reduce(out=sm[:, 0:1], in_=isc[:, 0:n_k], axis=AX.X, op=ALU.min), ["isc"], ["sm"])
                        V(lambda: nc.vector.tensor_scalar(out=isc[:, 0:NTOK], in0=isc[:, 0:NTOK], scalar1=pbc[:, 0:1], scalar2=None, op0=ALU.add), ["isc", "pbc"], ["isc"])
                        V(lambda: nc.vector.tensor_tensor(out=isc[:, n_k - 128:n_k], in0=isc[:, n_k - 128:n_k], in1=caus[:], op=ALU.add), ["isc", "caus"], ["isc"])
                        for it in range(N_IT):
                            V(lambda: nc.vector.tensor_scalar(out=sm[:, 2:3], in0=sm[:, 0:1], scalar1=sm[:, 1:2], scalar2=0.5, op0=ALU.add, op1=ALU.mult), ["sm"], ["sm"])
                            V(lambda: nc.vector.tensor_scalar(out=junk[:, 0:n_k], in0=isc[:, 0:n_k], scalar1=sm[:, 2:3], scalar2=None, op0=ALU.is_ge, op1=ALU.add, accum_out=sm[:, 3:4]),
                              ["isc", "sm"], ["junk", "sm"])
                            V(lambda: nc.vector.tensor_scalar(out=sm[:, 4:5], in0=sm[:, 3:4], scalar1=255.5, scalar2=None, op0=ALU.is_ge), ["sm"], ["sm"])
                            V(lambda: nc.vector.tensor_tensor(out=sm[:, 5:6], in0=sm[:, 2:3], in1=sm[:, 0:1], op=ALU.subtract), ["sm"], ["sm"])
                            V(lambda: nc.vector.scalar_tensor_tensor(out=sm[:, 0:1], in0=sm[:, 5:6], scalar=sm[:, 4:5], in1=sm[:, 0:1], op0=ALU.mult, op1=ALU.add), ["sm"], ["sm"])
                            V(lambda: nc.vector.tensor_tensor(out=sm[:, 5:6], in0=sm[:, 1:2], in1=sm[:, 2:3], op=ALU.subtract), ["sm"], ["sm"])
                            V(lambda: nc.vector.scalar_tensor_tensor(out=sm[:, 1:2], in0=sm[:, 5:6], scalar=sm[:, 4:5], in1=sm[:, 2:3], op0=ALU.mult, op1=ALU.add), ["sm"], ["sm"])
                        V(lambda: nc.vector.tensor_scalar(out=junk[:, 0:n_k], in0=isc[:, 0:n_k], scalar1=sm[:, 0:1], scalar2=None, op0=ALU.is_ge), ["isc", "sm"], ["junk"])
                        for kb0 in range(0, n_kb, 4):
                            nb = min(4, n_kb - kb0)
                            half = 0
                            for b in range(nb):
                                kb = kb0 + b
                                T(lambda b=b, kb=kb, half=half: nc.tensor.transpose(psb[:, half * 512 + b * 128:half * 512 + (b + 1) * 128], junk[:, kb * 128:(kb + 1) * 128], ident_bf[:]),
                                  ["junk", "ident_bf"], ["psb"], inc=(b == nb - 1))
                            A(lambda kb0=kb0, nb=nb, half=half: nc.scalar.copy(out=selT[:, kb0:kb0 + nb, :], in_=psb[:, half * 512:half * 512 + nb * 128].rearrange("p (b t) -> p b t", b=nb)),
                              ["psb"], [("selT", kb0)])
                        for g in range(2):
                            for kb in range(n_kb):
                                sc = kb % 2
                                T(lambda kb=kb, g=g, sc=sc: nc.tensor.matmul(ps[sc][:], lhsT=kT[:, g, kb * 128:(kb + 1) * 128], rhs=qT[:, 4 * g:4 * g + 4, qc], start=True, stop=True),
                                  ["kT", ("qT", i, g)], [("ps", sc)])
                                A(lambda sc=sc: nc.scalar.activation(out=ebuf[sc][:], in_=ps[sc][:], func=AF.Exp, scale=float(128 ** -0.5)), [("ps", sc)], [("ebuf", sc)])
                                V(lambda kb=kb, sc=sc: nc.vector.tensor_tensor(out=pT[sc][:], in0=ebuf[sc][:].rearrange("p (h t) -> p h t", h=4),
                                                                              in1=selT[:, kb:kb + 1, :].to_broadcast([128, 4, 128]), op=ALU.mult),
                                  [("ebuf", sc), ("selT", (kb // 4) * 4)], [("pT", sc)])
                                for hh in range(4):
                                    T(lambda kb=kb, g=g, sc=sc, hh=hh: nc.tensor.matmul(ps[2 + hh][:, 0:129], lhsT=pT[sc][:, hh, :], rhs=va[:, kb, g * 130:g * 130 + 129],
                                                                                        start=(kb == 0), stop=(kb == n_kb - 1)),
                                      [("pT", sc), "va"], [("ps", 2 + hh)], inc=(hh == 3 or kb == n_kb - 1))
                            for hh in range(4):
                                V(lambda hh=hh: nc.vector.reciprocal(out=sm[:, 6:7], in_=ps[2 + hh][:, 128:129]), [("ps", 2 + hh)], ["sm"])
                                V(lambda hh=hh: nc.vector.tensor_scalar(out=otok[:, hh, :], in0=ps[2 + hh][:, 0:128], scalar1=sm[:, 6:7], scalar2=None, op0=ALU.mult),
                                  [("ps", 2 + hh), "sm"], ["otok"])
                            for hh in range(4):
                                T(lambda hh=hh: nc.tensor.transpose(psb[:, hh * 128:(hh + 1) * 128], otok[:, hh, :], ident_bf[:]), ["otok", "ident_bf"], ["psb"], inc=(hh == 3))
                            A(lambda g=g: nc.scalar.copy(out=qT[:, 4 * g:4 * g + 4, qc], in_=psb[:, 0:512].rearrange("p (h t) -> p h t", h=4)), ["psb"], [("qT", i, g)])
                    S.barrier()
                with contextlib.ExitStack() as ph:
                    w_out = sb("w_out", [128, KC, D], BF16, stack=ph)
                    load_wcast(w_out, wout_d, D, "w_out")
                    out_proj(w_out, "w_out", lambda k, cols: qT[:, k, cols], lambda k, tg: [("qT", 4 * tg + j, gg) for j in range(4) for gg in range(2)])
                    S.barrier()

            ffn(wi2_d, wo2_d, g_f2, "g_f2")

            with contextlib.ExitStack() as ph:
                sq = sb("sq", [128, 2, 512], BF16, stack=ph)
                rstd = sb("rstd", [128, 512], stack=ph)
                yT = sb("yT", [128, KC, 512], stack=ph)
                yo = [sb("yo%d" % i, [128, D], stack=ph) for i in range(2)]
                io = 0
                for tg in range(4):
                    cols = slice(tg * 512, (tg + 1) * 512)
                    rstd_ps(lambda kc: hT[:, kc, cols], KC, 512, sq, 6, lambda kc: [("hT", kc)])
                    rstd_from_ps(rstd[:], 6, 512, D)
                    for kc in range(KC):
                        V(lambda kc=kc, cols=cols: nc.vector.scalar_tensor_tensor(out=yT[:, kc, :], in0=hT[:, kc, cols], scalar=g_fin[:, kc:kc + 1], in1=rstd[:],
                                                                                 op0=ALU.mult, op1=ALU.mult),
                          [("hT", kc), "g_fin", "rstd"], [("yT", kc)])
                    for j in range(4):
                        yb = io % 2
                        io += 1
                        for k2 in range(2):
                            pb = k2
                            for kq in range(4):
                                kc = k2 * 4 + kq
                                T(lambda kc=kc, kq=kq, j=j, pb=pb: nc.tensor.transpose(ps[pb][:, kq * 128:(kq + 1) * 128], yT[:, kc, j * 128:(j + 1) * 128], ident[:]),
                                  [("yT", kc), "ident"], [("ps", pb)], inc=(kq == 3))
                            if k2 == 0:
                                V(lambda yb=yb, pb=pb: nc.vector.tensor_copy(out=yo[yb][:, 0:512], in_=ps[pb][:]), [("ps", pb)], [("yo", yb)])
                            else:
                                A(lambda yb=yb, pb=pb: nc.scalar.copy(out=yo[yb][:, 512:1024], in_=ps[pb][:]), [("ps", pb)], [("yo", yb)])
                        tt = tg * 4 + j
                        S.dma("sp", y_d[tt * 128:(tt + 1) * 128, :], yo[yb][:], reads=[("yo", yb)], writes=[("y_dram", tt)])
                S.barrier()

        S.barrier()
    return nc


_PROG = {}


def _prog(launch):
    if launch not in _PROG:
        _PROG[launch] = build_program(launch)
    return _PROG[launch]


def _f32(a):
    return np.ascontiguousarray(np.asarray(a), dtype=np.float32)


def _consts():
    c = {"c_ident": np.eye(128, dtype=np.float32)}
    t = np.arange(64)
    c["c_tin64"] = np.where(t[:, None] <= t[None, :], -1.0 / 16.0, 0.0).astype(np.float32)
    c["c_uex64"] = np.where(t[:, None] > t[None, :], -1.0 / 16.0, 0.0).astype(np.float32)
    cm = (t[:, None] <= t[None, :]).astype(np.float32)
    c["c_cmask"] = np.tile(cm, (1, 4)).astype(np.float32)
    s = np.arange(128)
    c["c_caus"] = np.where(s[None, :] <= s[:, None], 0.0, NEG).astype(np.float32)
    r = np.arange(128)
    inv128 = (10000.0 ** (-(np.arange(0, 128, 2, dtype=np.float32)) / 128.0)).astype(np.float32)
    inv64 = (10000.0 ** (-(np.arange(0, 64, 2, dtype=np.float32)) / 64.0)).astype(np.float32)
    rope = np.zeros((128, 4), np.float32)
    rope[:, 0] = inv128[r % 64]
    rope[:, 1] = np.where(r < 64, -1.0, 1.0)
    rope[:, 2] = inv64[r % 32]
    rope[:, 3] = np.where((r % 64) < 32, -1.0, 1.0)
    c["c_rope"] = rope
    return c


def _rot_perm(n_heads, hd):
    idx = np.arange(n_heads * hd).reshape(n_heads, hd)
    return np.concatenate([idx[:, hd // 2:], idx[:, :hd // 2]], axis=1).reshape(-1)


def _run(launch, in_maps):
    nc = _prog(launch)
    ncores = int(os.environ.get("KCORES", str(N_CORES)))
    res = run_bass_kernel_spmd(nc, in_maps[:ncores], core_ids=list(range(ncores)))
    r = list(res.results)
    return r + [r[i % ncores] for i in range(ncores, N_CORES)]


def _kernel_fused(inputs):
    x = _f32(inputs["x"])
    pos = np.ascontiguousarray(inputs["positions"], dtype=np.int32)
    C = _consts()

    def row(a):
        return _f32(a).reshape(1, -1)

    gate_wb = np.concatenate([_f32(inputs["gla_gate_w"])[0], _f32(inputs["gla_gate_b"])[0][None, :]], axis=0)
    w_in1 = _f32(inputs["odd_w_in"][0])
    perm = np.concatenate([_rot_perm(8, 128), 1024 + _rot_perm(2, 128), 1536 + _rot_perm(4, 64), 1792 + _rot_perm(1, 64)])
    w_rot = np.ascontiguousarray(w_in1[:, perm])
    pa = {
        "c_tin64": C["c_tin64"], "c_uex64": C["c_uex64"], "c_cmask": C["c_cmask"],
        "g_ffn1_0": row(inputs["ffn1_norm"][0]), "g_mix0": row(inputs["mix_norm"][0]),
        "ffn_wi": _f32(inputs["ffn1_wi"][0]), "ffn_wo": _f32(inputs["ffn1_wo"][0]),
        "even_w_in": _f32(inputs["even_w_in"][0]), "gate_wb": _f32(gate_wb),
        "pool_w": _f32(inputs["pool_w"][0]), "pool_scale": row(inputs["pool_scale"][0]),
    }
    pb = {
        "c_rope": C["c_rope"],
        "gla_out_norm": row(inputs["gla_out_norm"][0]), "pool_w": _f32(inputs["pool_w"][0]), "pool_scale": row(inputs["pool_scale"][0]),
        "even_w_out": _f32(inputs["even_w_out"][0]),
        "g_ffn2_0": row(inputs["ffn2_norm"][0]), "g_ffn1_1": row(inputs["ffn1_norm"][1]), "g_mix1": row(inputs["mix_norm"][1]),
        "ffn2_wi": _f32(inputs["ffn2_wi"][0]), "ffn2_wo": _f32(inputs["ffn2_wo"][0]),
        "ffn1_wi": _f32(inputs["ffn1_wi"][1]), "ffn1_wo": _f32(inputs["ffn1_wo"][1]),
        "odd_w_in": w_in1, "odd_w_rot": w_rot,
    }
    pc = {
        "c_caus": C["c_caus"], "odd_w_out": _f32(inputs["odd_w_out"][0]),
        "g_ffn2_1": row(inputs["ffn2_norm"][1]), "g_final": row(inputs["final_norm"]),
        "ffn2_wi": _f32(inputs["ffn2_wi"][1]), "ffn2_wo": _f32(inputs["ffn2_wo"][1]),
    }
    shared = {"c_ident": C["c_ident"]}
    for pfx, d in (("A", pa), ("B", pb), ("C", pc)):
        for k, v in d.items():
            shared[pfx + "_" + k] = v
    tloc = np.arange(16, dtype=np.float32)
    maps = []
    for c in range(N_CORES):
        b, half = c // 2, c % 2
        m = dict(shared)
        m["A_x"] = np.ascontiguousarray(x[b, half * NTOK:(half + 1) * NTOK, :])
        invc = np.zeros((128, 4, 16), np.float32)
        for g, w in enumerate((2, 4, 8, 16)):
            cnt = np.full(16, float(w), np.float32) if half == 1 else np.minimum(tloc + 1.0, float(w))
            invc[:, g, :] = (1.0 / cnt)[None, :]
        m["B_c_invcnt"] = invc.reshape(128, 64)
        m["B_positions"] = np.ascontiguousarray(pos[b, half * NTOK:(half + 1) * NTOK]).reshape(1, NTOK)
        m["B_c_flag"] = np.full((128, 1), float(half), np.float32)
        m["C_c_pbias"] = np.zeros((128, 1), np.float32) if half == 1 else np.full((128, 1), NEG, np.float32)
        maps.append(m)
    rc = _run("F", maps)
    out = np.zeros((4, SEQ, D), dtype=np.float32)
    for c in range(N_CORES):
        b, half = c // 2, c % 2
        out[b, half * NTOK:(half + 1) * NTOK, :] = rc[c]["y"]
    return out


def kernel(**inputs):
    debug = inputs.pop("_debug", None)
    if debug is None and os.environ.get("KUNFUSED") != "1":
        return _kernel_fused(inputs)
    x = _f32(inputs["x"])
    pos = np.ascontiguousarray(inputs["positions"], dtype=np.int32)
    C = _consts()
    bf = ml_dtypes.bfloat16

    def row(a):
        return _f32(a).reshape(1, -1)

    gate_wb = np.concatenate([_f32(inputs["gla_gate_w"])[0], _f32(inputs["gla_gate_b"])[0][None, :]], axis=0)
    shared = {
        "c_ident": C["c_ident"], "c_tin64": C["c_tin64"], "c_uex64": C["c_uex64"], "c_cmask": C["c_cmask"],
        "g_ffn1_0": row(inputs["ffn1_norm"][0]), "g_mix0": row(inputs["mix_norm"][0]),
        "ffn_wi": _f32(inputs["ffn1_wi"][0]), "ffn_wo": _f32(inputs["ffn1_wo"][0]),
        "even_w_in": _f32(inputs["even_w_in"][0]), "gate_wb": _f32(gate_wb),
        "pool_w": _f32(inputs["pool_w"][0]), "pool_scale": row(inputs["pool_scale"][0]),
    }
    maps = []
    for c in range(N_CORES):
        b, half = c // 2, c % 2
        m = dict(shared)
        m["x"] = np.ascontiguousarray(x[b, half * NTOK:(half + 1) * NTOK, :])
        maps.append(m)
    if LITE:
        for m in maps:
            m.pop("ffn_wi"); m.pop("ffn_wo")
    ra = _run("A", maps)
    if debug == "A":
        return ra

    w_in1 = _f32(inputs["odd_w_in"][0])
    perm = np.concatenate([_rot_perm(8, 128), 1024 + _rot_perm(2, 128), 1536 + _rot_perm(4, 64), 1792 + _rot_perm(1, 64)])
    w_rot = np.ascontiguousarray(w_in1[:, perm])
    shared = {
        "c_ident": C["c_ident"], "c_rope": C["c_rope"],
        "gla_out_norm": row(inputs["gla_out_norm"][0]), "pool_w": _f32(inputs["pool_w"][0]), "pool_scale": row(inputs["pool_scale"][0]),
        "even_w_out": _f32(inputs["even_w_out"][0]),
        "g_ffn2_0": row(inputs["ffn2_norm"][0]), "g_ffn1_1": row(inputs["ffn1_norm"][1]), "g_mix1": row(inputs["mix_norm"][1]),
        "ffn2_wi": _f32(inputs["ffn2_wi"][0]), "ffn2_wo": _f32(inputs["ffn2_wo"][0]),
        "ffn1_wi": _f32(inputs["ffn1_wi"][1]), "ffn1_wo": _f32(inputs["ffn1_wo"][1]),
        "odd_w_in": w_in1, "odd_w_rot": w_rot,
    }
    tloc = np.arange(16, dtype=np.float32)
    maps = []
    for c in range(N_CORES):
        b, half = c // 2, c % 2
        m = dict(shared)
        for k in ("st_hT", "st_o", "st_qe", "st_sg", "st_pp", "st_bl", "st_u16"):
            m[k] = ra[c][k]
        if half == 1:
            m["x_sin"] = ra[c - 1]["st_send"]
            m["x_halo"] = ra[c - 1]["st_utail"]
        else:
            m["x_sin"] = np.zeros((64, 512), np.float32)
            m["x_halo"] = np.zeros((128, 64), np.float32)
        invc = np.zeros((128, 4, 16), np.float32)
        for g, w in enumerate((2, 4, 8, 16)):
            cnt = np.full(16, float(w), np.float32) if half == 1 else np.minimum(tloc + 1.0, float(w))
            invc[:, g, :] = (1.0 / cnt)[None, :]
        m["c_invcnt"] = invc.reshape(128, 64)
        m["positions"] = np.ascontiguousarray(pos[b, half * NTOK:(half + 1) * NTOK]).reshape(1, NTOK)
        maps.append(m)
    rb = _run("B", maps)
    if debug == "B":
        return ra, rb

    shared = {
        "c_ident": C["c_ident"], "c_caus": C["c_caus"],
        "odd_w_out": _f32(inputs["odd_w_out"][0]),
        "g_ffn2_1": row(inputs["ffn2_norm"][1]), "g_final": row(inputs["final_norm"]),
        "ffn2_wi": _f32(inputs["ffn2_wi"][1]), "ffn2_wo": _f32(inputs["ffn2_wo"][1]),
    }
    maps = []
    for c in range(N_CORES):
        b, half = c // 2, c % 2
        m = dict(shared)
        m["st_hT"] = rb[c]["st_hT_o"]
        for k in ("st_q", "st_k", "st_qi", "st_ki", "st_v", "st_wi"):
            m[k] = rb[c][k]
        if half == 1:
            m["x_k"], m["x_ki"], m["x_v"] = rb[c - 1]["st_k"], rb[c - 1]["st_ki"], rb[c - 1]["st_v"]
            m["c_pbias"] = np.zeros((128, 1), np.float32)
        else:
            m["x_k"] = np.zeros((128, 2 * NTOK), bf)
            m["x_ki"] = np.zeros((64, NTOK), bf)
            m["x_v"] = np.zeros((128, 16 * 260), bf)
            m["c_pbias"] = np.full((128, 1), NEG, np.float32)
        maps.append(m)
    rc = _run("C", maps)
    out = np.zeros((4, SEQ, D), dtype=np.float32)
    for c in range(N_CORES):
        b, half = c // 2, c % 2
        out[b, half * NTOK:(half + 1) * NTOK, :] = rc[c]["y"]
    return out
```

```python
import contextlib
import numpy as np
import ml_dtypes
import concourse.bass as bass
import concourse.mybir as mybir
from concourse.bass_utils import run_bass_kernel_spmd

F32 = mybir.dt.float32
BF16 = mybir.dt.bfloat16
I32 = mybir.dt.int32
ALU = mybir.AluOpType
AF = mybir.ActivationFunctionType
AX = mybir.AxisListType

D = 1024
KC = 8
DFF = 2816
FC = 22
NTOK = 2048
SEQ = 4096
EPS = 1e-6
N_CORES = 8
NEG = -1.0e30
N_IT = 13
import os
LITE = os.environ.get("KLITE") == "1"
KSTOP = int(os.environ.get("KSTOP", "99"))
KSKIP = set(os.environ.get("KSKIP", "").split(","))
TWO_PI = 2.0 * np.pi
CW1 = 6.28125
CW2 = TWO_PI - 6.28125


class Sched:
    def __init__(self, nc, es, n_dma_sems=6):
        self.nc = nc
        self.eng = {"pe": nc.tensor, "act": nc.scalar, "dve": nc.vector, "pool": nc.gpsimd, "sp": nc.sync}
        self.sems = {}
        self.cnt = {}
        for e in ("pe", "act", "dve", "pool"):
            self.sems[e] = es.enter_context(nc.semaphore("s_" + e))
            self.cnt[e] = 0
        self.dq = {}
        self.dq_next = {}
        for q in ("sp", "pool"):
            names = []
            for i in range(n_dma_sems):
                nm = "d_%s%d" % (q, i)
                self.sems[nm] = es.enter_context(nc.semaphore(nm))
                self.cnt[nm] = 0
                names.append(nm)
            self.dq[q] = names
            self.dq_next[q] = 0
        for nm in ("cc1", "cc2", "cc3", "cc4", "cc5"):
            self.sems[nm] = es.enter_context(nc.semaphore("s_" + nm))
            self.cnt[nm] = 0
        self.seen = {e: {} for e in self.eng}
        self.lastw = {}
        self.readers = {}

    def collective(self, name, fn):
        self.barrier()
        fn().then_inc(self.sems[name], 1)
        self.cnt[name] = 1
        self.barrier()

    def _wait(self, e, x, v):
        if v <= 0 or self.seen[e].get(x, 0) >= v:
            return
        self.eng[e].wait_ge(self.sems[x], v)
        self.seen[e][x] = v

    def _deps(self, e, reads, writes):
        d = {}

        def add(tok, war=False):
            if tok is None:
                return
            x, v = tok
            if x == e and e == "pe":
                return
            if d.get(x, 0) < v:
                d[x] = v

        for r in reads:
            add(self.lastw.get(r))
            if r == "psb" or (isinstance(r, tuple) and r[0] == "ps"):
                for x2, tok in self.readers.get(r, {}).items():
                    if x2 != e:
                        add(tok)
        for w in writes:
            add(self.lastw.get(w))
            for tok in self.readers.get(w, {}).values():
                add(tok, war=True)
        return d

    def _record(self, tok, reads, writes):
        for r in reads:
            self.readers.setdefault(r, {})[tok[0]] = tok
        for w in writes:
            self.lastw[w] = tok
            self.readers[w] = {}

    def op(self, e, fn, reads=(), writes=(), inc=True):
        d = self._deps(e, reads, writes)
        for x, v in d.items():
            self._wait(e, x, v)
        ins = fn()
        if inc:
            self.cnt[e] += 1
            ins.then_inc(self.sems[e], 1)
            tok = (e, self.cnt[e])
        else:
            tok = (e, self.cnt[e] + 1)
        self._record(tok, reads, writes)
        return ins

    def dma(self, q, out, in_, reads=(), writes=()):
        d = self._deps(q, reads, writes)
        i = self.dq_next[q]
        self.dq_next[q] = (i + 1) % len(self.dq[q])
        nm = self.dq[q][i]
        self._wait(q, nm, self.cnt[nm])
        for x, v in d.items():
            self._wait(q, x, v)
        self.cnt[nm] += 16
        self.eng[q].dma_start(out=out, in_=in_).then_inc(self.sems[nm], 16)
        self._record((nm, self.cnt[nm]), reads, writes)

    def barrier(self, engines=("pe", "act", "dve", "pool", "sp")):
        for e in engines:
            for x, v in self.cnt.items():
                if x == e and e == "pe":
                    continue
                self._wait(e, x, v)


def build_program(launch):
    nc = bass.Bass("TRN2", target_bir_lowering=False)

    FUSED = (launch == "F")
    parts = "ABC" if FUSED else launch
    CUR = ["A"]
    STATE = {}
    EXCH_IN = {}
    EXCH_OUT = {}
    if FUSED:
        def internal(name, shape, dt):
            return nc.dram_tensor(name, list(shape), dt, kind="Internal", addr_space="Local").ap()
        xa_src = internal("xa_src", [128, 1024], F32)
        xa_dst = internal("xa_dst", [256, 1024], F32)
        xk_src = internal("xk_src", [128, 4096], BF16)
        xk_dst = internal("xk_dst", [256, 4096], BF16)
        xki_src = internal("xki_src", [128, 2048], BF16)
        xki_dst = internal("xki_dst", [256, 2048], BF16)
        xv_src = [internal("xv_src%d" % i, [128, 2080], BF16) for i in range(2)]
        xv_dst = [internal("xv_dst%d" % i, [256, 2080], BF16) for i in range(2)]
        EXCH_OUT = {"st_send": xa_src[0:64, 0:512], "st_utail": xa_src[:, 512:576],
                    "st_k": xk_src[:, :], "st_ki": xki_src[0:64, :]}
        EXCH_IN = {"x_sin": xa_dst[0:64, 0:512], "x_halo": xa_dst[0:128, 512:576],
                   "x_k": xk_dst[0:128, :], "x_ki": xki_dst[0:64, :]}
        XCH2 = [("cc2", xk_src, xk_dst), ("cc3", xki_src, xki_dst), ("cc4", xv_src[0], xv_dst[0]), ("cc5", xv_src[1], xv_dst[1])]

    def din(name, shape, dt=F32):
        if FUSED:
            if name in STATE:
                return STATE[name]
            if name in EXCH_IN:
                return EXCH_IN[name]
            if name != "c_ident":
                name = CUR[0] + "_" + name
        return nc.dram_tensor(name, list(shape), dt, kind="ExternalInput").ap()

    def dout(name, shape, dt=F32):
        if FUSED and name != "y":
            if name in EXCH_OUT:
                STATE[name] = EXCH_OUT[name]
            else:
                STATE[name] = nc.dram_tensor("i_" + name, list(shape), dt, kind="Internal", addr_space="Local").ap()
            return STATE[name]
        return nc.dram_tensor(name, list(shape), dt, kind="ExternalOutput").ap()

    PAIRS = [[0, 1], [2, 3], [4, 5], [6, 7]]
    ident_d = din("c_ident", [128, 128])

    with contextlib.ExitStack() as es:
        S = Sched(nc, es)
        _uid = [0]

        def sb(name, shape, dt=F32, stack=es):
            _uid[0] += 1
            return stack.enter_context(nc.sbuf_tensor("%s_%d" % (name, _uid[0]), list(shape), dt))

        def V(fn, r=(), w=()):
            return S.op("dve", fn, reads=r, writes=w)

        def A(fn, r=(), w=()):
            return S.op("act", fn, reads=r, writes=w)

        def G(fn, r=(), w=()):
            return S.op("pool", fn, reads=r, writes=w)

        def T(fn, r=(), w=(), inc=True):
            return S.op("pe", fn, reads=r, writes=w, inc=inc)

        hT = sb("hT", [128, KC, NTOK])
        ident = sb("ident", [128, 128])
        ident_bf = sb("ident_bf", [128, 128], BF16)
        ones_bf = sb("ones_bf", [128, 128], BF16)
        eps_c = sb("eps_c", [128, 1])
        one_c = sb("one_c", [128, 1])
        ps = [es.enter_context(nc.psum_tensor("ps%d" % i, [128, 512], F32)) for i in range(7)]
        psb = es.enter_context(nc.psum_tensor("psb", [128, 1024], BF16))

        stage = [sb("stage%d" % i, [128, 1024]) for i in range(2)]
        _rr = [0]

        def cast_load(dst_ap, src_ap, view, dkey):
            i = _rr[0] % 2
            eng = "pool" if _rr[0] % 2 == 0 else "act"
            _rr[0] += 1
            st = view(stage[i])
            S.dma("sp", st, src_ap, writes=[("stage", i)])
            if eng == "pool":
                G(lambda: nc.gpsimd.tensor_copy(out=dst_ap, in_=st), [("stage", i)], [dkey])
            else:
                A(lambda: nc.scalar.copy(out=dst_ap, in_=st), [("stage", i)], [dkey])

        S.dma("sp", ident[:], ident_d[:, :], writes=["ident"])
        V(lambda: nc.vector.tensor_copy(out=ident_bf[:], in_=ident[:]), ["ident"], ["ident_bf"])
        V(lambda: nc.vector.memset(ones_bf[:], 1.0), [], ["ones_bf"])
        V(lambda: nc.vector.memset(eps_c[:], EPS), [], ["eps"])
        V(lambda: nc.vector.memset(one_c[:], 1.0), [], ["one"])

        def load_gain(dram_row, name):
            g = sb(name, [128, KC])
            with nc.allow_non_contiguous_dma(reason="tiny gain vector"):
                S.dma("sp", g[:], dram_row.rearrange("o (kc p) -> p (o kc)", p=128), writes=[name])
            return g

        def load_state(t_sb, d_ap, key, q="sp"):
            S.dma(q, t_sb, d_ap, writes=[key])

        def rstd_ps(src_fn, nk, n, sq, pbank, key_r):
            for kc in range(nk):
                A(lambda kc=kc: nc.scalar.activation(out=sq[:, kc % 2, 0:n], in_=src_fn(kc), func=AF.Square),
                  key_r(kc), [("sq", kc % 2)])
                T(lambda kc=kc: nc.tensor.matmul(ps[pbank][:, 0:n], lhsT=ones_bf[:], rhs=sq[:, kc % 2, 0:n], start=(kc == 0), stop=(kc == nk - 1)),
                  [("sq", kc % 2), "ones_bf"], [("ps", pbank)])

        def rstd_from_ps(rstd_out, pbank, n, denom):
            A(lambda: nc.scalar.activation(out=rstd_out, in_=ps[pbank][:, 0:n], func=AF.Ln, scale=1.0 / denom, bias=eps_c[:, 0:1]),
              [("ps", pbank), "eps"], ["rstd"])
            A(lambda: nc.scalar.activation(out=rstd_out, in_=rstd_out, func=AF.Exp, scale=-0.5), ["rstd"], ["rstd"])

        def norm_h(cols, gain, gname, sq, rstd, hnT, hn_key):
            rstd_ps(lambda kc: hT[:, kc, cols], KC, 512, sq, 6, lambda kc: [("hT", kc)])
            rstd_from_ps(rstd[:], 6, 512, D)
            for kc in range(KC):
                V(lambda kc=kc: nc.vector.scalar_tensor_tensor(out=hnT(kc), in0=hT[:, kc, cols], scalar=gain[:, kc:kc + 1], in1=rstd[:],
                                                               op0=ALU.mult, op1=ALU.mult),
                  [("hT", kc), gname, "rstd"], [hn_key])

        def ffn(wi_d, wo_d, gain, gname):
            with contextlib.ExitStack() as ph:
                hnT = sb("hnT", [128, KC, NTOK], BF16, stack=ph)
                actT = sb("actT", [128, 11, NTOK], BF16, stack=ph)
                wo_sb = sb("wo_sb", [128, 11, D], BF16, stack=ph)
                wg_sb = [sb("wg_sb%d" % i, [128, KC, 256], BF16, stack=ph) for i in range(2)]
                wu_sb = [sb("wu_sb%d" % i, [128, KC, 256], BF16, stack=ph) for i in range(2)]
                sq = sb("sq", [128, 2, 512], BF16, stack=ph)
                rstd = sb("rstd", [128, 512], stack=ph)
                sg = [sb("sg%d" % i, [128, 512], stack=ph) for i in range(2)]
                for tg in range(4):
                    cols = slice(tg * 512, (tg + 1) * 512)
                    norm_h(cols, gain, gname, sq, rstd, lambda kc, cols=cols: hnT[:, kc, cols], ("hnT", tg))
                it = 0
                ig = 0
                io = 0
                for ffh in range(2):
                    for fgl in range(6):
                        ncg = 2 if fgl < 5 else 1
                        wd = ncg * 128
                        col0 = (ffh * 11 + 2 * fgl) * 128
                        wb = ig % 2
                        ig += 1
                        v4 = lambda t, wd=wd: t[:, 0:4 * wd].rearrange("p (k c) -> p k c", k=4)
                        for hk in range(2):
                            cast_load(wg_sb[wb][:, hk * 4:(hk + 1) * 4, 0:wd],
                                      wi_d[hk * 512:(hk + 1) * 512, col0:col0 + wd].rearrange("(k p) c -> p k c", p=128), v4, ("wg", wb))
                            cast_load(wu_sb[wb][:, hk * 4:(hk + 1) * 4, 0:wd],
                                      wi_d[hk * 512:(hk + 1) * 512, DFF + col0:DFF + col0 + wd].rearrange("(k p) c -> p k c", p=128), v4, ("wu", wb))
                        for c in range(ncg):
                            fc = ffh * 11 + 2 * fgl + c
                            cast_load(wo_sb[:, 2 * fgl + c, :], wo_d[fc * 128:(fc + 1) * 128, :], lambda t: t[:, :], ("wo", 2 * fgl + c))
                        for c in range(ncg):
                            f = 2 * fgl + c
                            for tg in range(4):
                                tcols = slice(tg * 512, (tg + 1) * 512)
                                pg, pu = (0, 1) if it % 2 == 0 else (2, 3)
                                sgi = it % 2
                                it += 1
                                for kc in range(KC):
                                    T(lambda kc=kc, c=c, tcols=tcols, pg=pg, wb=wb: nc.tensor.matmul(
                                        ps[pg][:], lhsT=wg_sb[wb][:, kc, c * 128:(c + 1) * 128], rhs=hnT[:, kc, tcols],
                                        start=(kc == 0), stop=(kc == KC - 1)),
                                      [("wg", wb), ("hnT", tg)], [("ps", pg)], inc=(kc == KC - 1))
                                for kc in range(KC):
                                    T(lambda kc=kc, c=c, tcols=tcols, pu=pu, wb=wb: nc.tensor.matmul(
                                        ps[pu][:], lhsT=wu_sb[wb][:, kc, c * 128:(c + 1) * 128], rhs=hnT[:, kc, tcols],
                                        start=(kc == 0), stop=(kc == KC - 1)),
                                      [("wu", wb), ("hnT", tg)], [("ps", pu)], inc=(kc == KC - 1))
                                A(lambda pg=pg, sgi=sgi: nc.scalar.activation(out=sg[sgi][:], in_=ps[pg][:], func=AF.Silu),
                                  [("ps", pg)], [("sg", sgi)])
                                V(lambda pu=pu, sgi=sgi, f=f, tcols=tcols: nc.vector.tensor_tensor(
                                    out=actT[:, f, tcols], in0=ps[pu][:], in1=sg[sgi][:], op=ALU.mult),
                                  [("ps", pu), ("sg", sgi)], [("actT", f, tg)])
                    for dc in range(KC):
                        for tg in range(4):
                            po = 4 + (io % 2)
                            io += 1
                            tcols = slice(tg * 512, (tg + 1) * 512)
                            for f in range(11):
                                T(lambda f=f, dc=dc, tcols=tcols, po=po: nc.tensor.matmul(
                                    ps[po][:], lhsT=wo_sb[:, f, dc * 128:(dc + 1) * 128], rhs=actT[:, f, tcols],
                                    start=(f == 0), stop=(f == 10)),
                                  [("wo", f), ("actT", f, tg)], [("ps", po)], inc=(f == 10))
                            V(lambda dc=dc, tcols=tcols, po=po: nc.vector.scalar_tensor_tensor(
                                out=hT[:, dc, tcols], in0=ps[po][:], scalar=0.5, in1=hT[:, dc, tcols], op0=ALU.mult, op1=ALU.add),
                              [("ps", po), ("hT", dc)], [("hT", dc)])
                S.barrier()

        def load_wcast(dst, src_d, ncols, key, step=256):
            for c0 in range(0, ncols, step):
                c1 = min(ncols, c0 + step)
                wd = c1 - c0
                for hk in range(2):
                    cast_load(dst[:, hk * 4:(hk + 1) * 4, c0:c1],
                              src_d[hk * 512:(hk + 1) * 512, c0:c1].rearrange("(k p) c -> p k c", p=128),
                              lambda t, wd=wd: t[:, 0:4 * wd].rearrange("p (k c) -> p k c", k=4), key)

        def out_proj(w_sb, wkey, rhs_fn, rkeys):
            io = 0
            for tg in range(4):
                cols = slice(tg * 512, (tg + 1) * 512)
                for dc in range(KC):
                    po = 4 + (io % 2)
                    io += 1
                    for k in range(8):
                        T(lambda k=k, dc=dc, po=po, cols=cols: nc.tensor.matmul(
                            ps[po][:], lhsT=w_sb[:, k, dc * 128:(dc + 1) * 128], rhs=rhs_fn(k, cols), start=(k == 0), stop=(k == 7)),
                          [wkey] + rkeys(k, tg), [("ps", po)], inc=(k == 7))
                    V(lambda dc=dc, cols=cols, po=po: nc.vector.tensor_tensor(out=hT[:, dc, cols], in0=ps[po][:], in1=hT[:, dc, cols], op=ALU.add),
                      [("ps", po), ("hT", dc)], [("hT", dc)])

        def dump3(dram2d, sb3, n, reads, q="sp"):
            dv = dram2d.rearrange("p (n t) -> p n t", n=n)
            for i in range(n):
                S.dma(q, dv[:, i, :], sb3[:, i, :], reads=reads(i))

        def load3(sb3, dram2d, n, writes, q="sp"):
            dv = dram2d.rearrange("p (n t) -> p n t", n=n)
            for i in range(n):
                S.dma(q, sb3[:, i, :], dv[:, i, :], writes=writes(i))

        if "A" in parts:
            CUR[0] = "A"
            x_d = din("x", [NTOK, D])
            g_ffn = load_gain(din("g_ffn1_0", [1, D]), "g_ffn")
            g_mix = load_gain(din("g_mix0", [1, D]), "g_mix")
            if not LITE:
                wi_d = din("ffn_wi", [D, 2 * DFF])
                wo_d = din("ffn_wo", [DFF, D])
            w_in_d = din("even_w_in", [D, 2064])
            gw_d = din("gate_wb", [17, 256])
            poolw_d = din("pool_w", [4, 128, 128])
            pscale_d = din("pool_scale", [1, 512])
            tin_d = din("c_tin64", [64, 64])
            uex_d = din("c_uex64", [64, 64])
            cmask_d = din("c_cmask", [64, 256])
            o_hT = dout("st_hT", [128, KC * NTOK])
            o_o = dout("st_o", [128, 4 * NTOK])
            o_qe = dout("st_qe", [64, 4 * NTOK], BF16)
            o_sg = dout("st_sg", [128, 4 * NTOK], BF16)
            o_pp = dout("st_pp", [128, 4 * NTOK], BF16)
            o_bl = dout("st_bl", [64, 128])
            o_u16 = dout("st_u16", [128, 64])
            o_send = dout("st_send", [64, 512])
            o_utail = dout("st_utail", [128, 64])

            with contextlib.ExitStack() as ph:
                xin = sb("xin", [128, 4, D], stack=ph)
                if FUSED:
                    zf = sb("zf", [128, 1024], stack=ph)
                    zb = sb("zb", [128, 2048], BF16, stack=ph)
                    V(lambda: nc.vector.memset(zf[:], 0.0), [], ["zf"])
                    V(lambda: nc.vector.memset(zb[:], 0.0), [], ["zb"])
                    S.dma("sp", xa_src[:, :], zf[:], reads=["zf"])
                    S.dma("sp", xki_src[64:128, :], zb[64:128, :], reads=["zb"])
                for tg in range(4):
                    for j in range(4):
                        tt = tg * 4 + j
                        S.dma("sp", xin[:, j, :], x_d[tt * 128:(tt + 1) * 128, :], writes=[("xin", j)])
                    for kc in range(KC):
                        pb = kc % 2
                        for j in range(4):
                            T(lambda j=j, kc=kc, pb=pb: nc.tensor.transpose(ps[pb][:, j * 128:(j + 1) * 128], xin[:, j, kc * 128:(kc + 1) * 128], ident[:]),
                              [("xin", j), "ident"], [("ps", pb)], inc=(j == 3))
                        if kc % 2 == 0:
                            V(lambda kc=kc, pb=pb, tg=tg: nc.vector.tensor_copy(out=hT[:, kc, tg * 512:(tg + 1) * 512], in_=ps[pb][:]),
                              [("ps", pb)], [("hT", kc)])
                        else:
                            A(lambda kc=kc, pb=pb, tg=tg: nc.scalar.copy(out=hT[:, kc, tg * 512:(tg + 1) * 512], in_=ps[pb][:]),
                              [("ps", pb)], [("hT", kc)])
                S.barrier()

            if not LITE:
                ffn(wi_d, wo_d, g_ffn, "g_ffn")

            with contextlib.ExitStack() as ph:
                w_in = sb("w_in", [128, KC, 2064], BF16, stack=ph)
                if "wcast" not in KSKIP:
                    load_wcast(w_in, w_in_d, 2064, "w_in")
                gw32 = sb("gw32", [17, 256], stack=ph)
                gw = sb("gw", [17, 256], BF16, stack=ph)
                S.dma("sp", gw32[:], gw_d[:, :], writes=["gw32"])
                A(lambda: nc.scalar.copy(out=gw[:], in_=gw32[:]), ["gw32"], ["gw"])
                poolw = sb("poolw", [128, 4, 128], BF16, stack=ph)
                if "poolw" not in KSKIP:
                    cast_load(poolw[:], poolw_d.rearrange("g c d -> c g d"), lambda t: t[:, 0:512].rearrange("p (g d) -> p g d", g=4), "poolw")
                pscale = sb("pscale", [128, 4], stack=ph)
                if "pscale" not in KSKIP:
                    with nc.allow_non_contiguous_dma(reason="tiny"):
                        S.dma("sp", pscale[:], pscale_d.rearrange("o (g p) -> p (o g)", p=128), writes=["pscale"])
                tin32 = sb("tin32", [64, 64], stack=ph)
                uex32 = sb("uex32", [64, 64], stack=ph)
                tin = sb("tin", [64, 64], BF16, stack=ph)
                uex = sb("uex", [64, 64], BF16, stack=ph)
                cmask = sb("cmask", [64, 256], stack=ph)
                S.dma("sp", tin32[:], tin_d[:, :], writes=["tin32"])
                S.dma("sp", uex32[:], uex_d[:, :], writes=["uex32"])
                S.dma("sp", cmask[:], cmask_d[:, :], writes=["cmask"])
                A(lambda: nc.scalar.copy(out=tin[:], in_=tin32[:]), ["tin32"], ["tin"])
                A(lambda: nc.scalar.copy(out=uex[:], in_=uex32[:]), ["uex32"], ["uex"])
                o_all = sb("o_grp", [128, 4, 512], stack=ph)
                qe = sb("qe_grp", [64, 4, 512], BF16, stack=ph)
                sgT = sb("sg_grp", [128, 4, 512], BF16, stack=ph)
                pp = sb("pp_grp", [128, 4, 512], BF16, stack=ph)
                BL = sb("BL", [64, 4, 32], stack=ph)
                u16 = sb("u16", [128, 4, 16], stack=ph)
                Sst = sb("Sst", [64, 4, 128], stack=ph)
                Sbf = sb("Sbf", [64, 4, 128], BF16, stack=ph)
                hnT = sb("hnT", [128, KC, 512], BF16, stack=ph)
                sq = sb("sq", [128, 2, 512], BF16, stack=ph)
                rstd = sb("rstd", [128, 512], stack=ph)
                a1 = sb("a1", [17, 512], BF16, stack=ph)
                tmpz = sb("tmpz", [64, 256], stack=ph)
                sp32 = sb("sp32", [64, 256], stack=ph)
                sp = sb("sp_hi", [64, 8, 256], BF16, stack=ph)
                spl = sb("sp_lo", [64, 8, 256], BF16, stack=ph)
                eb = sb("eb", [64, 4, 512], stack=ph)
                enb = sb("enb", [64, 4, 512], stack=ph)
                ke = sb("ke", [64, 4, 512], BF16, stack=ph)
                er = sb("er", [64, 256], stack=ph)
                kd = sb("kd", [64, 8, 256], BF16, stack=ph)
                vt = sb("vt", [64, 8, 512], BF16, stack=ph)
                uT1 = sb("uT", [128, 4, 528], stack=ph)
                uT = [uT1, uT1]
                sw = [sb("sw%d" % i, [128, 528], stack=ph) for i in range(2)]
                pT = sb("pT", [128, 512], BF16, stack=ph)
                sTm = sb("sTm", [64, 256], BF16, stack=ph)

                V(lambda: nc.vector.memset(a1[:], 1.0), [], ["a1"])
                V(lambda: nc.vector.memset(Sst[:], 0.0), [], ["Sst"])
                V(lambda: nc.vector.memset(Sbf[:], 0.0), [], ["Sbf"])
                V(lambda: nc.vector.memset(uT1[:, :, 0:16], 0.0), [], [("uT", 0)])

                def proj_fm(pb, c0, m, rows=128):
                    for kc in range(KC):
                        T(lambda kc=kc: nc.tensor.matmul(ps[pb][0:m, :], lhsT=w_in[:, kc, c0:c0 + m], rhs=hnT[:, kc, :],
                                                         start=(kc == 0), stop=(kc == KC - 1)),
                          ["w_in", "hnT"], [("ps", pb)], inc=(kc == KC - 1))

                for tg in range(4 if KSTOP >= 2 else 0):
                    cols = slice(tg * 512, (tg + 1) * 512)
                    lc = slice(0, 512)
                    ub = 0
                    norm_h(cols, g_mix, "g_mix", sq, rstd, lambda kc: hnT[:, kc, :], "hnT")
                    if "alr" not in KSKIP:
                        proj_fm(0, 1536, 16)
                        V(lambda: nc.vector.tensor_copy(out=a1[0:16, :], in_=ps[0][0:16, :]), [("ps", 0)], ["a1"])
                    for ch in range(8 if "z" not in KSKIP else 0):
                        T(lambda ch=ch: nc.tensor.matmul(ps[2][0:64, 0:256], lhsT=a1[0:17, ch * 64:(ch + 1) * 64], rhs=gw[0:17, :], start=True, stop=True),
                          ["a1", "gw"], [("ps", 2)])
                        if "zact" in KSKIP:
                            continue
                        A(lambda: nc.scalar.activation(out=tmpz[:], in_=ps[2][0:64, 0:256], func=AF.Exp, scale=-1.0), [("ps", 2)], ["tmpz"])
                        if "zln" in KSKIP:
                            continue
                        A(lambda ch=ch: nc.scalar.activation(out=sp[:, ch, :], in_=tmpz[:], func=AF.Ln, bias=one_c[0:64, 0:1]), ["tmpz", "one"], [("sp", ch)])
                    for h in range(4 if "bT" not in KSKIP else 0):
                        for ch in range(8):
                            T(lambda ch=ch, h=h: nc.tensor.matmul(ps[3][0:64, ch * 64:(ch + 1) * 64], lhsT=sp[:, ch, h * 64:(h + 1) * 64], rhs=tin[:, :],
                                                                  start=True, stop=True),
                              [("sp", ch), "tin"], [("ps", 3)], inc=(ch == 7))
                        A(lambda h=h: nc.scalar.activation(out=eb[:, h, :], in_=ps[3][0:64, :], func=AF.Exp), [("ps", 3)], [("eb", h)])
                        A(lambda h=h: nc.scalar.activation(out=enb[:, h, :], in_=ps[3][0:64, :], func=AF.Exp, scale=-1.0), [("ps", 3)], [("enb", h)])
                        V(lambda h=h, tg=tg: nc.vector.tensor_copy(out=BL[:, h, tg * 8:(tg + 1) * 8], in_=ps[3][0:64, 63:512:64]), [("ps", 3)], ["BL"])
                    for h in range(4 if KSTOP >= 3 else 0):
                        proj_fm(0, h * 64, 64)
                        V(lambda h=h: nc.vector.scalar_tensor_tensor(out=qe[:, h, :], in0=ps[0][0:64, :], scalar=0.125, in1=eb[:, h, :],
                                                                                op0=ALU.mult, op1=ALU.mult),
                          [("ps", 0), ("eb", h)], ["qe"])
                        proj_fm(1, 256 + h * 64, 64)
                        V(lambda h=h: nc.vector.tensor_tensor(out=ke[:, h, :], in0=ps[1][0:64, :], in1=enb[:, h, :], op=ALU.mult),
                          [("ps", 1), ("enb", h)], ["ke"])
                    for ch in range(8 if KSTOP >= 3 else 0):
                        T(lambda ch=ch: nc.tensor.matmul(ps[2][0:64, 0:256], lhsT=uex[:, :], rhs=sp[:, ch, :], start=True, stop=True),
                          [("sp", ch), "uex"], [("ps", 2)])
                        A(lambda: nc.scalar.activation(out=er[:], in_=ps[2][0:64, 0:256], func=AF.Exp), [("ps", 2)], ["er"])
                        for kc in range(KC):
                            T(lambda kc=kc, ch=ch: nc.tensor.matmul(ps[0][0:64, 0:256], lhsT=hnT[:, kc, ch * 64:(ch + 1) * 64], rhs=w_in[:, kc, 256:512],
                                                                    start=(kc == 0), stop=(kc == KC - 1)),
                              ["w_in", "hnT"], [("ps", 0)], inc=(kc == KC - 1))
                        V(lambda ch=ch: nc.vector.tensor_tensor(out=kd[:, ch, :], in0=ps[0][0:64, 0:256], in1=er[:], op=ALU.mult),
                          [("ps", 0), "er"], [("kd", ch)])
                        for kc in range(KC):
                            T(lambda kc=kc, ch=ch: nc.tensor.matmul(ps[1][0:64, :], lhsT=hnT[:, kc, ch * 64:(ch + 1) * 64], rhs=w_in[:, kc, 512:1024],
                                                                    start=(kc == 0), stop=(kc == KC - 1)),
                              ["w_in", "hnT"], [("ps", 1)], inc=(kc == KC - 1))
                        A(lambda ch=ch: nc.scalar.copy(out=vt[:, ch, :], in_=ps[1][0:64, :]), [("ps", 1)], [("vt", ch)])
                    for h in range(4 if KSTOP >= 4 else 0):
                        proj_fm(h % 2, 1024 + h * 128, 128)
                        A(lambda h=h: nc.scalar.activation(out=sgT[:, h, :], in_=ps[h % 2][:], func=AF.Silu), [("ps", h % 2)], ["sgT"])
                    for g in range(4 if KSTOP >= 4 else 0):
                        proj_fm(g % 2, 1552 + g * 128, 128)
                        V(lambda g=g, ub=ub: nc.vector.tensor_copy(out=uT[ub][:, g, 16:528], in_=ps[g % 2][:]), [("ps", g % 2)], [("uT", ub)])
                    if tg == 0:
                        V(lambda: nc.vector.tensor_copy(out=u16[:], in_=uT[0][:, :, 16:32]), [("uT", 0)], ["u16"])
                    for ch in range(8 if KSTOP >= 5 else 0):
                        cg = tg * 8 + ch
                        ccol = slice(ch * 64, (ch + 1) * 64)
                        gcol = ccol
                        for h in range(4):
                            T(lambda h=h, ccol=ccol, gcol=gcol: nc.tensor.matmul(ps[4][0:64, h * 64:(h + 1) * 64], lhsT=ke[:, h, ccol], rhs=qe[:, h, gcol],
                                                                                start=True, stop=True),
                              ["ke", "qe"], [("ps", 4)], inc=(h == 3))
                        V(lambda: nc.vector.tensor_tensor(out=sTm[:], in0=ps[4][0:64, 0:256], in1=cmask[:], op=ALU.mult), [("ps", 4), "cmask"], ["sTm"])
                        ob = 5 if (ch // 2) % 2 == 0 else 6
                        for h in range(4):
                            oc = slice(h * 128 + (ch % 2) * 64, h * 128 + (ch % 2) * 64 + 64)
                            T(lambda h=h, ch=ch, oc=oc, ob=ob: nc.tensor.matmul(ps[ob][:, oc], lhsT=vt[:, ch, h * 128:(h + 1) * 128], rhs=sTm[:, h * 64:(h + 1) * 64],
                                                                               start=True, stop=False),
                              [("vt", ch), "sTm"], [("ps", ob)], inc=False)
                            T(lambda h=h, gcol=gcol, oc=oc, ob=ob: nc.tensor.matmul(ps[ob][:, oc], lhsT=Sbf[:, h, :], rhs=qe[:, h, gcol], start=False, stop=True),
                              ["Sbf", "qe"], [("ps", ob)], inc=(h == 3))
                        for h in range(4):
                            T(lambda h=h, ch=ch: nc.tensor.matmul(ps[3][0:64, h * 128:(h + 1) * 128], lhsT=kd[:, ch, h * 64:(h + 1) * 64], rhs=vt[:, ch, h * 128:(h + 1) * 128],
                                                                  start=True, stop=True),
                              [("kd", ch), ("vt", ch)], [("ps", 3)], inc=(h == 3))
                        for h in range(4):
                            V(lambda h=h, ch=ch: nc.vector.scalar_tensor_tensor(out=Sst[:, h, :], in0=Sst[:, h, :], scalar=eb[:, h, ch * 64 + 63:ch * 64 + 64],
                                                                               in1=ps[3][0:64, h * 128:(h + 1) * 128], op0=ALU.mult, op1=ALU.add),
                              ["Sst", ("eb", h), ("ps", 3)], ["Sst"])
                        A(lambda: nc.scalar.copy(out=Sbf[:], in_=Sst[:]), ["Sst"], ["Sbf"])
                        if ch % 2 == 1:
                            t0 = (ch // 2) * 128
                            A(lambda ob=ob, t0=t0: nc.scalar.copy(out=o_all[:, :, t0:t0 + 128], in_=ps[ob][:].rearrange("p (h t) -> p h t", h=4)),
                              [("ps", ob)], ["o_all"])
                    for g in range(4 if KSTOP >= 6 else 0):
                        w = 2 ** (g + 1)
                        src = uT[ub][:, g, :]
                        lo = 0
                        step = 1
                        k = 0
                        while step < w:
                            lo += step
                            dst = sw[k % 2]
                            G(lambda src=src, dst=dst, lo=lo, step=step: nc.gpsimd.tensor_tensor(out=dst[:, lo:528], in0=src[:, lo:528], in1=src[:, lo - step:528 - step], op=ALU.add),
                              [("uT", ub), ("sw", 0), ("sw", 1)], [("sw", k % 2)])
                            src = dst
                            step *= 2
                            k += 1
                        V(lambda src=src, g=g, ub=ub, w=w: nc.vector.scalar_tensor_tensor(out=pT[:], in0=src[:, 16:528], scalar=1.0 / w, in1=uT[ub][:, g, 16:528],
                                                                                         op0=ALU.mult, op1=ALU.subtract),
                          [("sw", 0), ("sw", 1), ("uT", ub)], ["pT"])
                        T(lambda g=g: nc.tensor.matmul(ps[g % 2][:], lhsT=poolw[:, g, :], rhs=pT[:], start=True, stop=True), ["poolw", "pT"], [("ps", g % 2)])
                        V(lambda g=g: nc.vector.tensor_scalar(out=pp[:, g, :], in0=ps[g % 2][:], scalar1=pscale[:, g:g + 1], scalar2=None, op0=ALU.mult),
                          [("ps", g % 2), "pscale"], ["pp"])
                    if KSTOP < 7:
                        continue
                    S.dma("sp", o_o.rearrange("p (h t) -> p h t", h=4)[:, :, cols], o_all[:], reads=["o_all"], writes=[("d_o", tg)])
                    S.dma("sp", o_qe.rearrange("p (h t) -> p h t", h=4)[:, :, cols], qe[:], reads=["qe"], writes=[("d_qe", tg)])
                    S.dma("sp", o_sg.rearrange("p (h t) -> p h t", h=4)[:, :, cols], sgT[:], reads=["sgT"], writes=[("d_sg", tg)])
                    S.dma("sp", o_pp.rearrange("p (h t) -> p h t", h=4)[:, :, cols], pp[:], reads=["pp"], writes=[("d_pp", tg)])
                    if tg == 3:
                        S.dma("sp", o_utail.rearrange("p (g t) -> p g t", g=4), uT1[:, :, 512:528], reads=[("uT", 0)], writes=["d_ut"])
                    else:
                        V(lambda: nc.vector.tensor_copy(out=uT1[:, :, 0:16], in_=uT1[:, :, 512:528]), [("uT", 0)], [("uT", 0)])
                S.barrier()
                if not FUSED:
                    dump3(o_hT, hT, KC, lambda k: [("hT", k)])
                if "dsmall" not in KSKIP:
                    S.dma("sp", o_bl[:, :], BL[:].rearrange("p h t -> p (h t)"), reads=["BL"])
                    S.dma("sp", o_u16[:, :], u16[:].rearrange("p h t -> p (h t)"), reads=["u16"])
                    S.dma("sp", o_send[:, :], Sst[:].rearrange("p h t -> p (h t)"), reads=["Sst"])
                S.barrier()

        if "B" in parts:
            CUR[0] = "B"
            if FUSED:
                S.collective("cc1", lambda: nc.gpsimd.collective_compute("AllGather", ALU.bypass, replica_groups=PAIRS, ins=[xa_src[:, :]], outs=[xa_dst[:, :]]))
                flag_d = din("c_flag", [128, 1])
            i_hT = din("st_hT", [128, KC * NTOK])
            i_o = din("st_o", [128, 4 * NTOK])
            i_qe = din("st_qe", [64, 4 * NTOK], BF16)
            i_sg = din("st_sg", [128, 4 * NTOK], BF16)
            i_pp = din("st_pp", [128, 4 * NTOK], BF16)
            i_bl = din("st_bl", [64, 128])
            i_u16 = din("st_u16", [128, 64])
            i_sin = din("x_sin", [64, 512])
            i_halo = din("x_halo", [128, 64])
            i_invc = din("c_invcnt", [128, 64])
            gon_d = din("gla_out_norm", [1, 512])
            poolw_d = din("pool_w", [4, 128, 128])
            pscale_d = din("pool_scale", [1, 512])
            wout_d = din("even_w_out", [D, D])
            g_f2 = load_gain(din("g_ffn2_0", [1, D]), "g_f2")
            g_f1 = load_gain(din("g_ffn1_1", [1, D]), "g_f1")
            g_mix = load_gain(din("g_mix1", [1, D]), "g_mix")
            wi2_d = din("ffn2_wi", [D, 2 * DFF])
            wo2_d = din("ffn2_wo", [DFF, D])
            wi1_d = din("ffn1_wi", [D, 2 * DFF])
            wo1_d = din("ffn1_wo", [DFF, D])
            w_in_d = din("odd_w_in", [D, 1860])
            w_rot_d = din("odd_w_rot", [D, 1600])
            pos_d = din("positions", [1, NTOK], I32)
            ropec_d = din("c_rope", [128, 4])
            o_hT = dout("st_hT_o", [128, KC * NTOK])
            o_q = dout("st_q", [128, 8 * NTOK], BF16)
            o_k = dout("st_k", [128, 2 * NTOK], BF16)
            o_qi = dout("st_qi", [64, 4 * NTOK], BF16)
            o_ki = dout("st_ki", [64, NTOK], BF16)
            o_v = dout("st_v", [128, 16 * 260], BF16)
            ov_parts = xv_src if FUSED else [o_v[:, 0:2080], o_v[:, 2080:4160]]
            o_wi = dout("st_wi", [128, 16 * 8])

            if not FUSED:
                load3(hT, i_hT, KC, lambda k: [("hT", k)])
            with contextlib.ExitStack() as ph:
                o_all = sb("o_all", [128, 4, NTOK], stack=ph)
                qe = sb("qe", [64, 4, NTOK], BF16, stack=ph)
                sgT = sb("sgT", [128, 4, NTOK], BF16, stack=ph)
                pp = sb("pp", [128, 4, NTOK], BF16, stack=ph)
                BL = sb("BL", [64, 4, 32], stack=ph)
                Sin_ = sb("Sin", [64, 4, 128], stack=ph)
                ue = sb("ue", [128, 4, 32], stack=ph)
                invc = sb("invc", [128, 4, 16], stack=ph)
                load3(o_all, i_o, 4, lambda k: ["o_all"])
                load3(qe, i_qe, 4, lambda k: ["qe"])
                load3(sgT, i_sg, 4, lambda k: ["sgT"])
                load3(pp, i_pp, 4, lambda k: ["pp"])
                S.dma("sp", BL[:].rearrange("p h t -> p (h t)"), i_bl[:, :], writes=["BL"])
                S.dma("sp", Sin_[:].rearrange("p h t -> p (h t)"), i_sin[:, :], writes=["Sin"])
                S.dma("sp", ue[:, :, 0:16], i_halo.rearrange("p (g t) -> p g t", g=4), writes=["ue"])
                S.dma("sp", ue[:, :, 16:32], i_u16.rearrange("p (g t) -> p g t", g=4), writes=["ue"])
                if FUSED:
                    flag = sb("flag", [128, 1], stack=ph)
                    S.dma("sp", flag[:], flag_d[:, :], writes=["flag"])
                    V(lambda: nc.vector.tensor_scalar(out=Sin_[:], in0=Sin_[:], scalar1=flag[0:64, 0:1], scalar2=None, op0=ALU.mult), ["Sin", "flag"], ["Sin"])
                    V(lambda: nc.vector.tensor_scalar(out=ue[:, :, 0:16], in0=ue[:, :, 0:16], scalar1=flag[:, 0:1], scalar2=None, op0=ALU.mult), ["ue", "flag"], ["ue"])
                S.dma("sp", invc[:].rearrange("p g t -> p (g t)"), i_invc[:, :], writes=["invc"])
                gon = sb("gon", [128, 4], stack=ph)
                pscale = sb("pscale", [128, 4], stack=ph)
                with nc.allow_non_contiguous_dma(reason="tiny"):
                    S.dma("sp", gon[:], gon_d.rearrange("o (g p) -> p (o g)", p=128), writes=["gon"])
                    S.dma("sp", pscale[:], pscale_d.rearrange("o (g p) -> p (o g)", p=128), writes=["pscale"])
                poolw = sb("poolw", [128, 4, 128], BF16, stack=ph)
                cast_load(poolw[:], poolw_d.rearrange("g c d -> c g d"), lambda t: t[:, 0:512].rearrange("p (g d) -> p g d", g=4), "poolw")
                w_out = sb("w_out", [128, KC, D], BF16, stack=ph)
                load_wcast(w_out, wout_d, D, "w_out")
                zer = sb("zer", [64, 32], stack=ph)
                Ein = sb("Ein", [64, 4, 32], stack=ph)
                E = sb("E", [64, 4, 32], stack=ph)
                Spb = [sb("Spb%d" % i, [64, 128], BF16, stack=ph) for i in range(4)]
                sq = sb("sq", [128, 2, 512], BF16, stack=ph)
                rstd = sb("rstd", [128, 512], stack=ph)
                tmpo = sb("tmpo", [128, 512], stack=ph)
                sw = [sb("sw%d" % i, [128, 32], stack=ph) for i in range(2)]
                p16 = sb("p16", [128, 16], stack=ph)
                p16b = sb("p16b", [128, 16], BF16, stack=ph)

                V(lambda: nc.vector.memset(zer[:], 0.0), [], ["zer"])
                for h in range(4):
                    V(lambda h=h: nc.vector.tensor_tensor_scan(out=Ein[:, h, :], data0=BL[:, h, :], data1=zer[:], initial=0.0, op0=ALU.add, op1=ALU.add),
                      ["BL", "zer"], ["Ein"])
                V(lambda: nc.vector.tensor_tensor(out=Ein[:], in0=Ein[:], in1=BL[:], op=ALU.subtract), ["Ein", "BL"], ["Ein"])
                A(lambda: nc.scalar.activation(out=E[:], in_=Ein[:], func=AF.Exp), ["Ein"], ["E"])
                kk = 0
                for tg in range(4):
                    cols = slice(tg * 512, (tg + 1) * 512)
                    for h in range(4):
                        pb = h % 2
                        for ch in range(8):
                            cg = tg * 8 + ch
                            gcol = slice(tg * 512 + ch * 64, tg * 512 + (ch + 1) * 64)
                            sbi = kk % 4
                            kk += 1
                            V(lambda h=h, cg=cg, sbi=sbi: nc.vector.tensor_scalar(out=Spb[sbi][:], in0=Sin_[:, h, :], scalar1=E[:, h, cg:cg + 1], scalar2=None, op0=ALU.mult),
                              ["Sin", "E"], [("Spb", sbi)])
                            T(lambda h=h, ch=ch, gcol=gcol, sbi=sbi, pb=pb: nc.tensor.matmul(ps[pb][:, ch * 64:(ch + 1) * 64], lhsT=Spb[sbi][:], rhs=qe[:, h, gcol], start=True, stop=True),
                              [("Spb", sbi), "qe"], [("ps", pb)])
                        V(lambda h=h, cols=cols, pb=pb: nc.vector.tensor_tensor(out=o_all[:, h, cols], in0=ps[pb][:], in1=o_all[:, h, cols], op=ALU.add),
                          [("ps", pb), "o_all"], ["o_all"])
                        A(lambda h=h, cols=cols: nc.scalar.activation(out=sq[:, 0, :], in_=o_all[:, h, cols], func=AF.Square), ["o_all"], [("sq", 0)])
                        T(lambda: nc.tensor.matmul(ps[6][:], lhsT=ones_bf[:], rhs=sq[:, 0, :], start=True, stop=True), [("sq", 0), "ones_bf"], [("ps", 6)])
                        rstd_from_ps(rstd[:], 6, 512, 128)
                        V(lambda h=h, cols=cols: nc.vector.scalar_tensor_tensor(out=tmpo[:], in0=o_all[:, h, cols], scalar=gon[:, h:h + 1], in1=rstd[:], op0=ALU.mult, op1=ALU.mult),
                          ["o_all", "gon", "rstd"], ["tmpo"])
                        V(lambda h=h, cols=cols: nc.vector.tensor_tensor(out=sgT[:, h, cols], in0=tmpo[:], in1=sgT[:, h, cols], op=ALU.mult),
                          ["tmpo", "sgT"], ["sgT"])
                for g in range(4):
                    w = 2 ** (g + 1)
                    src = ue[:, g, :]
                    lo, step, k = 0, 1, 0
                    while step < w:
                        lo += step
                        dst = sw[k % 2]
                        G(lambda src=src, dst=dst, lo=lo, step=step: nc.gpsimd.tensor_tensor(out=dst[:, lo:32], in0=src[:, lo:32], in1=src[:, lo - step:32 - step], op=ALU.add),
                          ["ue", ("sw", 0), ("sw", 1)], [("sw", k % 2)])
                        src = dst
                        step *= 2
                        k += 1
                    V(lambda src=src, g=g: nc.vector.tensor_tensor(out=p16[:], in0=src[:, 16:32], in1=invc[:, g, :], op=ALU.mult), [("sw", 0), ("sw", 1), "invc"], ["p16"])
                    V(lambda g=g: nc.vector.tensor_tensor(out=p16b[:], in0=p16[:], in1=ue[:, g, 16:32], op=ALU.subtract), ["p16", "ue"], ["p16b"])
                    T(lambda g=g: nc.tensor.matmul(ps[g % 2][:, 0:16], lhsT=poolw[:, g, :], rhs=p16b[:], start=True, stop=True), ["poolw", "p16b"], [("ps", g % 2)])
                    V(lambda g=g: nc.vector.tensor_scalar(out=pp[:, g, 0:16], in0=ps[g % 2][:, 0:16], scalar1=pscale[:, g:g + 1], scalar2=None, op0=ALU.mult),
                      [("ps", g % 2), "pscale"], ["pp"])
                out_proj(w_out, "w_out", lambda k, cols: (sgT[:, k, cols] if k < 4 else pp[:, k - 4, cols]), lambda k, tg: ["sgT", "pp"])
                S.barrier()

            ffn(wi2_d, wo2_d, g_f2, "g_f2")
            ffn(wi1_d, wo1_d, g_f1, "g_f1")

            with contextlib.ExitStack() as ph:
                w_in = sb("w_in", [128, KC, 1860], BF16, stack=ph)
                load_wcast(w_in, w_in_d, 1860, "w_in")
                w_rot = sb("w_rot", [128, KC, 1600], BF16, stack=ph)
                load_wcast(w_rot, w_rot_d, 1600, "w_rot")
                ropec = sb("ropec", [128, 4], stack=ph)
                S.dma("sp", ropec[:], ropec_d[:, :], writes=["ropec"])
                hnT = sb("hnT", [128, KC, 512], BF16, stack=ph)
                sq = sb("sq", [128, 2, 512], BF16, stack=ph)
                rstd = sb("rstd", [128, 512], stack=ph)
                posi = sb("posi", [128, 512], I32, stack=ph)
                posf = sb("posf", [128, 512], stack=ph)
                ang = sb("ang", [128, 512], stack=ph)
                ang2 = sb("ang2", [128, 512], stack=ph)
                ni = sb("ni", [128, 512], I32, stack=ph)
                nf = sb("nf", [128, 512], stack=ph)
                tabs = {nm: sb(nm, [128, 512], stack=ph) for nm in ("CS128", "SN128", "CS64", "SN64")}
                t1 = sb("t1", [128, 512], stack=ph)
                t2 = sb("t2", [128, 512], stack=ph)
                qbuf = sb("qbuf", [128, 8, 512], BF16, stack=ph)
                kbuf = sb("kbuf", [128, 2, 512], BF16, stack=ph)
                qibuf = sb("qibuf", [64, 4, 512], BF16, stack=ph)
                kibuf = sb("kibuf", [64, 512], BF16, stack=ph)
                vbuf = sb("vbuf", [128, 4, 2, 130], BF16, stack=ph)
                wibuf = sb("wibuf", [128, 4, 8], stack=ph)
                V(lambda: nc.vector.memset(vbuf[:, :, :, 128:129], 1.0), [], ["vbuf"])
                V(lambda: nc.vector.memset(vbuf[:, :, :, 129:130], 0.0), [], ["vbuf"])

                def sin_table(src_ang, out_tab, np_, sgn_col=None):
                    V(lambda: nc.vector.tensor_scalar(out=ni[0:np_, :], in0=src_ang[0:np_, :], scalar1=1.0 / TWO_PI, scalar2=None, op0=ALU.mult), ["ang"], ["ni"])
                    V(lambda: nc.vector.tensor_copy(out=nf[0:np_, :], in_=ni[0:np_, :]), ["ni"], ["nf"])
                    V(lambda: nc.vector.scalar_tensor_tensor(out=t1[0:np_, :], in0=nf[0:np_, :], scalar=-CW1, in1=src_ang[0:np_, :], op0=ALU.mult, op1=ALU.add), ["nf", "ang"], ["t1"])
                    V(lambda: nc.vector.scalar_tensor_tensor(out=t1[0:np_, :], in0=nf[0:np_, :], scalar=-CW2, in1=t1[0:np_, :], op0=ALU.mult, op1=ALU.add), ["nf", "t1"], ["t1"])
                    V(lambda: nc.vector.tensor_scalar(out=t1[0:np_, :], in0=t1[0:np_, :], scalar1=float(np.pi), scalar2=-float(np.pi), op0=ALU.min, op1=ALU.max), ["t1"], ["t1"])
                    A(lambda: nc.scalar.activation(out=out_tab[0:np_, :], in_=t1[0:np_, :], func=AF.Sin), ["t1"], ["tab"])
                    if sgn_col is not None:
                        V(lambda: nc.vector.tensor_scalar(out=out_tab[0:np_, :], in0=out_tab[0:np_, :], scalar1=sgn_col, scalar2=None, op0=ALU.mult), ["tab", "ropec"], ["tab"])

                def rope_proj(c0, r0, m, cs, sn, dst, dkey, pi):
                    pa, pbk = (0, 1) if pi % 2 == 0 else (2, 3)
                    for kc in range(KC):
                        T(lambda kc=kc: nc.tensor.matmul(ps[pa][0:m, :], lhsT=w_in[:, kc, c0:c0 + m], rhs=hnT[:, kc, :], start=(kc == 0), stop=(kc == KC - 1)),
                          ["w_in", "hnT"], [("ps", pa)], inc=(kc == KC - 1))
                    for kc in range(KC):
                        T(lambda kc=kc: nc.tensor.matmul(ps[pbk][0:m, :], lhsT=w_rot[:, kc, r0:r0 + m], rhs=hnT[:, kc, :], start=(kc == 0), stop=(kc == KC - 1)),
                          ["w_rot", "hnT"], [("ps", pbk)], inc=(kc == KC - 1))
                    V(lambda: nc.vector.tensor_tensor(out=t1[0:m, :], in0=ps[pa][0:m, :], in1=cs[0:m, :], op=ALU.mult), [("ps", pa), "tab"], ["t1"])
                    V(lambda: nc.vector.tensor_tensor(out=t2[0:m, :], in0=ps[pbk][0:m, :], in1=sn[0:m, :], op=ALU.mult), [("ps", pbk), "tab"], ["t2"])
                    G(lambda: nc.gpsimd.tensor_tensor(out=dst, in0=t1[0:m, :], in1=t2[0:m, :], op=ALU.add), ["t1", "t2"], [dkey])

                for tg in range(4):
                    cols = slice(tg * 512, (tg + 1) * 512)
                    norm_h(cols, g_mix, "g_mix", sq, rstd, lambda kc: hnT[:, kc, :], "hnT")
                    S.dma("sp", posi[:], pos_d[0:1, cols].to_broadcast([128, 512]), writes=["posi"])
                    V(lambda: nc.vector.tensor_copy(out=posf[:], in_=posi[:]), ["posi"], ["posf"])
                    for (inv_c, sgn_c, np_, csn, snn) in ((0, 1, 128, "CS128", "SN128"), (2, 3, 64, "CS64", "SN64")):
                        V(lambda inv_c=inv_c, np_=np_: nc.vector.tensor_scalar(out=ang[0:np_, :], in0=posf[0:np_, :], scalar1=ropec[0:np_, inv_c:inv_c + 1], scalar2=None, op0=ALU.mult),
                          ["posf", "ropec"], ["ang"])
                        sin_table(ang, tabs[snn], np_, ropec[0:np_, sgn_c:sgn_c + 1])
                        V(lambda np_=np_: nc.vector.tensor_scalar(out=ang2[0:np_, :], in0=ang[0:np_, :], scalar1=float(np.pi / 2), scalar2=None, op0=ALU.add), ["ang"], ["ang"])
                        sin_table(ang2, tabs[csn], np_, None)
                    pi = 0
                    for h in range(8):
                        rope_proj(h * 128, h * 128, 128, tabs["CS128"], tabs["SN128"], qbuf[:, h, :], "qbuf", pi)
                        pi += 1
                    for g in range(2):
                        rope_proj(1024 + g * 128, 1024 + g * 128, 128, tabs["CS128"], tabs["SN128"], kbuf[:, g, :], "kbuf", pi)
                        pi += 1
                    for h in range(4):
                        rope_proj(1536 + h * 64, 1280 + h * 64, 64, tabs["CS64"], tabs["SN64"], qibuf[:, h, :], "qibuf", pi)
                        pi += 1
                    rope_proj(1792, 1536, 64, tabs["CS64"], tabs["SN64"], kibuf[:, :], "kibuf", pi)
                    for j in range(4):
                        tcol = slice(j * 128, (j + 1) * 128)
                        for kc in range(KC):
                            T(lambda kc=kc, tcol=tcol: nc.tensor.matmul(ps[4][:, 0:256], lhsT=hnT[:, kc, tcol], rhs=w_in[:, kc, 1280:1536], start=(kc == 0), stop=(kc == KC - 1)),
                              ["w_in", "hnT"], [("ps", 4)], inc=(kc == KC - 1))
                        A(lambda j=j: nc.scalar.copy(out=vbuf[:, j, :, 0:128], in_=ps[4][:, 0:256].rearrange("p (g d) -> p g d", g=2)), [("ps", 4)], ["vbuf"])
                        for kc in range(KC):
                            T(lambda kc=kc, tcol=tcol: nc.tensor.matmul(ps[5][:, 0:4], lhsT=hnT[:, kc, tcol], rhs=w_in[:, kc, 1856:1860], start=(kc == 0), stop=(kc == KC - 1)),
                              ["w_in", "hnT"], [("ps", 5)], inc=(kc == KC - 1))
                        A(lambda j=j: nc.scalar.activation(out=wibuf[:, j, 0:4], in_=ps[5][:, 0:4], func=AF.Abs), [("ps", 5)], ["wibuf"])
                        A(lambda j=j: nc.scalar.activation(out=wibuf[:, j, 4:8], in_=ps[5][:, 0:4], func=AF.Sign), [("ps", 5)], ["wibuf"])
                    S.dma("sp", o_q.rearrange("p (h t) -> p h t", h=8)[:, :, cols], qbuf[:], reads=["qbuf"])
                    S.dma("sp", o_k.rearrange("p (h t) -> p h t", h=2)[:, :, cols], kbuf[:], reads=["kbuf"])
                    S.dma("sp", o_qi.rearrange("p (h t) -> p h t", h=4)[:, :, cols], qibuf[:], reads=["qibuf"])
                    S.dma("sp", o_ki[:, cols], kibuf[:], reads=["kibuf"])
                    S.dma("sp", ov_parts[tg // 2][:, (tg % 2) * 1040:(tg % 2 + 1) * 1040], vbuf[:].rearrange("p j g d -> p (j g d)"), reads=["vbuf"])
                    S.dma("sp", o_wi[:, tg * 32:(tg + 1) * 32], wibuf[:].rearrange("p j c -> p (j c)"), reads=["wibuf"])
                S.barrier()
            if not FUSED:
                dump3(o_hT, hT, KC, lambda k: [("hT", k)])
            S.barrier()

        if "C" in parts:
            CUR[0] = "C"
            if FUSED:
                for (cn, csrc, cdst) in XCH2:
                    S.collective(cn, lambda csrc=csrc, cdst=cdst: nc.gpsimd.collective_compute("AllGather", ALU.bypass, replica_groups=PAIRS, ins=[csrc[:, :]], outs=[cdst[:, :]]))
            i_hT = din("st_hT", [128, KC * NTOK])
            i_q = din("st_q", [128, 8 * NTOK], BF16)
            i_k = din("st_k", [128, 2 * NTOK], BF16)
            i_qi = din("st_qi", [64, 4 * NTOK], BF16)
            i_ki = din("st_ki", [64, NTOK], BF16)
            i_v = None if FUSED else din("st_v", [128, 16 * 260], BF16)
            i_wi = din("st_wi", [128, 16 * 8])
            x_k = din("x_k", [128, 2 * NTOK], BF16)
            x_ki = din("x_ki", [64, NTOK], BF16)
            x_v = None if FUSED else din("x_v", [128, 16 * 260], BF16)
            pb_d = din("c_pbias", [128, 1])
            caus_d = din("c_caus", [128, 128])
            wout_d = din("odd_w_out", [D, D])
            g_f2 = load_gain(din("g_ffn2_1", [1, D]), "g_f2")
            g_fin = load_gain(din("g_final", [1, D]), "g_fin")
            wi2_d = din("ffn2_wi", [D, 2 * DFF])
            wo2_d = din("ffn2_wo", [DFF, D])
            y_d = dout("y", [NTOK, D])

            if not FUSED:
                load3(hT, i_hT, KC, lambda k: [("hT", k)])
            with contextlib.ExitStack() as pq:
                qT = sb("qT", [128, 8, NTOK], BF16, stack=pq)
                load3(qT, i_q, 8, lambda k: [("qT", i, k // 4) for i in range(16)])
                with contextlib.ExitStack() as ph:
                    kT = sb("kT", [128, 2, SEQ], BF16, stack=ph)
                    S.dma("sp", kT[:, :, 0:NTOK], x_k.rearrange("p (g t) -> p g t", g=2), writes=["kT"])
                    S.dma("sp", kT[:, :, NTOK:SEQ], i_k.rearrange("p (g t) -> p g t", g=2), writes=["kT"])
                    va = sb("va", [128, 32, 260], BF16, stack=ph)
                    xv_parts = [d[0:128, :] for d in xv_dst] if FUSED else [x_v[:, 0:2080], x_v[:, 2080:4160]]
                    iv_parts = xv_src if FUSED else [i_v[:, 0:2080], i_v[:, 2080:4160]]
                    for hv in range(2):
                        S.dma("sp", va[:, hv * 8:(hv + 1) * 8, :], xv_parts[hv].rearrange("p (j c) -> p j c", j=8), writes=["va"])
                        S.dma("sp", va[:, 16 + hv * 8:16 + (hv + 1) * 8, :], iv_parts[hv].rearrange("p (j c) -> p j c", j=8), writes=["va"])
                    kiT = sb("kiT", [64, SEQ], BF16, stack=ph)
                    S.dma("sp", kiT[:, 0:NTOK], x_ki[:, :], writes=["kiT"])
                    S.dma("sp", kiT[:, NTOK:SEQ], i_ki[:, :], writes=["kiT"])
                    qiT = sb("qiT", [64, 4, NTOK], BF16, stack=ph)
                    load3(qiT, i_qi, 4, lambda k: ["qiT"])
                    wi = sb("wi", [128, 16, 8], stack=ph)
                    S.dma("sp", wi[:].rearrange("p j c -> p (j c)"), i_wi[:, :], writes=["wi"])
                    pbc = sb("pbc", [128, 1], stack=ph)
                    S.dma("sp", pbc[:], pb_d[:, :], writes=["pbc"])
                    caus = sb("caus", [128, 128], stack=ph)
                    S.dma("sp", caus[:], caus_d[:, :], writes=["caus"])
                    isc = sb("isc", [128, SEQ], stack=ph)
                    junk = sb("junk", [128, SEQ], BF16, stack=ph)
                    selT = sb("selT", [128, 32, 128], BF16, stack=ph)
                    rl = [sb("rl%d" % i, [128, 512], stack=ph) for i in range(2)]
                    ebuf = [sb("ebuf%d" % i, [128, 512], BF16, stack=ph) for i in range(2)]
                    pT = [sb("pTb%d" % i, [128, 4, 128], BF16, stack=ph) for i in range(2)]
                    otok = sb("otok", [128, 4, 128], BF16, stack=ph)
                    sm = sb("sm", [128, 8], stack=ph)

                    for i in range(16):
                        qc = slice(i * 128, (i + 1) * 128)
                        n_kb = 16 + i + 1
                        n_k = n_kb * 128
                        ngr = (n_k + 511) // 512
                        for kg in range(ngr):
                            k0 = kg * 512
                            w = min(512, n_k - k0)
                            for h in range(4):
                                T(lambda h=h, k0=k0, w=w: nc.tensor.matmul(ps[2 + h][:, 0:w], lhsT=qiT[:, h, qc], rhs=kiT[:, k0:k0 + w], start=True, stop=True),
                                  ["qiT", "kiT"], [("ps", 2 + h)])
                            for h in range(4):
                                rb = h % 2
                                A(lambda h=h, rb=rb, w=w: nc.scalar.activation(out=rl[rb][:, 0:w], in_=ps[2 + h][:, 0:w], func=AF.Relu, scale=wi[:, i, h:h + 1]),
                                  [("ps", 2 + h), "wi"], [("rl", rb)])
                                if h == 0:
                                    V(lambda rb=rb, k0=k0, w=w: nc.vector.tensor_scalar(out=isc[:, k0:k0 + w], in0=rl[rb][:, 0:w], scalar1=wi[:, i, 4:5], scalar2=None, op0=ALU.mult),
                                      [("rl", rb), "wi"], ["isc"])
                                else:
                                    V(lambda h=h, rb=rb, k0=k0, w=w: nc.vector.scalar_tensor_tensor(out=isc[:, k0:k0 + w], in0=rl[rb][:, 0:w], scalar=wi[:, i, 4 + h:5 + h],
                                                                                                    in1=isc[:, k0:k0 + w], op0=ALU.mult, op1=ALU.add),
                                      [("rl", rb), "wi", "isc"], ["isc"])
                        V(lambda: nc.vector.tensor_reduce(out=sm[:, 1:2], in_=isc[:, 0:n_k], axis=AX.X, op=ALU.max), ["isc"], ["sm"])
                        V(lambda: nc.vector.tensor_reduce(out=sm[:, 0:1], in_=isc[:, 0:n_k], axis=AX.X, op=ALU.min), ["isc"], ["sm"])
                        V(lambda: nc.vector.tensor_scalar(out=isc[:, 0:NTOK], in0=isc[:, 0:NTOK], scalar1=pbc[:, 0:1], scalar2=None, op0=ALU.add), ["isc", "pbc"], ["isc"])
                        V(lambda: nc.vector.tensor_tensor(out=isc[:, n_k - 128:n_k], in0=isc[:, n_k - 128:n_k], in1=caus[:], op=ALU.add), ["isc", "caus"], ["isc"])
                        V(lambda: nc.vector.tensor_tensor(out=sm[:, 5:6], in0=sm[:, 1:2], in1=sm[:, 0:1], op=ALU.subtract), ["sm"], ["sm"])
                        for it in range(N_IT):
                            f = float(2.0 ** -(it + 1))
                            V(lambda f=f: nc.vector.scalar_tensor_tensor(out=sm[:, 2:3], in0=sm[:, 5:6], scalar=f, in1=sm[:, 0:1], op0=ALU.mult, op1=ALU.add), ["sm"], ["sm"])
                            V(lambda: nc.vector.tensor_scalar(out=junk[:, 0:n_k], in0=isc[:, 0:n_k], scalar1=sm[:, 2:3], scalar2=None, op0=ALU.is_ge, op1=ALU.add, accum_out=sm[:, 3:4]),
                              ["isc", "sm"], ["junk", "sm"])
                            V(lambda f=f: nc.vector.tensor_scalar(out=sm[:, 4:5], in0=sm[:, 3:4], scalar1=255.5, scalar2=f, op0=ALU.is_ge, op1=ALU.mult), ["sm"], ["sm"])
                            V(lambda: nc.vector.scalar_tensor_tensor(out=sm[:, 0:1], in0=sm[:, 4:5], scalar=sm[:, 5:6], in1=sm[:, 0:1], op0=ALU.mult, op1=ALU.add), ["sm"], ["sm"])
                        V(lambda: nc.vector.tensor_scalar(out=junk[:, 0:n_k], in0=isc[:, 0:n_k], scalar1=sm[:, 0:1], scalar2=None, op0=ALU.is_ge), ["isc", "sm"], ["junk"])
                        for kb0 in range(0, n_kb, 4):
                            nb = min(4, n_kb - kb0)
                            half = 0
                            for b in range(nb):
                                kb = kb0 + b
                                T(lambda b=b, kb=kb, half=half: nc.tensor.transpose(psb[:, half * 512 + b * 128:half * 512 + (b + 1) * 128], junk[:, kb * 128:(kb + 1) * 128], ident_bf[:]),
                                  ["junk", "ident_bf"], ["psb"], inc=(b == nb - 1))
                            A(lambda kb0=kb0, nb=nb, half=half: nc.scalar.copy(out=selT[:, kb0:kb0 + nb, :], in_=psb[:, half * 512:half * 512 + nb * 128].rearrange("p (b t) -> p b t", b=nb)),
                              ["psb"], [("selT", kb0)])
                        for g in range(2):
                            def score(kb, g=g):
                                sc = kb % 2
                                T(lambda kb=kb, g=g, sc=sc: nc.tensor.matmul(ps[sc][:], lhsT=kT[:, g, kb * 128:(kb + 1) * 128], rhs=qT[:, 4 * g:4 * g + 4, qc], start=True, stop=True),
                                  ["kT", ("qT", i, g)], [("ps", sc)])

                            score(0)
                            for kb in range(n_kb):
                                sc = kb % 2
                                if kb + 1 < n_kb:
                                    score(kb + 1)
                                A(lambda sc=sc: nc.scalar.activation(out=ebuf[sc][:], in_=ps[sc][:], func=AF.Exp, scale=float(128 ** -0.5)), [("ps", sc)], [("ebuf", sc)])
                                V(lambda kb=kb, sc=sc: nc.vector.tensor_tensor(out=pT[sc][:], in0=ebuf[sc][:].rearrange("p (h t) -> p h t", h=4),
                                                                              in1=selT[:, kb:kb + 1, :].to_broadcast([128, 4, 128]), op=ALU.mult),
                                  [("ebuf", sc), ("selT", (kb // 4) * 4)], [("pT", sc)])
                                for hh in range(4):
                                    T(lambda kb=kb, g=g, sc=sc, hh=hh: nc.tensor.matmul(ps[2 + hh][:, 0:129], lhsT=pT[sc][:, hh, :], rhs=va[:, kb, g * 130:g * 130 + 129],
                                                                                        start=(kb == 0), stop=(kb == n_kb - 1)),
                                      [("pT", sc), "va"], [("ps", 2 + hh)], inc=(hh == 3 or kb == n_kb - 1))
                            for hh in range(4):
                                V(lambda hh=hh: nc.vector.reciprocal(out=sm[:, 6:7], in_=ps[2 + hh][:, 128:129]), [("ps", 2 + hh)], ["sm"])
                                V(lambda hh=hh: nc.vector.tensor_scalar(out=otok[:, hh, :], in0=ps[2 + hh][:, 0:128], scalar1=sm[:, 6:7], scalar2=None, op0=ALU.mult),
                                  [("ps", 2 + hh), "sm"], ["otok"])
                            for hh in range(4):
                                T(lambda hh=hh: nc.tensor.transpose(psb[:, hh * 128:(hh + 1) * 128], otok[:, hh, :], ident_bf[:]), ["otok", "ident_bf"], ["psb"], inc=(hh == 3))
                            A(lambda g=g: nc.scalar.copy(out=qT[:, 4 * g:4 * g + 4, qc], in_=psb[:, 0:512].rearrange("p (h t) -> p h t", h=4)), ["psb"], [("qT", i, g)])
                    S.barrier()
                with contextlib.ExitStack() as ph:
                    w_out = sb("w_out", [128, KC, D], BF16, stack=ph)
                    load_wcast(w_out, wout_d, D, "w_out")
                    out_proj(w_out, "w_out", lambda k, cols: qT[:, k, cols], lambda k, tg: [("qT", 4 * tg + j, gg) for j in range(4) for gg in range(2)])
                    S.barrier()

            ffn(wi2_d, wo2_d, g_f2, "g_f2")

            with contextlib.ExitStack() as ph:
                sq = sb("sq", [128, 2, 512], BF16, stack=ph)
                rstd = sb("rstd", [128, 512], stack=ph)
                yT = sb("yT", [128, KC, 512], stack=ph)
                yo = [sb("yo%d" % i, [128, D], stack=ph) for i in range(2)]
                io = 0
                for tg in range(4):
                    cols = slice(tg * 512, (tg + 1) * 512)
                    rstd_ps(lambda kc: hT[:, kc, cols], KC, 512, sq, 6, lambda kc: [("hT", kc)])
                    rstd_from_ps(rstd[:], 6, 512, D)
                    for kc in range(KC):
                        V(lambda kc=kc, cols=cols: nc.vector.scalar_tensor_tensor(out=yT[:, kc, :], in0=hT[:, kc, cols], scalar=g_fin[:, kc:kc + 1], in1=rstd[:],
                                                                                 op0=ALU.mult, op1=ALU.mult),
                          [("hT", kc), "g_fin", "rstd"], [("yT", kc)])
                    for j in range(4):
                        yb = io % 2
                        io += 1
                        for k2 in range(2):
                            pb = k2
                            for kq in range(4):
                                kc = k2 * 4 + kq
                                T(lambda kc=kc, kq=kq, j=j, pb=pb: nc.tensor.transpose(ps[pb][:, kq * 128:(kq + 1) * 128], yT[:, kc, j * 128:(j + 1) * 128], ident[:]),
                                  [("yT", kc), "ident"], [("ps", pb)], inc=(kq == 3))
                            if k2 == 0:
                                V(lambda yb=yb, pb=pb: nc.vector.tensor_copy(out=yo[yb][:, 0:512], in_=ps[pb][:]), [("ps", pb)], [("yo", yb)])
                            else:
                                A(lambda yb=yb, pb=pb: nc.scalar.copy(out=yo[yb][:, 512:1024], in_=ps[pb][:]), [("ps", pb)], [("yo", yb)])
                        tt = tg * 4 + j
                        S.dma("sp", y_d[tt * 128:(tt + 1) * 128, :], yo[yb][:], reads=[("yo", yb)], writes=[("y_dram", tt)])
                S.barrier()

        S.barrier()
    return nc


_PROG = {}


def _prog(launch):
    if launch not in _PROG:
        _PROG[launch] = build_program(launch)
    return _PROG[launch]


def _f32(a):
    return np.ascontiguousarray(np.asarray(a), dtype=np.float32)


def _consts():
    c = {"c_ident": np.eye(128, dtype=np.float32)}
    t = np.arange(64)
    c["c_tin64"] = np.where(t[:, None] <= t[None, :], -1.0 / 16.0, 0.0).astype(np.float32)
    c["c_uex64"] = np.where(t[:, None] > t[None, :], -1.0 / 16.0, 0.0).astype(np.float32)
    cm = (t[:, None] <= t[None, :]).astype(np.float32)
    c["c_cmask"] = np.tile(cm, (1, 4)).astype(np.float32)
    s = np.arange(128)
    c["c_caus"] = np.where(s[None, :] <= s[:, None], 0.0, NEG).astype(np.float32)
    r = np.arange(128)
    inv128 = (10000.0 ** (-(np.arange(0, 128, 2, dtype=np.float32)) / 128.0)).astype(np.float32)
    inv64 = (10000.0 ** (-(np.arange(0, 64, 2, dtype=np.float32)) / 64.0)).astype(np.float32)
    rope = np.zeros((128, 4), np.float32)
    rope[:, 0] = inv128[r % 64]
    rope[:, 1] = np.where(r < 64, -1.0, 1.0)
    rope[:, 2] = inv64[r % 32]
    rope[:, 3] = np.where((r % 64) < 32, -1.0, 1.0)
    c["c_rope"] = rope
    return c


def _rot_perm(n_heads, hd):
    idx = np.arange(n_heads * hd).reshape(n_heads, hd)
    return np.concatenate([idx[:, hd // 2:], idx[:, :hd // 2]], axis=1).reshape(-1)


def _run(launch, in_maps):
    nc = _prog(launch)
    ncores = int(os.environ.get("KCORES", str(N_CORES)))
    res = run_bass_kernel_spmd(nc, in_maps[:ncores], core_ids=list(range(ncores)))
    r = list(res.results)
    return r + [r[i % ncores] for i in range(ncores, N_CORES)]


def _kernel_fused(inputs):
    x = _f32(inputs["x"])
    pos = np.ascontiguousarray(inputs["positions"], dtype=np.int32)
    C = _consts()

    def row(a):
        return _f32(a).reshape(1, -1)

    gate_wb = np.concatenate([_f32(inputs["gla_gate_w"])[0], _f32(inputs["gla_gate_b"])[0][None, :]], axis=0)
    w_in1 = _f32(inputs["odd_w_in"][0])
    perm = np.concatenate([_rot_perm(8, 128), 1024 + _rot_perm(2, 128), 1536 + _rot_perm(4, 64), 1792 + _rot_perm(1, 64)])
    w_rot = np.ascontiguousarray(w_in1[:, perm])
    pa = {
        "c_tin64": C["c_tin64"], "c_uex64": C["c_uex64"], "c_cmask": C["c_cmask"],
        "g_ffn1_0": row(inputs["ffn1_norm"][0]), "g_mix0": row(inputs["mix_norm"][0]),
        "ffn_wi": _f32(inputs["ffn1_wi"][0]), "ffn_wo": _f32(inputs["ffn1_wo"][0]),
        "even_w_in": _f32(inputs["even_w_in"][0]), "gate_wb": _f32(gate_wb),
        "pool_w": _f32(inputs["pool_w"][0]), "pool_scale": row(inputs["pool_scale"][0]),
    }
    pb = {
        "c_rope": C["c_rope"],
        "gla_out_norm": row(inputs["gla_out_norm"][0]), "pool_w": _f32(inputs["pool_w"][0]), "pool_scale": row(inputs["pool_scale"][0]),
        "even_w_out": _f32(inputs["even_w_out"][0]),
        "g_ffn2_0": row(inputs["ffn2_norm"][0]), "g_ffn1_1": row(inputs["ffn1_norm"][1]), "g_mix1": row(inputs["mix_norm"][1]),
        "ffn2_wi": _f32(inputs["ffn2_wi"][0]), "ffn2_wo": _f32(inputs["ffn2_wo"][0]),
        "ffn1_wi": _f32(inputs["ffn1_wi"][1]), "ffn1_wo": _f32(inputs["ffn1_wo"][1]),
        "odd_w_in": w_in1, "odd_w_rot": w_rot,
    }
    pc = {
        "c_caus": C["c_caus"], "odd_w_out": _f32(inputs["odd_w_out"][0]),
        "g_ffn2_1": row(inputs["ffn2_norm"][1]), "g_final": row(inputs["final_norm"]),
        "ffn2_wi": _f32(inputs["ffn2_wi"][1]), "ffn2_wo": _f32(inputs["ffn2_wo"][1]),
    }
    shared = {"c_ident": C["c_ident"]}
    for pfx, d in (("A", pa), ("B", pb), ("C", pc)):
        for k, v in d.items():
            shared[pfx + "_" + k] = v
    tloc = np.arange(16, dtype=np.float32)
    maps = []
    for c in range(N_CORES):
        b, half = c // 2, c % 2
        m = dict(shared)
        m["A_x"] = np.ascontiguousarray(x[b, half * NTOK:(half + 1) * NTOK, :])
        invc = np.zeros((128, 4, 16), np.float32)
        for g, w in enumerate((2, 4, 8, 16)):
            cnt = np.full(16, float(w), np.float32) if half == 1 else np.minimum(tloc + 1.0, float(w))
            invc[:, g, :] = (1.0 / cnt)[None, :]
        m["B_c_invcnt"] = invc.reshape(128, 64)
        m["B_positions"] = np.ascontiguousarray(pos[b, half * NTOK:(half + 1) * NTOK]).reshape(1, NTOK)
        m["B_c_flag"] = np.full((128, 1), float(half), np.float32)
        m["C_c_pbias"] = np.zeros((128, 1), np.float32) if half == 1 else np.full((128, 1), NEG, np.float32)
        maps.append(m)
    rc = _run("F", maps)
    out = np.zeros((4, SEQ, D), dtype=np.float32)
    for c in range(N_CORES):
        b, half = c // 2, c % 2
        out[b, half * NTOK:(half + 1) * NTOK, :] = rc[c]["y"]
    return out


def kernel(**inputs):
    debug = inputs.pop("_debug", None)
    if debug is None and os.environ.get("KUNFUSED") != "1":
        return _kernel_fused(inputs)
    x = _f32(inputs["x"])
    pos = np.ascontiguousarray(inputs["positions"], dtype=np.int32)
    C = _consts()
    bf = ml_dtypes.bfloat16

    def row(a):
        return _f32(a).reshape(1, -1)

    gate_wb = np.concatenate([_f32(inputs["gla_gate_w"])[0], _f32(inputs["gla_gate_b"])[0][None, :]], axis=0)
    shared = {
        "c_ident": C["c_ident"], "c_tin64": C["c_tin64"], "c_uex64": C["c_uex64"], "c_cmask": C["c_cmask"],
        "g_ffn1_0": row(inputs["ffn1_norm"][0]), "g_mix0": row(inputs["mix_norm"][0]),
        "ffn_wi": _f32(inputs["ffn1_wi"][0]), "ffn_wo": _f32(inputs["ffn1_wo"][0]),
        "even_w_in": _f32(inputs["even_w_in"][0]), "gate_wb": _f32(gate_wb),
        "pool_w": _f32(inputs["pool_w"][0]), "pool_scale": row(inputs["pool_scale"][0]),
    }
    maps = []
    for c in range(N_CORES):
        b, half = c // 2, c % 2
        m = dict(shared)
        m["x"] = np.ascontiguousarray(x[b, half * NTOK:(half + 1) * NTOK, :])
        maps.append(m)
    if LITE:
        for m in maps:
            m.pop("ffn_wi"); m.pop("ffn_wo")
    ra = _run("A", maps)
    if debug == "A":
        return ra

    w_in1 = _f32(inputs["odd_w_in"][0])
    perm = np.concatenate([_rot_perm(8, 128), 1024 + _rot_perm(2, 128), 1536 + _rot_perm(4, 64), 1792 + _rot_perm(1, 64)])
    w_rot = np.ascontiguousarray(w_in1[:, perm])
    shared = {
        "c_ident": C["c_ident"], "c_rope": C["c_rope"],
        "gla_out_norm": row(inputs["gla_out_norm"][0]), "pool_w": _f32(inputs["pool_w"][0]), "pool_scale": row(inputs["pool_scale"][0]),
        "even_w_out": _f32(inputs["even_w_out"][0]),
        "g_ffn2_0": row(inputs["ffn2_norm"][0]), "g_ffn1_1": row(inputs["ffn1_norm"][1]), "g_mix1": row(inputs["mix_norm"][1]),
        "ffn2_wi": _f32(inputs["ffn2_wi"][0]), "ffn2_wo": _f32(inputs["ffn2_wo"][0]),
        "ffn1_wi": _f32(inputs["ffn1_wi"][1]), "ffn1_wo": _f32(inputs["ffn1_wo"][1]),
        "odd_w_in": w_in1, "odd_w_rot": w_rot,
    }
    tloc = np.arange(16, dtype=np.float32)
    maps = []
    for c in range(N_CORES):
        b, half = c // 2, c % 2
        m = dict(shared)
        for k in ("st_hT", "st_o", "st_qe", "st_sg", "st_pp", "st_bl", "st_u16"):
            m[k] = ra[c][k]
        if half == 1:
            m["x_sin"] = ra[c - 1]["st_send"]
            m["x_halo"] = ra[c - 1]["st_utail"]
        else:
            m["x_sin"] = np.zeros((64, 512), np.float32)
            m["x_halo"] = np.zeros((128, 64), np.float32)
        invc = np.zeros((128, 4, 16), np.float32)
        for g, w in enumerate((2, 4, 8, 16)):
            cnt = np.full(16, float(w), np.float32) if half == 1 else np.minimum(tloc + 1.0, float(w))
            invc[:, g, :] = (1.0 / cnt)[None, :]
        m["c_invcnt"] = invc.reshape(128, 64)
        m["positions"] = np.ascontiguousarray(pos[b, half * NTOK:(half + 1) * NTOK]).reshape(1, NTOK)
        maps.append(m)
    rb = _run("B", maps)
    if debug == "B":
        return ra, rb

    shared = {
        "c_ident": C["c_ident"], "c_caus": C["c_caus"],
        "odd_w_out": _f32(inputs["odd_w_out"][0]),
        "g_ffn2_1": row(inputs["ffn2_norm"][1]), "g_final": row(inputs["final_norm"]),
        "ffn2_wi": _f32(inputs["ffn2_wi"][1]), "ffn2_wo": _f32(inputs["ffn2_wo"][1]),
    }
    maps = []
    for c in range(N_CORES):
        b, half = c // 2, c % 2
        m = dict(shared)
        m["st_hT"] = rb[c]["st_hT_o"]
        for k in ("st_q", "st_k", "st_qi", "st_ki", "st_v", "st_wi"):
            m[k] = rb[c][k]
        if half == 1:
            m["x_k"], m["x_ki"], m["x_v"] = rb[c - 1]["st_k"], rb[c - 1]["st_ki"], rb[c - 1]["st_v"]
            m["c_pbias"] = np.zeros((128, 1), np.float32)
        else:
            m["x_k"] = np.zeros((128, 2 * NTOK), bf)
            m["x_ki"] = np.zeros((64, NTOK), bf)
            m["x_v"] = np.zeros((128, 16 * 260), bf)
            m["c_pbias"] = np.full((128, 1), NEG, np.float32)
        maps.append(m)
    rc = _run("C", maps)
    out = np.zeros((4, SEQ, D), dtype=np.float32)
    for c in range(N_CORES):
        b, half = c // 2, c % 2
        out[b, half * NTOK:(half + 1) * NTOK, :] = rc[c]["y"]
    return out
```
